# Optimizing a Trainium2 kernel written in Bass

```python
import jax, jax.numpy as jnp
from jax import lax
import numpy as np

D_MODEL = 1024
BATCH = 32
SEQ = 2048
DEPTH = 4

HEAD_DIM = 64
N_MIXERS = 4
GROUP_WIDTH = D_MODEL // N_MIXERS
GROUP_HEADS = GROUP_WIDTH // HEAD_DIM
MIX_WIDTH = N_MIXERS * GROUP_WIDTH
BLOCK = 128
GMLP_CHUNK = 128
MLA_Q_RANK = 192
MLA_KV_RANK = 128
MLA_NOPE_DIM = 64
MLA_ROPE_DIM = 32
MLA_V_DIM = 64
DILATED_BRANCHES = ((128, 1), (512, 4), (2048, 16))
MLSTM_CHUNK = 128
CONV_WIDTH = 4
D_FF = ((8 * D_MODEL + 3 * 256 - 1) // (3 * 256)) * 256
ROPE_THETA = 10000.0
LN_EPS = 1e-5
RMS_EPS = 1e-6
DEEPNORM_ALPHA = (2 * DEPTH) ** 0.25
DEEPNORM_BETA = (8 * DEPTH) ** -0.25
A_COLS = 2 * GROUP_WIDTH
B_COLS = MLA_Q_RANK + MLA_KV_RANK + MLA_ROPE_DIM
C_COLS = 3 * GROUP_WIDTH
D_COLS = 4 * GROUP_WIDTH + 2 * GROUP_HEADS
IN_COLS = A_COLS + B_COLS + C_COLS + D_COLS

kernel_name = 'hymba_style_gmlp_mla_dilated_mlstm_deepnorm'


def _layer_norm(x, g, b):
    xf = x.astype(jnp.float32)
    mu = xf.mean(-1, keepdims=True)
    var = jnp.square(xf - mu).mean(-1, keepdims=True)
    return ((xf - mu) * lax.rsqrt(var + LN_EPS) * g + b).astype(x.dtype)


def _rms_norm(x, g):
    xf = x.astype(jnp.float32)
    return (xf * lax.rsqrt(jnp.square(xf).mean(-1, keepdims=True) + RMS_EPS) * g).astype(x.dtype)


def _rope(x, pos):
    half = x.shape[-1] // 2
    inv = jnp.power(ROPE_THETA, -jnp.arange(half, dtype=jnp.float32) / half)
    ang = pos.astype(jnp.float32)[..., None] * inv
    cos = jnp.cos(ang)[:, :, None, :]
    sin = jnp.sin(ang)[:, :, None, :]
    x1 = x[..., :half].astype(jnp.float32)
    x2 = x[..., half:].astype(jnp.float32)
    return jnp.concatenate([x1 * cos - x2 * sin, x1 * sin + x2 * cos], -1).astype(x.dtype)


def _causal_block_attention(q, k, v, scale):
    bsz, seq, heads, dq = q.shape
    nb = seq // BLOCK
    qb = q.reshape(bsz, nb, BLOCK, heads, dq).swapaxes(0, 1)
    key_pos = jnp.arange(seq)

    def one_block(args):
        q_blk, start = args
        s = jnp.einsum('bqhd,bkhd->bhqk', q_blk, k).astype(jnp.float32) * scale
        q_pos = start + jnp.arange(BLOCK)
        s = jnp.where(key_pos[None, :] <= q_pos[:, None], s, -jnp.inf)
        p = jax.nn.softmax(s, axis=-1).astype(v.dtype)
        return jnp.einsum('bhqk,bkhd->bqhd', p, v)

    out = lax.map(one_block, (qb, jnp.arange(nb, dtype=jnp.int32) * BLOCK))
    return out.swapaxes(0, 1).reshape(bsz, seq, heads, v.shape[-1])


def _gmlp_mixer(z, ln_g, ln_b, w_s, b_s):
    bsz, seq, _ = z.shape
    u, v = jnp.split(jax.nn.gelu(z, approximate=False), 2, axis=-1)
    v = _layer_norm(v, ln_g, ln_b)
    n_chunks = seq // GMLP_CHUNK
    v = v.reshape(bsz, n_chunks, GMLP_CHUNK, GROUP_HEADS, HEAD_DIM)
    causal = jnp.tril(jnp.ones((GMLP_CHUNK, GMLP_CHUNK), dtype=bool))
    w_causal = jnp.where(causal[None], w_s, 0.0)
    mixed = jnp.einsum('hts,bcshd->bcthd', w_causal, v) + b_s.T[None, None, :, :, None]
    return u * mixed.reshape(bsz, seq, GROUP_WIDTH)


def _mla_mixer(z, pos, q_norm_g, kv_norm_g, w_uq, w_ukv):
    bsz, seq, _ = z.shape
    c_q, c_kv, k_r = jnp.split(z, [MLA_Q_RANK, MLA_Q_RANK + MLA_KV_RANK], axis=-1)
    q = (_rms_norm(c_q, q_norm_g) @ w_uq).reshape(bsz, seq, GROUP_HEADS, MLA_NOPE_DIM + MLA_ROPE_DIM)
    q = jnp.concatenate([q[..., :MLA_NOPE_DIM], _rope(q[..., MLA_NOPE_DIM:], pos)], -1)
    kv = (_rms_norm(c_kv, kv_norm_g) @ w_ukv).reshape(bsz, seq, GROUP_HEADS, MLA_NOPE_DIM + MLA_V_DIM)
    k_nope, v = kv[..., :MLA_NOPE_DIM], kv[..., MLA_NOPE_DIM:]
    k_rope = jnp.broadcast_to(_rope(k_r[:, :, None, :], pos), (bsz, seq, GROUP_HEADS, MLA_ROPE_DIM))
    k = jnp.concatenate([k_nope, k_rope], -1)
    o = _causal_block_attention(q, k, v, (MLA_NOPE_DIM + MLA_ROPE_DIM) ** -0.5)
    return o.reshape(bsz, seq, GROUP_HEADS * MLA_V_DIM)


def _dilated_branch(q, k, v, window, dilation):
    bsz, seq, heads, dh = q.shape
    n_back = window // dilation
    span = dilation * BLOCK
    padded = -(-seq // span) * span
    sub_len = padded // dilation
    nb = sub_len // BLOCK

    def to_residue_blocks(x):
        x = jnp.pad(x, ((0, 0), (0, padded - seq), (0, 0), (0, 0)))
        x = x.reshape(bsz, sub_len, dilation, heads, dh).transpose(0, 2, 1, 3, 4)
        return x.reshape(bsz * dilation, nb, BLOCK, heads, dh)

    qb, kb, vb = to_residue_blocks(q), to_residue_blocks(k), to_residue_blocks(v)
    k_prev = jnp.pad(kb, ((0, 0), (1, 0), (0, 0), (0, 0), (0, 0)))[:, :-1]
    v_prev = jnp.pad(vb, ((0, 0), (1, 0), (0, 0), (0, 0), (0, 0)))[:, :-1]
    kk = jnp.concatenate([k_prev, kb], axis=2)
    vv = jnp.concatenate([v_prev, vb], axis=2)
    s = jnp.einsum('gnqhd,gnkhd->gnhqk', qb, kk).astype(jnp.float32) * (dh ** -0.5)
    rel = (BLOCK + jnp.arange(BLOCK))[:, None] - jnp.arange(2 * BLOCK)[None, :]
    band = (rel >= 0) & (rel <= n_back)
    not_pad = (jnp.arange(nb)[:, None, None] > 0) | (jnp.arange(2 * BLOCK)[None, None, :] >= BLOCK)
    mask = band[None] & not_pad
    s = jnp.where(mask[None, :, None], s, -jnp.inf)
    lse = jax.nn.logsumexp(s, axis=-1)
    p = jnp.exp(s - lse[..., None]).astype(v.dtype)
    o = jnp.einsum('gnhqk,gnkhd->gnqhd', p, vv)
    o = o.reshape(bsz, dilation, sub_len, heads, dh).transpose(0, 2, 1, 3, 4)
    o = o.reshape(bsz, padded, heads, dh)[:, :seq]
    lse = lse.transpose(0, 1, 3, 2).reshape(bsz, dilation, sub_len, heads).transpose(0, 2, 1, 3)
    lse = lse.reshape(bsz, padded, heads)[:, :seq]
    return o, lse


def _dilated_mixer(z, pos):
    bsz, seq, _ = z.shape
    q, k, v = [t.reshape(bsz, seq, GROUP_HEADS, HEAD_DIM) for t in jnp.split(z, 3, axis=-1)]
    q, k = _rope(q, pos), _rope(k, pos)
    outs, lses = [], []
    for window, dilation in DILATED_BRANCHES:
        o, lse = _dilated_branch(q, k, v, window, dilation)
        outs.append(o)
        lses.append(lse)
    w = jax.nn.softmax(jnp.stack(lses, 0), axis=0).astype(v.dtype)
    o = jnp.einsum('rbsh,rbshd->bshd', w, jnp.stack(outs, 0))
    return o.reshape(bsz, seq, GROUP_WIDTH)


def _causal_conv(x, w, b):
    ch = x.shape[-1]
    y = lax.conv_general_dilated(x, w[:, None, :], window_strides=(1,), padding=[(CONV_WIDTH - 1, 0)],
                                 dimension_numbers=('NWC', 'WIO', 'NWC'), feature_group_count=ch)
    return y + b


def _mlstm_chunkwise(q, k, v, i_pre, f_pre):
    dtype = v.dtype
    q, k, v = q.astype(jnp.float32), k.astype(jnp.float32), v.astype(jnp.float32)
    i_log = i_pre.astype(jnp.float32)
    f_log = jax.nn.log_sigmoid(f_pre.astype(jnp.float32))
    bsz, seq, heads, dh = q.shape
    n_chunks = seq // MLSTM_CHUNK
    causal = jnp.tril(jnp.ones((MLSTM_CHUNK, MLSTM_CHUNK), dtype=bool))

    def chunks(x):
        return x.reshape((bsz, n_chunks, MLSTM_CHUNK) + x.shape[2:]).swapaxes(0, 1)

    def step(carry, xs):
        c_mat, n_vec, m_run = carry
        qc, kc, vc, ic, fc = xs
        b_cum = jnp.cumsum(fc, axis=1).transpose(0, 2, 1)
        ic = ic.transpose(0, 2, 1)
        d_log = b_cum[:, :, :, None] - b_cum[:, :, None, :] + ic[:, :, None, :]
        d_log = jnp.where(causal, d_log, -jnp.inf)
        m_inter = b_cum + m_run[:, :, None]
        m_t = jnp.maximum(m_inter, d_log.max(-1))
        w_intra = jnp.exp(d_log - m_t[..., None])
        sqk = jnp.einsum('bthd,bshd->bhts', qc, kc) * w_intra
        scale = jnp.exp(m_inter - m_t)
        num = jnp.einsum('bhts,bshd->bthd', sqk, vc) + \
            scale.transpose(0, 2, 1)[..., None] * jnp.einsum('bthk,bhkv->bthv', qc, c_mat)
        den = sqk.sum(-1) + scale * jnp.einsum('bthk,bhk->bht', qc, n_vec)
        h = num / jnp.maximum(jnp.abs(den), jnp.exp(-m_t)).transpose(0, 2, 1)[..., None]
        b_end = b_cum[:, :, -1]
        w_end = b_end[:, :, None] - b_cum + ic
        m_new = jnp.maximum(b_end + m_run, w_end.max(-1))
        decay = jnp.exp(b_end + m_run - m_new)
        w_k = jnp.exp(w_end - m_new[..., None])
        c_mat = decay[..., None, None] * c_mat + jnp.einsum('bhs,bshk,bshv->bhkv', w_k, kc, vc)
        n_vec = decay[..., None] * n_vec + jnp.einsum('bhs,bshk->bhk', w_k, kc)
        return (c_mat, n_vec, m_new), h

    init = (jnp.zeros((bsz, heads, dh, dh), jnp.float32),
            jnp.zeros((bsz, heads, dh), jnp.float32),
            jnp.zeros((bsz, heads), jnp.float32))
    _, h = lax.scan(step, init, (chunks(q), chunks(k), chunks(v), chunks(i_log), chunks(f_log)))
    return h.swapaxes(0, 1).reshape(bsz, seq, heads, dh).astype(dtype)


def _mlstm_mixer(z, conv_w, conv_b, igate_b, fgate_b):
    bsz, seq, _ = z.shape
    qk, v, o, gates = jnp.split(z, [2 * GROUP_WIDTH, 3 * GROUP_WIDTH, 4 * GROUP_WIDTH], axis=-1)
    qk = jax.nn.silu(_causal_conv(qk, conv_w, conv_b))
    q, k = jnp.split(qk, 2, axis=-1)
    q = q.reshape(bsz, seq, GROUP_HEADS, HEAD_DIM)
    k = k.reshape(bsz, seq, GROUP_HEADS, HEAD_DIM) * (HEAD_DIM ** -0.5)
    v = v.reshape(bsz, seq, GROUP_HEADS, HEAD_DIM)
    i_pre = gates[..., :GROUP_HEADS] + igate_b
    f_pre = gates[..., GROUP_HEADS:] + fgate_b
    h = _mlstm_chunkwise(q, k, v, i_pre, f_pre)
    return jax.nn.sigmoid(o) * h.reshape(bsz, seq, GROUP_WIDTH)


def setup_inputs(seed: int = 0) -> dict:
    key = jax.random.key(seed)
    ks = jax.random.split(key, 23)

    def nrm(k, shape, scale):
        return jax.random.normal(k, shape, jnp.float32) * scale

    L = DEPTH
    x = nrm(ks[0], (BATCH, SEQ, D_MODEL), 1.0)
    offsets = jax.random.randint(ks[1], (BATCH, 1), 0, 4096, dtype=jnp.int32)
    positions = offsets + jnp.arange(SEQ, dtype=jnp.int32)[None, :]
    return {
        'x': x,
        'positions': positions,
        'w_in': nrm(ks[2], (L, D_MODEL, IN_COLS), D_MODEL ** -0.5),
        'a_ln_g': 1.0 + nrm(ks[3], (L, GROUP_WIDTH), 0.02),
        'a_ln_b': nrm(ks[4], (L, GROUP_WIDTH), 0.02),
        'a_ws': nrm(ks[5], (L, GROUP_HEADS, GMLP_CHUNK, GMLP_CHUNK), GMLP_CHUNK ** -0.5),
        'a_bs': 1.0 + nrm(ks[6], (L, GROUP_HEADS, GMLP_CHUNK), 0.02),
        'b_q_norm': 1.0 + nrm(ks[7], (L, MLA_Q_RANK), 0.02),
        'b_kv_norm': 1.0 + nrm(ks[8], (L, MLA_KV_RANK), 0.02),
        'b_w_uq': nrm(ks[9], (L, MLA_Q_RANK, GROUP_HEADS * (MLA_NOPE_DIM + MLA_ROPE_DIM)), MLA_Q_RANK ** -0.5),
        'b_w_ukv': nrm(ks[10], (L, MLA_KV_RANK, GROUP_HEADS * (MLA_NOPE_DIM + MLA_V_DIM)), MLA_KV_RANK ** -0.5),
        'd_conv_w': nrm(ks[11], (L, CONV_WIDTH, 2 * GROUP_WIDTH), CONV_WIDTH ** -0.5),
        'd_conv_b': nrm(ks[12], (L, 2 * GROUP_WIDTH), 0.02),
        'd_igate_b': nrm(ks[13], (L, GROUP_HEADS), 0.1),
        'd_fgate_b': jnp.linspace(3.0, 6.0, GROUP_HEADS, dtype=jnp.float32)[None, :] + nrm(ks[14], (L, GROUP_HEADS), 0.02),
        'w_out': nrm(ks[15], (L, MIX_WIDTH, D_MODEL), DEEPNORM_BETA * MIX_WIDTH ** -0.5),
        'ln1_g': 1.0 + nrm(ks[16], (L, D_MODEL), 0.02),
        'ln1_b': nrm(ks[17], (L, D_MODEL), 0.02),
        'w_gate': nrm(ks[18], (L, D_MODEL, D_FF), D_MODEL ** -0.5),
        'w_up': nrm(ks[19], (L, D_MODEL, D_FF), D_MODEL ** -0.5),
        'w_down': nrm(ks[20], (L, D_FF, D_MODEL), DEEPNORM_BETA * D_FF ** -0.5),
        'ln2_g': 1.0 + nrm(ks[21], (L, D_MODEL), 0.02),
        'ln2_b': nrm(ks[22], (L, D_MODEL), 0.02),
    }


def reference(x, positions, w_in, a_ln_g, a_ln_b, a_ws, a_bs, b_q_norm, b_kv_norm, b_w_uq, b_w_ukv,
              d_conv_w, d_conv_b, d_igate_b, d_fgate_b, w_out, ln1_g, ln1_b, w_gate, w_up, w_down,
              ln2_g, ln2_b):
    for l in range(DEPTH):
        z = x @ w_in[l]
        z_a, z_b, z_c, z_d = jnp.split(z, [A_COLS, A_COLS + B_COLS, A_COLS + B_COLS + C_COLS], axis=-1)
        y_a = _gmlp_mixer(z_a, a_ln_g[l], a_ln_b[l], a_ws[l], a_bs[l])
        y_b = _mla_mixer(z_b, positions, b_q_norm[l], b_kv_norm[l], b_w_uq[l], b_w_ukv[l])
        y_c = _dilated_mixer(z_c, positions)
        y_d = _mlstm_mixer(z_d, d_conv_w[l], d_conv_b[l], d_igate_b[l], d_fgate_b[l])
        y = jnp.concatenate([y_a, y_b, y_c, y_d], axis=-1) @ w_out[l]
        x = _layer_norm(DEEPNORM_ALPHA * x + y, ln1_g[l], ln1_b[l])
        h = (jax.nn.silu(x @ w_gate[l]) * (x @ w_up[l])) @ w_down[l]
        x = _layer_norm(DEEPNORM_ALPHA * x + h, ln2_g[l], ln2_b[l])
    return x
```

```python
import math
import os
import numpy as np
from contextlib import ExitStack
import concourse.bass as bass
import concourse.mybir as mybir
from concourse.bass_utils import run_bass_kernel_spmd

F32 = mybir.dt.float32
BF16 = mybir.dt.bfloat16
I32 = mybir.dt.int32
ALU = mybir.AluOpType
AF = mybir.ActivationFunctionType

D = 1024
S = 2048
T = 16
NG = 4
DFF = 2816
ALPHA = 8.0 ** 0.25
A0, B0, C0, D0 = 0, 512, 864, 1632
NCONST = 520
NPP = 28
NPR = 520
DSTOP = int(os.environ.get('DSTOP', '0'))
REORDER = int(os.environ.get('KREORDER', '600'))
SAME_GAP = int(os.environ.get('KSAMEGAP', '2'))


class Sched:
    def __init__(self, nc, es):
        self.nc = nc
        self.es = es
        self.E = {"pe": nc.tensor, "act": nc.scalar, "dve": nc.vector, "pool": nc.gpsimd, "sp": nc.sync}
        self.ops = []
        self.tiles = {}
        self.chan_ops = {}

    def tile(self, name, shape, dt, psum=False):
        ctx = (self.nc.psum_tensor if psum else self.nc.sbuf_tensor)(name, list(shape), dt)
        t = self.es.enter_context(ctx)
        self.tiles[name] = [None, {}]
        return t

    def _names(self, aps):
        r = []
        for a in aps:
            if a is None or isinstance(a, (int, float)):
                continue
            n = a.tensor.name
            if n in self.tiles and n not in r:
                r.append(n)
        return r

    def op(self, eng, fn, reads, writes, dma=None, ndma=1, cost=200.0, tail=0.0):
        rn = self._names(reads)
        wn = self._names(writes)
        deps = set()
        for n in rn:
            st = self.tiles[n]
            if st[0] is not None:
                deps.add(st[0])
        for n in wn:
            st = self.tiles[n]
            if st[0] is not None:
                deps.add(st[0])
            deps.update(st[1].values())
        oid = len(self.ops)
        chan = ("dma:" + dma) if dma else eng
        self.ops.append(dict(eng=eng, fn=fn, deps=deps, chan=chan, ndma=ndma, isdma=dma is not None, cost=cost, tail=tail))
        self.chan_ops.setdefault(chan, []).append(oid)
        for n in rn:
            if n not in wn:
                self.tiles[n][1][chan] = oid
        for n in wn:
            self.tiles[n][0] = oid
            self.tiles[n][1] = {}
        return oid

    def reorder(self, window=600):
        import heapq
        ops = self.ops
        n = len(ops)
        succ = [[] for _ in range(n)]
        indeg = [0] * n
        for i, o in enumerate(ops):
            for d in o["deps"]:
                succ[d].append(i)
            indeg[i] = len(o["deps"])
        fin = [0.0] * n
        ready_t = [0.0] * n
        engs = list(self.E)
        free = {e: 0.0 for e in engs}
        fut = {e: [] for e in engs}
        av = {e: [] for e in engs}
        deferred = []
        done = [False] * n
        base = 0
        order = []

        def admit(i):
            if i < base + window:
                heapq.heappush(fut[ops[i]["eng"]], (ready_t[i], i))
            else:
                heapq.heappush(deferred, i)
        for i in range(n):
            if indeg[i] == 0:
                admit(i)
        nsched = 0
        while nsched < n:
            best = None
            for e in engs:
                f, a = fut[e], av[e]
                while f and f[0][0] <= free[e]:
                    rt, i = heapq.heappop(f)
                    heapq.heappush(a, i)
                if a:
                    cand = (free[e], a[0], e, True)
                elif f:
                    cand = (f[0][0], f[0][1], e, False)
                else:
                    continue
                if best is None or cand[:2] < best[:2]:
                    best = cand
            if best is None:
                i = heapq.heappop(deferred)
                heapq.heappush(fut[ops[i]["eng"]], (ready_t[i], i))
                continue
            st, i, e, from_av = best
            if from_av:
                heapq.heappop(av[e])
            else:
                heapq.heappop(fut[e])
            o = ops[i]
            free[e] = st + o["cost"]
            fin[i] = st + o["cost"] + o["tail"]
            done[i] = True
            order.append(i)
            nsched += 1
            while base < n and done[base]:
                base += 1
            while deferred and deferred[0] < base + window:
                j = heapq.heappop(deferred)
                heapq.heappush(fut[ops[j]["eng"]], (ready_t[j], j))
            for j in succ[i]:
                lat = 60.0 if ops[j]["eng"] == e else 350.0
                if fin[i] + lat > ready_t[j]:
                    ready_t[j] = fin[i] + lat
                indeg[j] -= 1
                if indeg[j] == 0:
                    admit(j)
        remap = {old: new for new, old in enumerate(order)}
        new_ops = []
        for old in order:
            o = ops[old]
            o["deps"] = {remap[d] for d in o["deps"]}
            new_ops.append(o)
        self.ops = new_ops
        self.chan_ops = {}
        for i, o in enumerate(new_ops):
            self.chan_ops.setdefault(o["chan"], []).append(i)
        self.sim_ns = max(fin) if fin else 0.0

    def emit(self):
        if REORDER:
            self.reorder(REORDER)
        ops = self.ops
        pos = {}
        for c, l in self.chan_ops.items():
            for i, o in enumerate(l):
                pos[o] = (c, i)
        seen = {e: {} for e in self.E}
        sig = set()
        for oid, o in enumerate(ops):
            need = {}
            e = o["eng"]
            for d in o["deps"]:
                c, i = pos[d]
                if c == "pe" and e == "pe":
                    continue
                if c == e and e in ("dve", "act") and pos[oid][1] - i - 1 >= SAME_GAP:
                    continue
                if i > seen[e].get(c, -1) and i > need.get(c, -1):
                    need[c] = i
            o["waits"] = need
            for c, i in need.items():
                seen[e][c] = i
                sig.add(self.chan_ops[c][i])
        sems = {}
        for c in self.chan_ops:
            sems[c] = self.es.enter_context(self.nc.semaphore("s_" + c.replace(":", "_")))
        cnt = {c: 0 for c in self.chan_ops}
        sigval = {}
        for oid, o in enumerate(ops):
            c = o["chan"]
            if o["isdma"]:
                cnt[c] += 16 * o["ndma"]
                sigval[oid] = cnt[c]
            elif oid in sig:
                cnt[c] += 1
                sigval[oid] = cnt[c]
        for oid, o in enumerate(ops):
            e = self.E[o["eng"]]
            for c, i in o["waits"].items():
                e.wait_ge(sems[c], sigval[self.chan_ops[c][i]])
            ins = o["fn"](e)
            if o["isdma"]:
                if not isinstance(ins, (list, tuple)):
                    ins = [ins]
                assert len(ins) == o["ndma"]
                for x in ins:
                    x.then_inc(sems[o["chan"]], 16)
            elif oid in sig:
                ins.then_inc(sems[o["chan"]], 1)
        return len(ops)

    @staticmethod
    def _fs(ap):
        n = 1
        for d in list(ap.shape)[1:]:
            n *= int(d)
        return n

    def mm(self, out, lhsT, rhs, start=True, stop=True):
        c = max(64, self._fs(rhs)) / 2.2 * (4.0 if rhs.dtype == F32 else 1.0) + 8.0
        self.op("pe", lambda e: e.matmul(out, lhsT, rhs, start=start, stop=stop), [lhsT, rhs], [out], cost=c, tail=150.0)

    def tr(self, out, in_, ident):
        c = max(64, self._fs(ident)) / 2.2 + 8.0
        self.op("pe", lambda e: e.transpose(out, in_, ident), [in_, ident], [out], cost=c, tail=150.0)

    def act(self, out, in_, func, bias=None, scale=None, eng="act"):
        kw = {}
        if bias is not None:
            kw["bias"] = bias
        if scale is not None:
            kw["scale"] = scale
        self.op(eng, lambda e: e.activation(out, in_, func, **kw), [in_, bias, scale], [out], cost=200.0 + self._fs(out) / 1.2)

    def tt(self, out, in0, in1, op, eng="dve"):
        self.op(eng, lambda e: e.tensor_tensor(out=out, in0=in0, in1=in1, op=op), [in0, in1], [out],
                cost=(70.0 + self._fs(out) / 0.96) if eng == "dve" else (200.0 + self._fs(out) / 0.5))

    def ts(self, out, in0, s1, op0, s2=None, op1=None, eng="dve"):
        if op1 is None:
            self.op(eng, lambda e: e.tensor_scalar(out=out, in0=in0, scalar1=s1, scalar2=None, op0=op0),
                    [in0, s1], [out], cost=70.0 + self._fs(out) / 0.96)
        else:
            self.op(eng, lambda e: e.tensor_scalar(out=out, in0=in0, scalar1=s1, scalar2=s2, op0=op0, op1=op1),
                    [in0, s1, s2], [out], cost=70.0 + self._fs(out) / 0.96)

    def stt(self, out, in0, scalar, in1, op0, op1):
        self.op("dve", lambda e: e.scalar_tensor_tensor(out=out, in0=in0, scalar=scalar, in1=in1, op0=op0, op1=op1),
                [in0, scalar, in1], [out], cost=70.0 + self._fs(out) / 0.96)

    def cp(self, out, in_, eng="dve"):
        if eng == "act":
            self.op("act", lambda e: e.copy(out, in_), [in_], [out], cost=200.0 + self._fs(out) / 1.2)
        else:
            self.op(eng, lambda e: e.tensor_copy(out=out, in_=in_), [in_], [out],
                    cost=(70.0 + self._fs(out) / 0.96) if eng == "dve" else (200.0 + self._fs(out) / 0.6))

    def memset(self, ap, v, eng="dve"):
        self.op(eng, lambda e: e.memset(ap, v), [], [ap], cost=(70.0 + self._fs(ap) / 0.96) if eng == "dve" else (200.0 + self._fs(ap) / 0.6))

    def dma(self, eng, chan, pairs, reads=(), writes=()):
        pairs = list(pairs)
        self.op(eng, lambda e: [e.dma_start(out=o, in_=i) for (o, i) in pairs],
                list(reads) + [i for (_, i) in pairs], list(writes) + [o for (o, _) in pairs],
                dma=chan, ndma=len(pairs), cost=(1200.0 if eng == "pool" else 150.0) * len(pairs),
                tail=2500.0 + sum(self._fs(o) * 128 * 4 for (o, _) in pairs) / 150.0)


class Pool:
    def __init__(self, sch, name, shape, dt, n, psum=False):
        self.t = [sch.tile(f"{name}{i}", shape, dt, psum) for i in range(n)]
        self.i = 0

    def next(self):
        t = self.t[self.i % len(self.t)]
        self.i += 1
        return t


def build(NL=4, NS=4, dbg=None):
    nc = bass.Bass("TRN2", target_bir_lowering=False, dynamic_dma_scratch_size=4096)

    def din(name, shape, dt=F32):
        return nc.dram_tensor(name, list(shape), dt, kind="ExternalInput").ap()

    x_d = din("x", [NS, S, D])
    pos_d = din("pos", [NS, S], I32)
    w_in = din("w_in", [NL, D, 2664])
    a_ws = din("a_ws", [NL, 4, 128, 128])
    w_uq = din("b_w_uq", [NL, 192, 384])
    w_ukv = din("b_w_ukv", [NL, 128, 512])
    w_out = din("w_out", [NL, D, D])
    w_gate = din("w_gate", [NL, D, DFF])
    w_up = din("w_up", [NL, D, DFF])
    w_down = din("w_down", [NL, DFF, D])
    lnp = din("lnp", [NL, 4, D])
    pp_d = din("pp", [NL, 128, NPP])
    pr_d = din("pr", [NL, NPR])
    cst_d = din("cst", [128, NCONST])
    out_d = nc.dram_tensor("out", [NS, S, D], F32, kind="ExternalOutput").ap()
    dbg_d = None
    if dbg:
        dbg_d = nc.dram_tensor("dbg", [128, 8, S], F32, kind="ExternalOutput").ap()

    with ExitStack() as es:
        sc = Sched(nc, es)
        X = [sc.tile(f"X{t}", [128, D], F32) for t in range(T)]
        XTt = [sc.tile(f"XT{g}", [128, 4096], BF16) for g in range(NG)]
        XT = [t[:, :].rearrange("p (k c) -> p k c", k=8) for t in XTt]
        YB = [sc.tile(f"YB{g}", [128, 2, 512], BF16) for g in range(NG)]
        CST = sc.tile("CST", [128, NCONST], F32)
        IDB = sc.tile("IDB", [128, 128], BF16)
        TRIB = sc.tile("TRIB", [128, 128], BF16)
        LOWB = sc.tile("LOWB", [128, 128], BF16)
        MSK2 = sc.tile("MSK2", [128, 2, 128], BF16)
        ONEB = sc.tile("ONEB", [128, 128], BF16)
        IDF = CST[:, 0:128]
        TRIF = CST[:, 128:256]
        ONEF = CST[:, 384:512]
        COSC = sc.tile("COSC", [128, S], BF16)
        SINC = sc.tile("SINC", [128, S], BF16)
        PP = sc.tile("PP", [128, NPP], F32)
        PR = sc.tile("PR", [128, NPR], F32)
        WP = Pool(sc, "WP", [128, 8192], BF16, 2)
        PS = Pool(sc, "PS", [128, 512], F32, 6, psum=True)
        PSL = Pool(sc, "PSL", [128, 512], F32, 2, psum=True)
        SF = Pool(sc, "SF", [128, 512], F32, 4)
        SB = Pool(sc, "SB", [128, 512], BF16, 4)
        SM = Pool(sc, "SM", [128, 64], F32, 8)
        BIG = [sc.tile(f"BIG{i}", [128, 2048], F32) for i in range(3)]
        GB = sc.tile("GB", [128, 1024], F32)
        MID = [sc.tile(f"MID{i}", [128, 2048], BF16) for i in range(5)]
        LNGB = BIG[0][:, :].rearrange("p (a d) -> p a d", a=2)
        WOD = sc.tile("WOD", [128, 2048], BF16)
        WG8 = sc.tile("WG8", [128, 64], BF16)

        sc.dma("sp", "CST", [(CST[:], cst_d[:, :])])
        sc.cp(IDB[:], CST[:, 0:128])
        sc.cp(TRIB[:], CST[:, 128:256])
        sc.cp(LOWB[:], CST[:, 256:384])
        sc.cp(ONEB[:], CST[:, 384:512])
        sc.cp(MSK2[:, 0, :], CST[:, 256:384])
        sc.cp(MSK2[:, 1, :], CST[:, 128:256])

        def wview(wt, n, k=8):
            return wt[:, 0:k * n].rearrange("p (k c) -> p k c", k=k)

        def load_w(wt, src2d, n, k=8, off=0, pairs=None):
            v = wt[:, off:off + k * n].rearrange("p (k c) -> p k c", k=k)
            pr_ = (v, src2d.rearrange("(k p) c -> p k c", p=128))
            if pairs is None:
                sc.dma("pool", wt.name, [pr_])
            else:
                pairs.append(pr_)
            return v

        def make_xt(t):
            g, j = divmod(t, 4)
            for half in range(2):
                ps = PS.next()
                for kk in range(4):
                    k = half * 4 + kk
                    sc.tr(ps[:, kk * 128:(kk + 1) * 128], X[t][:, k * 128:(k + 1) * 128], IDF)
                dst = XT[g][:, half * 4:(half + 1) * 4, j * 128:(j + 1) * 128]
                src = ps[:, :].rearrange("p (k c) -> p k c", k=4)
                sc.cp(dst, src, eng="act")

        def layer_norm(t, gi):
            st = SM.next()
            for h in range(2):
                sc.op("dve", lambda e, h=h, st=st: e.bn_stats(out=st[:, h * 6:(h + 1) * 6], in_=X[t][:, h * 512:(h + 1) * 512]),
                      [X[t][:]], [st[:]])
            sc.op("dve", lambda e, st=st: e.bn_aggr(out=st[:, 16:18], in_=st[:, 0:12]), [st[:]], [st[:]])
            sc.ts(st[:, 18:19], st[:, 17:18], 1e-5, ALU.add)
            sc.act(st[:, 19:20], st[:, 18:19], AF.Sqrt)
            sc.op("dve", lambda e, st=st: e.reciprocal(out=st[:, 20:21], in_=st[:, 19:20]), [st[:]], [st[:]])
            sc.stt(st[:, 21:22], st[:, 16:17], -1.0, st[:, 20:21], ALU.mult, ALU.mult)
            sc.act(X[t][:], X[t][:], AF.Identity, bias=st[:, 21:22], scale=st[:, 20:21])
            sc.tt(X[t][:], X[t][:], LNGB[:, 0, :], ALU.mult)
            sc.tt(X[t][:], X[t][:], LNGB[:, 1, :], ALU.add)

        def load_lngb(l, which):
            sc.dma("sp", "LNGB", [(LNGB[:, 0, :], lnp[l, 2 * which:2 * which + 1, :].broadcast_to([128, D])),
                                  (LNGB[:, 1, :], lnp[l, 2 * which + 1:2 * which + 2, :].broadcast_to([128, D]))])

        first_acc = [True]

        def outproj_partial(l, m, wo):
            for t in range(T):
                g, j = divmod(t, 4)
                for cb in range(2):
                    ps = PS.next()
                    for k in range(2):
                        sc.mm(ps[:, :], YB[g][:, k, j * 128:(j + 1) * 128], wo[:, k, cb * 512:(cb + 1) * 512],
                              start=(k == 0), stop=(k == 1))
                    xs = X[t][:, cb * 512:(cb + 1) * 512]
                    if m == 0:
                        sc.stt(xs, xs, ALPHA, ps[:, :], ALU.mult, ALU.add)
                    else:
                        sc.tt(xs, xs, ps[:, :], ALU.add)

        def dump(m):
            if dbg:
                for g in range(NG):
                    tmp = SF.next()
                    for k in range(2):
                        sc.cp(tmp[:, :], YB[g][:, k, :])
                        sc.dma("sp", tmp.name, [(dbg_d[:, 2 * m + k, g * 512:(g + 1) * 512], tmp[:, :])])

        def mixer_a(l):
            wt = WP.next()
            pairs = []
            wa = load_w(wt, w_in[l, :, A0:A0 + 512], 512, pairs=pairs)
            wo = load_w(wt, w_out[l, 0:256, :], 1024, k=2, off=4096, pairs=pairs)
            sc.dma("pool", wt.name, pairs)
            wsT = MID[0]
            wsr = BIG[1]
            sc.dma("sp", wsr.name, [(wsr[:, 0:512].rearrange("p (h s) -> p h s", h=4),
                                     a_ws[l].rearrange("h t s -> t h s"))])
            ps = PS.next()
            for h in range(4):
                sc.tr(ps[:, h * 128:(h + 1) * 128], wsr[:, h * 128:(h + 1) * 128], IDF)
            sc.tt(wsT[:, 0:512].rearrange("p (h t) -> p h t", h=4), ps[:, :].rearrange("p (h t) -> p h t", h=4),
                  CST[:, 128:256].unsqueeze(1).broadcast_to([128, 4, 128]), ALU.mult)
            for t in range(T):
                g, j = divmod(t, 4)
                ps = PS.next()
                for k in range(8):
                    sc.mm(ps[:, :], XT[g][:, k, j * 128:(j + 1) * 128], wa[:, k, :], start=(k == 0), stop=(k == 7))
                gl = SF.next()
                sc.act(gl[:, :], ps[:, :], AF.Gelu)
                st = SM.next()
                sc.op("dve", lambda e, st=st, gl=gl: e.bn_stats(out=st[:, 0:6], in_=gl[:, 256:512]), [gl[:]], [st[:]])
                sc.op("dve", lambda e, st=st: e.bn_aggr(out=st[:, 16:18], in_=st[:, 0:6]), [st[:]], [st[:]])
                sc.ts(st[:, 18:19], st[:, 17:18], 1e-5, ALU.add)
                sc.act(st[:, 19:20], st[:, 18:19], AF.Sqrt)
                sc.op("dve", lambda e, st=st: e.reciprocal(out=st[:, 20:21], in_=st[:, 19:20]), [st[:]], [st[:]])
                sc.ts(gl[:, 256:512], gl[:, 256:512], st[:, 16:17], ALU.subtract, st[:, 20:21], ALU.mult)
                sc.tt(gl[:, 256:512], gl[:, 256:512], PR[:, 0:256], ALU.mult)
                vn = SB.next()
                sc.tt(vn[:, 0:256], gl[:, 256:512], PR[:, 256:512], ALU.add)
                pm = PS.next()
                for h in range(4):
                    sc.mm(pm[:, h * 64:(h + 1) * 64], wsT[:, h * 128:(h + 1) * 128], vn[:, h * 64:(h + 1) * 64])
                ya = SB.next()
                for h in range(4):
                    sc.stt(ya[:, h * 64:(h + 1) * 64], pm[:, h * 64:(h + 1) * 64], PP[:, 24 + h:25 + h],
                           gl[:, h * 64:(h + 1) * 64], ALU.add, ALU.mult)
                pt = PS.next()
                ptb = pt[:, 0:128].bitcast(BF16)
                for k in range(2):
                    sc.tr(ptb[:, k * 128:(k + 1) * 128], ya[:, k * 128:(k + 1) * 128], IDB[:])
                sc.cp(YB[g][:, :, j * 128:(j + 1) * 128], ptb[:, :].rearrange("p (k c) -> p k c", k=2), eng="act")
            dump(0)
            outproj_partial(l, 0, wo)

        def rope64(dst, src, gs):
            t1 = SF.next(); t2 = SF.next()
            sc.tt(t1[64:128, :], src[64:128, :], COSC[64:128, gs], ALU.mult)
            sc.tt(t2[64:96, :], src[96:128, :], SINC[96:128, gs], ALU.mult)
            sc.tt(t2[96:128, :], src[64:96, :], SINC[64:96, gs], ALU.mult)
            sc.tt(dst, t1[64:128, :], t2[64:128, :], ALU.add)

        def mixer_b(l):
            wt = WP.next()
            pairs = []
            wcq = load_w(wt, w_in[l, :, B0:B0 + 192], 192, pairs=pairs)
            wckv = load_w(wt, w_in[l, :, B0 + 192:B0 + 320], 128, off=1536, pairs=pairs)
            wo = load_w(wt, w_out[l, 256:512, :], 1024, k=2, off=3584, pairs=pairs)
            wkv = wt[:, 6656:6656 + 512]
            pairs.append((wkv, w_ukv[l]))
            krs = load_w(wt, w_in[l, :, B0 + 320:B0 + 352], 32, off=7168, pairs=pairs)
            uqs = wt[:, 7424:7424 + 768].rearrange("p (k c) -> p k c", k=2)
            pairs.append((uqs[:, 0, :], w_uq[l, 0:128, :]))
            pairs.append((uqs[0:64, 1, :], w_uq[l, 128:192, :]))
            sc.dma("pool", wt.name, pairs)
            wkr = wt[:, 2560:2560 + 1024].rearrange("p (k c) -> p k c", k=8)
            sc.memset(wt[:, 2560:3584], 0.0)
            sc.cp(wkr[:, :, 64:96:2], krs[:, :, 0:16])
            sc.cp(wkr[:, :, 96:128:2], krs[:, :, 16:32])
            wq = wt[:, 5632:5632 + 1024].rearrange("p (k h c) -> p k h c", k=2, h=4)
            sc.memset(wt[:, 5632:6656], 0.0)
            uq4 = wt[:, 7424:7424 + 768].rearrange("p (k h c) -> p k h c", k=2, h=4)
            for kc, rows in ((0, 128), (1, 64)):
                sc.cp(wq[0:rows, kc, :, 0:64], uq4[0:rows, kc, :, 0:64])
                sc.cp(wq[0:rows, kc, :, 64:96:2], uq4[0:rows, kc, :, 64:80])
                sc.cp(wq[0:rows, kc, :, 96:128:2], uq4[0:rows, kc, :, 80:96])

            CQ0 = MID[0]; CQ1K = MID[1]; CKVN = MID[2]
            for g in range(NG):
                gs = slice(g * 512, (g + 1) * 512)
                pq0 = PS.next(); pq1 = PS.next(); pkv = PS.next(); pkr = PS.next()
                for k in range(8):
                    sc.mm(pq0[:, :], wcq[:, k, 0:128], XT[g][:, k, :], start=(k == 0), stop=(k == 7))
                for k in range(8):
                    sc.mm(pq1[0:64, :], wcq[:, k, 128:192], XT[g][:, k, :], start=(k == 0), stop=(k == 7))
                for k in range(8):
                    sc.mm(pkv[:, :], wckv[:, k, :], XT[g][:, k, :], start=(k == 0), stop=(k == 7))
                for k in range(8):
                    sc.mm(pkr[:, :], wkr[:, k, :], XT[g][:, k, :], start=(k == 0), stop=(k == 7))
                s0 = SF.next(); s1 = SF.next(); s2 = SF.next()
                sc.act(s0[:, :], pq0[:, :], AF.Square)
                sc.act(s1[0:64, :], pq1[0:64, :], AF.Square)
                sc.act(s2[:, :], pkv[:, :], AF.Square)
                pss = PS.next()
                sc.mm(pss[:, :], ONEF, s0[:, :], start=True, stop=False)
                sc.mm(pss[:, :], CST[0:64, 384:512], s1[0:64, :], start=False, stop=True)
                rq = SF.next()
                sc.ts(rq[:, :], pss[:, :], 1.0 / 192, ALU.mult, 1e-6, ALU.add)
                sc.act(rq[:, :], rq[:, :], AF.Sqrt)
                sc.op("dve", lambda e, rq=rq: e.reciprocal(out=rq[:, :], in_=rq[:, :]), [rq[:]], [rq[:]])
                sc.stt(CQ0[:, gs], pq0[:, :], PP[:, 0:1], rq[:, :], ALU.mult, ALU.mult)
                sc.stt(CQ1K[0:64, gs], pq1[0:64, :], PP[0:64, 1:2], rq[0:64, :], ALU.mult, ALU.mult)
                pss2 = PS.next()
                sc.mm(pss2[:, :], ONEF, s2[:, :])
                rk = SF.next()
                sc.ts(rk[:, :], pss2[:, :], 1.0 / 128, ALU.mult, 1e-6, ALU.add)
                sc.act(rk[:, :], rk[:, :], AF.Sqrt)
                sc.op("dve", lambda e, rk=rk: e.reciprocal(out=rk[:, :], in_=rk[:, :]), [rk[:]], [rk[:]])
                sc.stt(CKVN[:, gs], pkv[:, :], PP[:, 2:3], rk[:, :], ALU.mult, ALU.mult)
                rope64(CQ1K[64:128, gs], pkr, gs)

            scale = 96.0 ** -0.5
            for h in range(4):
                QT = MID[3]; KT = MID[4]
                va = BIG[h % 2][:, :].bitcast(BF16)[:, 0:2048].rearrange("p (t c) -> p t c", t=T)
                for g in range(NG):
                    gs = slice(g * 512, (g + 1) * 512)
                    pq = PS.next()
                    sc.mm(pq[:, :], wq[:, 0, h, :], CQ0[:, gs], start=True, stop=False)
                    sc.mm(pq[:, :], wq[0:64, 1, h, :], CQ1K[0:64, gs], start=False, stop=True)
                    sc.cp(QT[0:64, gs], pq[0:64, :], eng="act")
                    rope64(QT[64:128, gs], pq, gs)
                    pk = PS.next()
                    sc.mm(pk[0:64, :], wkv[:, h * 128:h * 128 + 64], CKVN[:, gs])
                    sc.cp(KT[0:64, gs], pk[0:64, :], eng="act")
                    sc.cp(KT[64:128, gs], CQ1K[64:128, gs], eng="pool")
                    pv = PS.next()
                    for j in range(4):
                        t = g * 4 + j
                        sc.mm(pv[:, j * 64:(j + 1) * 64], CKVN[:, t * 128:(t + 1) * 128], wkv[:, h * 128 + 64:h * 128 + 128])
                    sc.cp(va[:, g * 4:(g + 1) * 4, 0:64], pv[:, 0:256].rearrange("p (j c) -> p j c", j=4), eng="act")
                    sc.memset(va[:, g * 4:(g + 1) * 4, 64:128], 1.0, eng="pool")
                for Qb in range(4):
                    po = PSL.next()
                    nkb = 4 * Qb + 4
                    for kb in range(nkb):
                        qs = max(Qb * 512, kb * 128)
                        qe = (Qb + 1) * 512
                        n = qe - qs
                        ps = PS.next()
                        sc.mm(ps[:, 0:n], KT[:, kb * 128:(kb + 1) * 128], QT[:, qs:qe])
                        pt = SB.next()
                        sc.act(pt[:, 0:n], ps[:, 0:n], AF.Exp, scale=scale)
                        if kb * 128 >= Qb * 512:
                            sc.tt(pt[:, 0:128], pt[:, 0:128], TRIB[:], ALU.mult, eng="pool")
                        sc.mm(po[:, qs - Qb * 512:512], va[:, kb, :], pt[:, 0:n], start=(kb == 0), stop=(kb == nkb - 1))
                    rd = SF.next()
                    sc.cp(rd[0:64, :], po[64:128, :], eng="act")
                    sc.op("dve", lambda e, rd=rd: e.reciprocal(out=rd[0:64, :], in_=rd[0:64, :]), [rd[:]], [rd[:]])
                    hp = (h % 2) * 64
                    sc.tt(YB[Qb][hp:hp + 64, h // 2, :], po[0:64, :], rd[0:64, :], ALU.mult)
            dump(1)
            outproj_partial(l, 1, wo)

        def mixer_c(l):
            wt = WP.next()
            pairs = []
            wc = load_w(wt, w_in[l, :, C0:C0 + 768], 768, pairs=pairs)
            wo = load_w(wt, w_out[l, 512:768, :], 1024, k=2, off=6144, pairs=pairs)
            sc.dma("pool", wt.name, pairs)
            QC = [MID[0], MID[1]]; KC = [MID[2], MID[3]]; VCt = MID[4]
            for g in range(NG):
                gs = slice(g * 512, (g + 1) * 512)
                for ci in range(4):
                    ps = PS.next()
                    for k in range(8):
                        sc.mm(ps[:, :], wc[:, k, ci * 128:(ci + 1) * 128], XT[g][:, k, :], start=(k == 0), stop=(k == 7))
                    dst = (QC if ci < 2 else KC)[ci % 2]
                    t1 = SF.next(); t2 = SF.next()
                    sc.tt(t1[:, :], ps[:, :], COSC[:, gs], ALU.mult)
                    for q in range(4):
                        src = (q ^ 1) * 32
                        sc.tt(t2[q * 32:(q + 1) * 32, :], ps[src:src + 32, :], SINC[src:src + 32, gs], ALU.mult)
                    sc.tt(dst[:, gs], t1[:, :], t2[:, :], ALU.add)
            vcnt = 0
            for h in range(4):
                c, hp = h // 2, (h % 2) * 64
                if h % 2 == 0:
                    for g in range(NG):
                        ps = PS.next()
                        for k in range(8):
                            sc.mm(ps[:, :], wc[:, k, (4 + c) * 128:(5 + c) * 128], XT[g][:, k, :], start=(k == 0), stop=(k == 7))
                        sc.cp(VCt[:, g * 512:(g + 1) * 512], ps[:, :], eng="act")
                ACC = BIG[1]
                first = True
                for d in (1, 4, 16):
                    nb = 16 // d
                    va = BIG[0][:, :].bitcast(BF16)[:, (vcnt % 2) * 2048:(vcnt % 2 + 1) * 2048].rearrange("p (b c) -> p b c", b=16)
                    vcnt += 1

                    def sel(r, n, d=d):
                        st = n * 128 * d + r
                        return slice(st, st + 127 * d + 1, d)
                    blocks = [(r, n) for r in range(d) for n in range(nb)]
                    for bi in range(0, 16, 4):
                        pt = PS.next()
                        ptb = pt[:, 0:128].bitcast(BF16)
                        for q in range(4):
                            r, n = blocks[bi + q]
                            sc.tr(ptb[:, q * 64:(q + 1) * 64], VCt[hp:hp + 64, sel(r, n)], IDB[hp:hp + 64, hp:hp + 64])
                        sc.cp(va[:, bi:bi + 4, 0:64], ptb[:, :].rearrange("p (q c) -> p q c", q=4), eng="act")
                    sc.memset(va[:, :, 64:128], 1.0, eng="pool")
                    for bi, (r, n) in enumerate(blocks):
                        ps = PS.next()
                        two = n > 0
                        if two:
                            sc.mm(ps[:, 0:128], KC[c][hp:hp + 64, sel(r, n - 1)], QC[c][hp:hp + 64, sel(r, n)])
                        sc.mm(ps[:, 128:256], KC[c][hp:hp + 64, sel(r, n)], QC[c][hp:hp + 64, sel(r, n)])
                        pt = SB.next()
                        lo = 0 if two else 128
                        sc.act(pt[:, lo:256], ps[:, lo:256], AF.Exp, scale=0.125)
                        sc.tt(pt[:, lo:256], pt[:, lo:256], MSK2[:, :, :].rearrange("p a b -> p (a b)")[:, lo:256], ALU.mult, eng="pool")
                        po = PS.next()
                        if two:
                            sc.mm(po[:, 0:128], va[:, bi - 1, :], pt[:, 0:128], start=True, stop=False)
                        sc.mm(po[:, 0:128], va[:, bi, :], pt[:, 128:256], start=(not two), stop=True)
                        if first:
                            sc.cp(ACC[:, sel(r, n)], po[:, 0:128], eng="act")
                        else:
                            sc.tt(ACC[:, sel(r, n)], ACC[:, sel(r, n)], po[:, 0:128], ALU.add)
                    first = False
                for g in range(NG):
                    gs = slice(g * 512, (g + 1) * 512)
                    rd = SF.next()
                    sc.cp(rd[0:64, :], ACC[64:128, gs], eng="act")
                    sc.op("dve", lambda e, rd=rd: e.reciprocal(out=rd[0:64, :], in_=rd[0:64, :]), [rd[:]], [rd[:]])
                    sc.tt(YB[g][hp:hp + 64, c, :], ACC[0:64, gs], rd[0:64, :], ALU.mult)
            dump(2)
            outproj_partial(l, 2, wo)

        def mixer_d(l):
            wt = WP.next()
            pairs = []
            wqk = load_w(wt, w_in[l, :, D0:D0 + 512], 512, pairs=pairs)
            wvo = load_w(wt, w_in[l, :, D0 + 512:D0 + 1024], 512, off=4096, pairs=pairs)
            sc.dma("pool", wt.name, pairs)
            wg8 = load_w(WG8, w_in[l, :, D0 + 1024:D0 + 1032], 8)
            wo = load_w(WOD, w_out[l, 768:1024, :], 1024, k=2)
            pg = PSL.next()
            for t in range(T):
                g, j = divmod(t, 4)
                for k in range(8):
                    sc.mm(pg[:, t * 8:(t + 1) * 8], XT[g][:, k, j * 128:(j + 1) * 128], wg8[:, k, :], start=(k == 0), stop=(k == 7))

            def tb(i):
                return GB[:, i * 64:(i + 1) * 64]

            def tb3(i):
                return GB[:, i * 64:(i + 1) * 64].rearrange("p (c h) -> p c h", h=4)
            pg3 = pg[:, 0:128].rearrange("p (c e) -> p c e", e=8)
            GI, GF, AB, EX, LN_, FC, BI_, BB, AA, WK, THR, DEC, TOT = range(13)
            sc.tt(tb3(GI), pg3[:, :, 0:4], PR[:, 512:516].unsqueeze(1).broadcast_to([128, 16, 4]), ALU.add)
            sc.tt(tb3(GF), pg3[:, :, 4:8], PR[:, 516:520].unsqueeze(1).broadcast_to([128, 16, 4]), ALU.add)
            sc.act(tb(AB), tb(GF), AF.Abs)
            sc.act(tb(EX), tb(AB), AF.Exp, scale=-1.0)
            sc.act(tb(LN_), tb(EX), AF.Ln, bias=1.0)
            sc.stt(tb(FC), tb(GF), 0.0, tb(LN_), ALU.min, ALU.subtract)
            if DSTOP == 1:
                return
            pc = PS.next()
            sc.mm(pc[:, 0:64], TRIF, tb(FC))
            sc.mm(pc[:, 64:128], ONEF, tb(FC))
            sc.cp(tb(TOT), pc[:, 64:128])
            for h in range(4):
                v_in = GB[:, TOT * 64 + h:TOT * 64 + 64:4]
                v_out = GB[:, BI_ * 64 + h:BI_ * 64 + 64:4]
                sc.op("dve", lambda e, v_in=v_in, v_out=v_out: e.tensor_tensor_scan(
                    out=v_out, data0=CST[:, 384:400], data1=v_in, initial=0.0, op0=ALU.mult, op1=ALU.add), [GB[:], CST[:]], [GB[:]])
            sc.tt(tb(BI_), tb(BI_), tb(TOT), ALU.subtract)
            sc.tt(tb(BB), pc[:, 0:64], tb(BI_), ALU.add)
            sc.tt(tb(AA), tb(GI), tb(BB), ALU.subtract)
            if DSTOP == 2:
                return
            pa = PS.next()
            sc.tr(pa[0:64, 0:128], tb(AA), IDF)
            cm = SM.next()
            sc.op("dve", lambda e: e.tensor_reduce(out=cm[0:64, 0:1], in_=pa[0:64, 0:128], axis=mybir.AxisListType.X, op=ALU.max),
                  [pa[:]], [cm[:]])
            pb = PS.next()
            sc.tr(pb[0:1, 0:64], cm[0:64, 0:1], CST[0:64, 0:64])
            R = SF.next()
            sc.cp(R[0:1, 128:192], pb[0:1, 0:64])
            for h in range(4):
                sc.op("dve", lambda e, h=h: e.tensor_tensor_scan(
                    out=R[0:1, h:64:4], data0=R[0:1, 128 + h:192:4], data1=CST[0:1, 260:276], initial=0.0,
                    op0=ALU.max, op1=ALU.max), [R[:], CST[:]], [R[:]])
            sc.memset(R[0:1, 64:68], 0.0)
            sc.cp(R[0:1, 68:128], R[0:1, 0:60])
            pm = PS.next()
            sc.mm(pm[:, 0:128], CST[0:1, 384:512], R[0:1, 0:128])
            MR = SF.next()
            sc.cp(MR[:, 0:128], pm[:, 0:128])
            sc.tt(tb(WK), tb(AA), MR[:, 0:64], ALU.subtract)
            sc.act(tb(WK), tb(WK), AF.Exp)
            sc.ts(tb(WK), tb(WK), 0.125, ALU.mult)
            sc.tt(tb(THR), tb(BB), MR[:, 0:64], ALU.add)
            sc.act(tb(THR), tb(THR), AF.Exp, scale=-1.0)
            sc.tt(tb(DEC), MR[:, 64:128], MR[:, 0:64], ALU.subtract)
            sc.act(tb(DEC), tb(DEC), AF.Exp)
            if DSTOP == 3:
                return
            QK = [MID[0], MID[1], MID[2], MID[3]]
            for ci in range(4):
                Z = BIG[0]
                for g in range(NG):
                    ps = PS.next()
                    for k in range(8):
                        sc.mm(ps[:, :], wqk[:, k, ci * 128:(ci + 1) * 128], XT[g][:, k, :], start=(k == 0), stop=(k == 7))
                    sc.cp(Z[:, g * 512:(g + 1) * 512], ps[:, :], eng="act")
                A = BIG[1]
                sc.ts(A[:, :], Z[:, :], PP[:, 3 + ci * 4 + 3:4 + ci * 4 + 3], ALU.mult, PP[:, 19 + ci:20 + ci], ALU.add)
                for j in range(3):
                    sh = 3 - j
                    sc.stt(A[:, sh:S], Z[:, 0:S - sh], PP[:, 3 + ci * 4 + j:4 + ci * 4 + j], A[:, sh:S], ALU.mult, ALU.add)
                sc.act(QK[ci][:, :], A[:, :], AF.Silu)
            if DSTOP == 4:
                return
            CS = SF.next()
            cs3 = CS[:, 0:256].rearrange("p (a c) -> p a c", a=2)
            sc.memset(CS[:, 0:256], 0.0)
            SFd = [BIG[2][:, i * 512:(i + 1) * 512] for i in range(4)]
            for c in range(T):
                g, j = divmod(c, 4)
                cs = slice(c * 128, (c + 1) * 128)
                pvo = PS.next()
                for k in range(8):
                    sc.mm(pvo[:, :], XT[g][:, k, j * 128:(j + 1) * 128], wvo[:, k, :], start=(k == 0), stop=(k == 7))
                vaug = SB.next()
                va3 = vaug[:, 0:512].rearrange("p (h c) -> p h c", h=4)
                sc.cp(va3[:, :, 0:64], pvo[:, 0:256].rearrange("p (h c) -> p h c", h=4), eng="act")
                sc.memset(va3[:, :, 64:128], 1.0, eng="pool")
                so = MID[4][:, (c % 2) * 256:(c % 2) * 256 + 256]
                sc.act(so, pvo[:, 256:512], AF.Sigmoid)
                pk = PS.next()
                pkb = pk[:, 0:128].bitcast(BF16)
                for i in range(2):
                    sc.tr(pkb[:, i * 128:(i + 1) * 128], QK[2 + i][:, cs], IDB[:])
                kw = SB.next()
                sc.tt(kw[:, 0:256].rearrange("p (h c) -> p h c", h=4), pkb[:, :].rearrange("p (h c) -> p h c", h=4),
                      GB[:, WK * 64 + c * 4:WK * 64 + c * 4 + 4].unsqueeze(2).broadcast_to([128, 4, 64]), ALU.mult)
                pS2 = [PS.next(), PS.next()]
                for h in range(4):
                    hp = (h % 2) * 64
                    sc.mm(pS2[h % 2][:, (h // 2) * 128:(h // 2 + 1) * 128], QK[2 + h // 2][hp:hp + 64, cs], QK[h // 2][hp:hp + 64, cs])
                sq = SB.next()
                for h in range(4):
                    sc.stt(sq[:, h * 128:(h + 1) * 128], pS2[h % 2][:, (h // 2) * 128:(h // 2 + 1) * 128],
                           GB[:, WK * 64 + c * 4 + h:WK * 64 + c * 4 + h + 1], TRIB[:], ALU.mult, ALU.mult)
                for hf in range(2):
                    rows = slice(hf * 64, hf * 64 + 64)
                    dec = GB[rows, DEC * 64 + c * 4 + hf:DEC * 64 + c * 4 + 4:2]
                    sc.tt(cs3[rows, :, :], cs3[rows, :, :], dec.unsqueeze(2).broadcast_to([64, 2, 128]), ALU.mult)
                cb = SB.next()
                cb3 = cb[:, 0:256].rearrange("p (a c) -> p a c", a=2)
                sc.cp(cb[:, 0:256], CS[:, 0:256], eng="act")
                ph2 = [PS.next(), PS.next()]
                for h in range(4):
                    hp = (h % 2) * 64
                    po_ = ph2[h % 2][:, (h // 2) * 128:(h // 2 + 1) * 128]
                    sc.mm(po_, sq[:, h * 128:(h + 1) * 128], va3[:, h, :], start=True, stop=False)
                    sc.mm(po_, QK[h // 2][hp:hp + 64, cs], cb3[hp:hp + 64, h // 2, :], start=False, stop=True)
                pu = PS.next()
                for a in range(2):
                    sc.mm(pu[:, a * 256:(a + 1) * 256], kw[:, a * 128:(a + 1) * 128], vaug[:, a * 256:(a + 1) * 256])
                pu3 = pu[:, 0:512].rearrange("p (a c) -> p a c", a=2)
                sc.tt(cs3[0:64, :, :], cs3[0:64, :, :], pu3[0:64, :, 0:128], ALU.add)
                sc.tt(cs3[64:128, :, :], cs3[64:128, :, :], pu3[64:128, :, 128:256], ALU.add)
                dn = SM.next()
                for par in range(2):
                    p3 = ph2[par][:, 0:256].rearrange("p (a c) -> p a c", a=2)
                    sc.act(dn[:, par:4:2], p3[:, :, 64], AF.Abs)
                sc.tt(dn[:, 0:4], dn[:, 0:4], GB[:, THR * 64 + c * 4:THR * 64 + c * 4 + 4], ALU.max)
                sc.op("dve", lambda e, dn=dn: e.reciprocal(out=dn[:, 4:8], in_=dn[:, 0:4]), [dn[:]], [dn[:]])
                hy = SFd[c % 4]
                hy3 = hy[:, 0:256].rearrange("p (h c) -> p h c", h=4)
                for par in range(2):
                    p3 = ph2[par][:, 0:256].rearrange("p (a c) -> p a c", a=2)
                    sc.tt(hy3[:, par:4:2, :], p3[:, :, 0:64],
                          dn[:, 4 + par:8:2].unsqueeze(2).broadcast_to([128, 2, 64]), ALU.mult)
                yd = SB.next()
                sc.tt(yd[:, 0:256], hy[:, 0:256], so, ALU.mult, eng="pool")
                pt = PS.next()
                ptb = pt[:, 0:128].bitcast(BF16)
                for k in range(2):
                    sc.tr(ptb[:, k * 128:(k + 1) * 128], yd[:, k * 128:(k + 1) * 128], IDB[:])
                sc.cp(YB[g][:, :, j * 128:(j + 1) * 128], ptb[:, :].rearrange("p (k c) -> p k c", k=2), eng="act")
            dump(3)
            outproj_partial(l, 3, wo)

        def ffn(l):
            chunks = [(i * 512, 512) for i in range(5)] + [(2560, 256)]
            for ci, (c0, cw) in enumerate(chunks):
                nhb = cw // 128
                wt = WP.next()
                pairs = []
                wg = load_w(wt, w_gate[l, :, c0:c0 + cw], cw, pairs=pairs)
                wu = load_w(wt, w_up[l, :, c0:c0 + cw], cw, off=4096, pairs=pairs)
                sc.dma("pool", wt.name, pairs)
                wt2 = BIG[1 + ci % 2][:, :].bitcast(BF16)
                vd = wt2[:, 0:nhb * 1024].rearrange("p (k c) -> p k c", k=nhb)
                sc.dma("pool", f"BIG{1 + ci % 2}", [(vd, w_down[l, c0:c0 + cw, :].rearrange("(k p) c -> p k c", p=128))])
                wd = vd
                for g in range(NG):
                    hT = MID[(ci * NG + g) % 2]
                    for hb in range(nhb):
                        pg_ = PS.next(); pu_ = PS.next()
                        for k in range(8):
                            sc.mm(pg_[:, :], wg[:, k, hb * 128:(hb + 1) * 128], XT[g][:, k, :], start=(k == 0), stop=(k == 7))
                        for k in range(8):
                            sc.mm(pu_[:, :], wu[:, k, hb * 128:(hb + 1) * 128], XT[g][:, k, :], start=(k == 0), stop=(k == 7))
                        sl = SF.next()
                        sc.act(sl[:, :], pg_[:, :], AF.Silu)
                        sc.tt(hT[:, hb * 512:(hb + 1) * 512], sl[:, :], pu_[:, :], ALU.mult)
                    for j in range(4):
                        t = g * 4 + j
                        for cb in range(2):
                            ps = PS.next()
                            for hb in range(nhb):
                                sc.mm(ps[:, :], hT[:, hb * 512 + j * 128:hb * 512 + (j + 1) * 128], wd[:, hb, cb * 512:(cb + 1) * 512],
                                      start=(hb == 0), stop=(hb == nhb - 1))
                            xs = X[t][:, cb * 512:(cb + 1) * 512]
                            if ci == 0:
                                sc.stt(xs, xs, ALPHA, ps[:, :], ALU.mult, ALU.add)
                            else:
                                sc.tt(xs, xs, ps[:, :], ALU.add)

        def rope_tables(s):
            PI = XTt[0][:, :].bitcast(F32)
            PIi = XTt[0][:, :].bitcast(I32)
            PF = XTt[1][:, :].bitcast(F32)
            RR = XTt[2][:, :].bitcast(F32)
            KK = XTt[3][:, :].bitcast(F32)
            KKi = XTt[3][:, :].bitcast(I32)
            sc.dma("sp", "XT0", [(PIi, pos_d[s:s + 1, :].broadcast_to([128, S]))])
            sc.cp(PF, PIi)
            two_pi = 2.0 * math.pi
            c1 = 6.28125
            c2 = float(np.float32(two_pi - c1))
            c3 = float(two_pi - c1 - c2)
            sc.ts(RR, PF, CST[:, 513:514], ALU.mult)
            for shift, dst, sgn in ((0.0, SINC, True), (math.pi / 2, COSC, False)):
                sc.ts(KKi, RR, shift, ALU.add, 1.0 / two_pi, ALU.mult)
                sc.cp(PI, KKi)
                sc.stt(KK, PI, -c1, RR, ALU.mult, ALU.add)
                sc.stt(KK, PI, -c2, KK, ALU.mult, ALU.add)
                sc.stt(KK, PI, -c3, KK, ALU.mult, ALU.add)
                sc.ts(KK, KK, shift, ALU.add)
                sc.ts(KK, KK, math.pi, ALU.min, -math.pi, ALU.max)
                if sgn:
                    sc.act(KK, KK, AF.Sin)
                    sc.ts(dst[:, :], KK, CST[:, 514:515], ALU.mult)
                else:
                    sc.act(dst[:, :], KK, AF.Sin)

        for s in range(NS):
            rope_tables(s)
            for q in range(4):
                sc.dma("sp", f"Xld{q}", [(X[q * 4 + i][:, :], x_d[s, (q * 4 + i) * 128:(q * 4 + i + 1) * 128, :]) for i in range(4)])
            for t in range(T):
                make_xt(t)
            for l in range(NL):
                sc.dma("sp", "PP", [(PP[:, :], pp_d[l])])
                sc.dma("sp", "PR", [(PR[:, :], pr_d[l:l + 1, :].broadcast_to([128, NPR]))])
                acc_m = [0]
                for nm, fn in (("a", mixer_a), ("b", mixer_b), ("c", mixer_c), ("d", mixer_d)):
                    if dbg is None or nm in dbg:
                        fn(l)
                load_lngb(l, 0)
                for t in range(T):
                    layer_norm(t, 0)
                    make_xt(t)
                load_lngb(l, 1)
                if dbg is None or "f" in dbg:
                    ffn(l)
                for t in range(T):
                    layer_norm(t, 1)
                    if l < NL - 1:
                        make_xt(t)
            for q in range(4):
                sc.dma("sp", f"Xld{q}", [(out_d[s, (q * 4 + i) * 128:(q * 4 + i + 1) * 128, :], X[q * 4 + i][:, :]) for i in range(4)])
        sc.op("sp", lambda e: e.nop(), [X[t][:] for t in range(T)], [X[t][:] for t in range(T)])
        nops = sc.emit()
    return nc, nops


def make_consts():
    c = np.zeros((128, NCONST), np.float32)
    p = np.arange(128)
    c[:, 0:128] = np.eye(128, dtype=np.float32)
    c[:, 128:256] = (p[:, None] <= p[None, :]).astype(np.float32)
    c[:, 256:384] = (p[:, None] >= p[None, :]).astype(np.float32)
    c[:, 384:512] = 1.0
    th = np.float32(10000.0)
    c[:, 512] = np.power(th, -(np.arange(16, dtype=np.float32) / np.float32(16)))[p % 16]
    c[:, 513] = np.power(th, -(np.arange(32, dtype=np.float32) / np.float32(32)))[p % 32]
    c[:, 514] = np.where((p % 64) < 32, 1.0, -1.0)
    return c


def prep_inputs(inp, NL=4):
    f = lambda a: np.ascontiguousarray(np.asarray(a, dtype=np.float32))
    lnp = np.stack([f(inp["ln1_g"]), f(inp["ln1_b"]), f(inp["ln2_g"]), f(inp["ln2_b"])], axis=1)[:NL]
    pp = np.zeros((NL, 128, NPP), np.float32)
    pr = np.zeros((NL, NPR), np.float32)
    for l in range(NL):
        qn = f(inp["b_q_norm"])[l]
        pp[l, :, 0] = qn[0:128]
        pp[l, 0:64, 1] = qn[128:192]
        pp[l, :, 2] = f(inp["b_kv_norm"])[l]
        cw = f(inp["d_conv_w"])[l]
        for ci in range(4):
            for j in range(4):
                pp[l, :, 3 + ci * 4 + j] = cw[j, ci * 128:(ci + 1) * 128]
            pp[l, :, 19 + ci] = f(inp["d_conv_b"])[l, ci * 128:(ci + 1) * 128]
        pp[l, :, 24:28] = f(inp["a_bs"])[l].T
        pr[l, 0:256] = f(inp["a_ln_g"])[l]
        pr[l, 256:512] = f(inp["a_ln_b"])[l]
        pr[l, 512:516] = f(inp["d_igate_b"])[l]
        pr[l, 516:520] = f(inp["d_fgate_b"])[l]
    shared = dict(w_in=f(inp["w_in"])[:NL], a_ws=f(inp["a_ws"])[:NL], b_w_uq=f(inp["b_w_uq"])[:NL],
                  b_w_ukv=f(inp["b_w_ukv"])[:NL], w_out=f(inp["w_out"])[:NL], w_gate=f(inp["w_gate"])[:NL],
                  w_up=f(inp["w_up"])[:NL], w_down=f(inp["w_down"])[:NL], lnp=np.ascontiguousarray(lnp),
                  pp=pp, pr=pr, cst=make_consts())
    return shared


_CACHE = {}


def kernel(**inputs):
    n = 8
    x = np.asarray(inputs["x"], dtype=np.float32)
    pos = np.asarray(inputs["positions"], dtype=np.int32)
    shared = prep_inputs(inputs)
    if "nc" not in _CACHE:
        _CACHE["nc"] = build(4, 4)[0]
    nc = _CACHE["nc"]
    in_maps = []
    for c in range(n):
        m = dict(shared)
        m["x"] = np.ascontiguousarray(x[c * 4:(c + 1) * 4])
        m["pos"] = np.ascontiguousarray(pos[c * 4:(c + 1) * 4])
        in_maps.append(m)
    res = run_bass_kernel_spmd(nc, in_maps, core_ids=list(range(n)))
    return np.concatenate([r["out"] for r in res.results], axis=0)
```

```python
import math
import os
import numpy as np
from contextlib import ExitStack
import concourse.bass as bass
import concourse.mybir as mybir
from concourse.bass_utils import run_bass_kernel_spmd

F32 = mybir.dt.float32
BF16 = mybir.dt.bfloat16
I32 = mybir.dt.int32
ALU = mybir.AluOpType
AF = mybir.ActivationFunctionType

D = 1024
S = 2048
T = 16
NG = 4
DFF = 2816
ALPHA = 8.0 ** 0.25
A0, B0, C0, D0 = 0, 512, 864, 1632
NCONST = 520
NPP = 28
NPR = 520
DSTOP = int(os.environ.get('DSTOP', '0'))
REORDER = int(os.environ.get('KREORDER', '600'))
SAME_GAP = int(os.environ.get('KSAMEGAP', '2'))


class Sched:
    def __init__(self, nc, es):
        self.nc = nc
        self.es = es
        self.E = {"pe": nc.tensor, "act": nc.scalar, "dve": nc.vector, "pool": nc.gpsimd, "sp": nc.sync}
        self.ops = []
        self.tiles = {}
        self.chan_ops = {}

    def tile(self, name, shape, dt, psum=False):
        ctx = (self.nc.psum_tensor if psum else self.nc.sbuf_tensor)(name, list(shape), dt)
        t = self.es.enter_context(ctx)
        self.tiles[name] = [None, {}]
        return t

    def _names(self, aps):
        r = []
        for a in aps:
            if a is None or isinstance(a, (int, float)):
                continue
            n = a.tensor.name
            if n in self.tiles and n not in r:
                r.append(n)
        return r

    def op(self, eng, fn, reads, writes, dma=None, ndma=1, cost=200.0, tail=0.0):
        rn = self._names(reads)
        wn = self._names(writes)
        deps = set()
        for n in rn:
            st = self.tiles[n]
            if st[0] is not None:
                deps.add(st[0])
        for n in wn:
            st = self.tiles[n]
            if st[0] is not None:
                deps.add(st[0])
            deps.update(st[1].values())
        oid = len(self.ops)
        chan = ("dma:" + dma) if dma else eng
        self.ops.append(dict(eng=eng, fn=fn, deps=deps, chan=chan, ndma=ndma, isdma=dma is not None, cost=cost, tail=tail))
        self.chan_ops.setdefault(chan, []).append(oid)
        for n in rn:
            if n not in wn:
                self.tiles[n][1][chan] = oid
        for n in wn:
            self.tiles[n][0] = oid
            self.tiles[n][1] = {}
        return oid

    def reorder(self, window=600):
        import heapq
        ops = self.ops
        n = len(ops)
        succ = [[] for _ in range(n)]
        indeg = [0] * n
        for i, o in enumerate(ops):
            for d in o["deps"]:
                succ[d].append(i)
            indeg[i] = len(o["deps"])
        blevel = [0.0] * n
        for i in range(n - 1, -1, -1):
            o = ops[i]
            b = 0.0
            for j in succ[i]:
                v = blevel[j] + (60.0 if ops[j]["eng"] == o["eng"] else 350.0)
                if v > b:
                    b = v
            blevel[i] = b + o["cost"] + o["tail"]
        fin = [0.0] * n
        ready_t = [0.0] * n
        engs = list(self.E)
        free = {e: 0.0 for e in engs}
        fut = {e: [] for e in engs}
        av = {e: [] for e in engs}
        deferred = []
        done = [False] * n
        base = 0
        order = []

        def admit(i):
            if i < base + window:
                heapq.heappush(fut[ops[i]["eng"]], (ready_t[i], i))
            else:
                heapq.heappush(deferred, i)
        for i in range(n):
            if indeg[i] == 0:
                admit(i)
        nsched = 0
        while nsched < n:
            best = None
            for e in engs:
                f, a = fut[e], av[e]
                while f and f[0][0] <= free[e]:
                    rt, i = heapq.heappop(f)
                    heapq.heappush(a, i)
                if a:
                    cand = (free[e], a[0], e, True)
                elif f:
                    cand = (f[0][0], f[0][1], e, False)
                else:
                    continue
                if best is None or cand[:2] < best[:2]:
                    best = cand
            if best is None:
                i = heapq.heappop(deferred)
                heapq.heappush(fut[ops[i]["eng"]], (ready_t[i], i))
                continue
            st, i, e, from_av = best
            if from_av:
                heapq.heappop(av[e])
            else:
                heapq.heappop(fut[e])
            o = ops[i]
            free[e] = st + o["cost"]
            fin[i] = st + o["cost"] + o["tail"]
            done[i] = True
            order.append(i)
            nsched += 1
            while base < n and done[base]:
                base += 1
            while deferred and deferred[0] < base + window:
                j = heapq.heappop(deferred)
                heapq.heappush(fut[ops[j]["eng"]], (ready_t[j], j))
            for j in succ[i]:
                lat = 60.0 if ops[j]["eng"] == e else 350.0
                if fin[i] + lat > ready_t[j]:
                    ready_t[j] = fin[i] + lat
                indeg[j] -= 1
                if indeg[j] == 0:
                    admit(j)
        remap = {old: new for new, old in enumerate(order)}
        new_ops = []
        for old in order:
            o = ops[old]
            o["deps"] = {remap[d] for d in o["deps"]}
            new_ops.append(o)
        self.ops = new_ops
        self.chan_ops = {}
        for i, o in enumerate(new_ops):
            self.chan_ops.setdefault(o["chan"], []).append(i)
        self.sim_ns = max(fin) if fin else 0.0

    def emit(self):
        if REORDER:
            self.reorder(REORDER)
        ops = self.ops
        pos = {}
        for c, l in self.chan_ops.items():
            for i, o in enumerate(l):
                pos[o] = (c, i)
        seen = {e: {} for e in self.E}
        sig = set()
        for oid, o in enumerate(ops):
            need = {}
            e = o["eng"]
            for d in o["deps"]:
                c, i = pos[d]
                if c == "pe" and e == "pe":
                    continue
                if c == e and e in ("dve", "act") and pos[oid][1] - i - 1 >= SAME_GAP:
                    continue
                if i > seen[e].get(c, -1) and i > need.get(c, -1):
                    need[c] = i
            o["waits"] = need
            for c, i in need.items():
                seen[e][c] = i
                sig.add(self.chan_ops[c][i])
        sems = {}
        for c in self.chan_ops:
            sems[c] = self.es.enter_context(self.nc.semaphore("s_" + c.replace(":", "_")))
        cnt = {c: 0 for c in self.chan_ops}
        sigval = {}
        for oid, o in enumerate(ops):
            c = o["chan"]
            if o["isdma"]:
                cnt[c] += 16 * o["ndma"]
                sigval[oid] = cnt[c]
            elif oid in sig:
                cnt[c] += 1
                sigval[oid] = cnt[c]
        for oid, o in enumerate(ops):
            e = self.E[o["eng"]]
            for c, i in o["waits"].items():
                e.wait_ge(sems[c], sigval[self.chan_ops[c][i]])
            ins = o["fn"](e)
            if o["isdma"]:
                if not isinstance(ins, (list, tuple)):
                    ins = [ins]
                assert len(ins) == o["ndma"]
                for x in ins:
                    x.then_inc(sems[o["chan"]], 16)
            elif oid in sig:
                ins.then_inc(sems[o["chan"]], 1)
        return len(ops)

    @staticmethod
    def _fs(ap):
        n = 1
        for d in list(ap.shape)[1:]:
            n *= int(d)
        return n

    def mm(self, out, lhsT, rhs, start=True, stop=True):
        c = max(64, self._fs(rhs)) / 2.2 * (4.0 if rhs.dtype == F32 else 1.0) + 8.0
        self.op("pe", lambda e: e.matmul(out, lhsT, rhs, start=start, stop=stop), [lhsT, rhs], [out], cost=c, tail=150.0)

    def tr(self, out, in_, ident):
        c = max(64, self._fs(ident)) / 2.2 + 8.0
        self.op("pe", lambda e: e.transpose(out, in_, ident), [in_, ident], [out], cost=c, tail=150.0)

    def act(self, out, in_, func, bias=None, scale=None, eng="act"):
        kw = {}
        if bias is not None:
            kw["bias"] = bias
        if scale is not None:
            kw["scale"] = scale
        self.op(eng, lambda e: e.activation(out, in_, func, **kw), [in_, bias, scale], [out], cost=200.0 + self._fs(out) / 1.2)

    def tt(self, out, in0, in1, op, eng="dve"):
        self.op(eng, lambda e: e.tensor_tensor(out=out, in0=in0, in1=in1, op=op), [in0, in1], [out],
                cost=(70.0 + self._fs(out) / 0.96) if eng == "dve" else (200.0 + self._fs(out) / 0.5))

    def ts(self, out, in0, s1, op0, s2=None, op1=None, eng="dve"):
        if op1 is None:
            self.op(eng, lambda e: e.tensor_scalar(out=out, in0=in0, scalar1=s1, scalar2=None, op0=op0),
                    [in0, s1], [out], cost=70.0 + self._fs(out) / 0.96)
        else:
            self.op(eng, lambda e: e.tensor_scalar(out=out, in0=in0, scalar1=s1, scalar2=s2, op0=op0, op1=op1),
                    [in0, s1, s2], [out], cost=70.0 + self._fs(out) / 0.96)

    def stt(self, out, in0, scalar, in1, op0, op1):
        self.op("dve", lambda e: e.scalar_tensor_tensor(out=out, in0=in0, scalar=scalar, in1=in1, op0=op0, op1=op1),
                [in0, scalar, in1], [out], cost=70.0 + self._fs(out) / 0.96)

    def cp(self, out, in_, eng="dve"):
        if eng == "act":
            self.op("act", lambda e: e.copy(out, in_), [in_], [out], cost=200.0 + self._fs(out) / 1.2)
        else:
            self.op(eng, lambda e: e.tensor_copy(out=out, in_=in_), [in_], [out],
                    cost=(70.0 + self._fs(out) / 0.96) if eng == "dve" else (200.0 + self._fs(out) / 0.6))

    def memset(self, ap, v, eng="dve"):
        self.op(eng, lambda e: e.memset(ap, v), [], [ap], cost=(70.0 + self._fs(ap) / 0.96) if eng == "dve" else (200.0 + self._fs(ap) / 0.6))

    def dma(self, eng, chan, pairs, reads=(), writes=()):
        pairs = list(pairs)
        self.op(eng, lambda e: [e.dma_start(out=o, in_=i) for (o, i) in pairs],
                list(reads) + [i for (_, i) in pairs], list(writes) + [o for (o, _) in pairs],
                dma=chan, ndma=len(pairs), cost=(1200.0 if eng == "pool" else 150.0) * len(pairs),
                tail=2500.0 + sum(self._fs(o) * 128 * 4 for (o, _) in pairs) / 150.0)


class Pool:
    def __init__(self, sch, name, shape, dt, n, psum=False):
        self.t = [sch.tile(f"{name}{i}", shape, dt, psum) for i in range(n)]
        self.i = 0

    def next(self):
        t = self.t[self.i % len(self.t)]
        self.i += 1
        return t


def build(NL=4, NS=4, dbg=None):
    nc = bass.Bass("TRN2", target_bir_lowering=False, dynamic_dma_scratch_size=4096)

    def din(name, shape, dt=F32):
        return nc.dram_tensor(name, list(shape), dt, kind="ExternalInput").ap()

    x_d = din("x", [NS, S, D])
    pos_d = din("pos", [NS, S], I32)
    w_in = din("w_in", [NL, D, 2664])
    a_ws = din("a_ws", [NL, 4, 128, 128])
    w_uq = din("b_w_uq", [NL, 192, 384])
    w_ukv = din("b_w_ukv", [NL, 128, 512])
    w_out = din("w_out", [NL, D, D])
    w_gate = din("w_gate", [NL, D, DFF])
    w_up = din("w_up", [NL, D, DFF])
    w_down = din("w_down", [NL, DFF, D])
    lnp = din("lnp", [NL, 4, D])
    pp_d = din("pp", [NL, 128, NPP])
    pr_d = din("pr", [NL, NPR])
    cst_d = din("cst", [128, NCONST])
    out_d = nc.dram_tensor("out", [NS, S, D], F32, kind="ExternalOutput").ap()
    dbg_d = None
    if dbg:
        dbg_d = nc.dram_tensor("dbg", [128, 8, S], F32, kind="ExternalOutput").ap()

    with ExitStack() as es:
        sc = Sched(nc, es)
        X = [sc.tile(f"X{t}", [128, D], F32) for t in range(T)]
        XTt = [sc.tile(f"XT{g}", [128, 4096], BF16) for g in range(NG)]
        XT = [t[:, :].rearrange("p (k c) -> p k c", k=8) for t in XTt]
        YB = [sc.tile(f"YB{g}", [128, 2, 512], BF16) for g in range(NG)]
        CST = sc.tile("CST", [128, NCONST], F32)
        IDB = sc.tile("IDB", [128, 128], BF16)
        TRIB = sc.tile("TRIB", [128, 128], BF16)
        LOWB = sc.tile("LOWB", [128, 128], BF16)
        MSK2 = sc.tile("MSK2", [128, 2, 128], BF16)
        ONEB = sc.tile("ONEB", [128, 128], BF16)
        IDF = CST[:, 0:128]
        TRIF = CST[:, 128:256]
        ONEF = CST[:, 384:512]
        COSC = sc.tile("COSC", [128, S], BF16)
        SINC = sc.tile("SINC", [128, S], BF16)
        PP = sc.tile("PP", [128, NPP], F32)
        PR = sc.tile("PR", [128, NPR], F32)
        WP = Pool(sc, "WP", [128, 8192], BF16, 2)
        PS = Pool(sc, "PS", [128, 512], F32, 6, psum=True)
        PSL = Pool(sc, "PSL", [128, 512], F32, 2, psum=True)
        SF = Pool(sc, "SF", [128, 512], F32, 4)
        SB = Pool(sc, "SB", [128, 512], BF16, 8)
        SM = Pool(sc, "SM", [128, 64], F32, 8)
        BIG = [sc.tile(f"BIG{i}", [128, 2048], F32) for i in range(3)]
        GB = sc.tile("GB", [128, 1024], F32)
        MID = [sc.tile(f"MID{i}", [128, 2048], BF16) for i in range(5)]
        LNGB = BIG[0][:, :].rearrange("p (a d) -> p a d", a=2)
        WOD = sc.tile("WOD", [128, 2048], BF16)
        WG8 = sc.tile("WG8", [128, 64], BF16)

        sc.dma("sp", "CST", [(CST[:], cst_d[:, :])])
        sc.cp(IDB[:], CST[:, 0:128])
        sc.cp(TRIB[:], CST[:, 128:256])
        sc.cp(LOWB[:], CST[:, 256:384])
        sc.cp(ONEB[:], CST[:, 384:512])
        sc.cp(MSK2[:, 0, :], CST[:, 256:384])
        sc.cp(MSK2[:, 1, :], CST[:, 128:256])

        def wview(wt, n, k=8):
            return wt[:, 0:k * n].rearrange("p (k c) -> p k c", k=k)

        def load_w(wt, src2d, n, k=8, off=0, pairs=None):
            v = wt[:, off:off + k * n].rearrange("p (k c) -> p k c", k=k)
            pr_ = (v, src2d.rearrange("(k p) c -> p k c", p=128))
            if pairs is None:
                sc.dma("pool", wt.name, [pr_])
            else:
                pairs.append(pr_)
            return v

        def make_xt(t):
            g, j = divmod(t, 4)
            for half in range(2):
                ps = PS.next()
                for kk in range(4):
                    k = half * 4 + kk
                    sc.tr(ps[:, kk * 128:(kk + 1) * 128], X[t][:, k * 128:(k + 1) * 128], IDF)
                dst = XT[g][:, half * 4:(half + 1) * 4, j * 128:(j + 1) * 128]
                src = ps[:, :].rearrange("p (k c) -> p k c", k=4)
                sc.cp(dst, src, eng="act")

        def layer_norm(t, gi):
            st = SM.next()
            for h in range(2):
                sc.op("dve", lambda e, h=h, st=st: e.bn_stats(out=st[:, h * 6:(h + 1) * 6], in_=X[t][:, h * 512:(h + 1) * 512]),
                      [X[t][:]], [st[:]])
            sc.op("dve", lambda e, st=st: e.bn_aggr(out=st[:, 16:18], in_=st[:, 0:12]), [st[:]], [st[:]])
            sc.ts(st[:, 18:19], st[:, 17:18], 1e-5, ALU.add)
            sc.act(st[:, 19:20], st[:, 18:19], AF.Sqrt)
            sc.op("dve", lambda e, st=st: e.reciprocal(out=st[:, 20:21], in_=st[:, 19:20]), [st[:]], [st[:]])
            sc.stt(st[:, 21:22], st[:, 16:17], -1.0, st[:, 20:21], ALU.mult, ALU.mult)
            sc.act(X[t][:], X[t][:], AF.Identity, bias=st[:, 21:22], scale=st[:, 20:21])
            sc.tt(X[t][:], X[t][:], LNGB[:, 0, :], ALU.mult)
            sc.tt(X[t][:], X[t][:], LNGB[:, 1, :], ALU.add, eng="pool")

        def load_lngb(l, which):
            sc.dma("sp", "LNGB", [(LNGB[:, 0, :], lnp[l, 2 * which:2 * which + 1, :].broadcast_to([128, D])),
                                  (LNGB[:, 1, :], lnp[l, 2 * which + 1:2 * which + 2, :].broadcast_to([128, D]))])

        first_acc = [True]

        def outproj_partial(l, m, wo):
            for t in range(T):
                g, j = divmod(t, 4)
                for cb in range(2):
                    ps = PS.next()
                    for k in range(2):
                        sc.mm(ps[:, :], YB[g][:, k, j * 128:(j + 1) * 128], wo[:, k, cb * 512:(cb + 1) * 512],
                              start=(k == 0), stop=(k == 1))
                    xs = X[t][:, cb * 512:(cb + 1) * 512]
                    if m == 0:
                        sc.stt(xs, xs, ALPHA, ps[:, :], ALU.mult, ALU.add)
                    else:
                        sc.tt(xs, xs, ps[:, :], ALU.add)

        def dump(m):
            if dbg:
                for g in range(NG):
                    tmp = SF.next()
                    for k in range(2):
                        sc.cp(tmp[:, :], YB[g][:, k, :])
                        sc.dma("sp", tmp.name, [(dbg_d[:, 2 * m + k, g * 512:(g + 1) * 512], tmp[:, :])])

        def mixer_a(l):
            wt = WP.next()
            pairs = []
            wa = load_w(wt, w_in[l, :, A0:A0 + 512], 512, pairs=pairs)
            wo = load_w(wt, w_out[l, 0:256, :], 1024, k=2, off=4096, pairs=pairs)
            sc.dma("pool", wt.name, pairs)
            wsT = MID[0]
            wsr = BIG[1]
            sc.dma("sp", wsr.name, [(wsr[:, 0:512].rearrange("p (h s) -> p h s", h=4),
                                     a_ws[l].rearrange("h t s -> t h s"))])
            ps = PS.next()
            for h in range(4):
                sc.tr(ps[:, h * 128:(h + 1) * 128], wsr[:, h * 128:(h + 1) * 128], IDF)
            sc.tt(wsT[:, 0:512].rearrange("p (h t) -> p h t", h=4), ps[:, :].rearrange("p (h t) -> p h t", h=4),
                  CST[:, 128:256].unsqueeze(1).broadcast_to([128, 4, 128]), ALU.mult)
            for t in range(T):
                g, j = divmod(t, 4)
                ps = PS.next()
                for k in range(8):
                    sc.mm(ps[:, :], XT[g][:, k, j * 128:(j + 1) * 128], wa[:, k, :], start=(k == 0), stop=(k == 7))
                gl = SF.next()
                sc.act(gl[:, :], ps[:, :], AF.Gelu)
                st = SM.next()
                sc.op("dve", lambda e, st=st, gl=gl: e.bn_stats(out=st[:, 0:6], in_=gl[:, 256:512]), [gl[:]], [st[:]])
                sc.op("dve", lambda e, st=st: e.bn_aggr(out=st[:, 16:18], in_=st[:, 0:6]), [st[:]], [st[:]])
                sc.ts(st[:, 18:19], st[:, 17:18], 1e-5, ALU.add)
                sc.act(st[:, 19:20], st[:, 18:19], AF.Sqrt)
                sc.op("dve", lambda e, st=st: e.reciprocal(out=st[:, 20:21], in_=st[:, 19:20]), [st[:]], [st[:]])
                sc.ts(gl[:, 256:512], gl[:, 256:512], st[:, 16:17], ALU.subtract, st[:, 20:21], ALU.mult)
                sc.tt(gl[:, 256:512], gl[:, 256:512], PR[:, 0:256], ALU.mult)
                vn = SB.next()
                sc.tt(vn[:, 0:256], gl[:, 256:512], PR[:, 256:512], ALU.add)
                pm = PS.next()
                for h in range(4):
                    sc.mm(pm[:, h * 64:(h + 1) * 64], wsT[:, h * 128:(h + 1) * 128], vn[:, h * 64:(h + 1) * 64])
                ya = SB.next()
                for h in range(4):
                    sc.stt(ya[:, h * 64:(h + 1) * 64], pm[:, h * 64:(h + 1) * 64], PP[:, 24 + h:25 + h],
                           gl[:, h * 64:(h + 1) * 64], ALU.add, ALU.mult)
                pt = PS.next()
                ptb = pt[:, 0:128].bitcast(BF16)
                for k in range(2):
                    sc.tr(ptb[:, k * 128:(k + 1) * 128], ya[:, k * 128:(k + 1) * 128], IDB[:])
                sc.cp(YB[g][:, :, j * 128:(j + 1) * 128], ptb[:, :].rearrange("p (k c) -> p k c", k=2), eng="act")
            dump(0)
            outproj_partial(l, 0, wo)

        def rope64(dst, src, gs):
            t1 = SF.next(); t2 = SF.next()
            sc.tt(t1[64:128, :], src[64:128, :], COSC[64:128, gs], ALU.mult)
            sc.tt(t2[64:96, :], src[96:128, :], SINC[96:128, gs], ALU.mult)
            sc.tt(t2[96:128, :], src[64:96, :], SINC[64:96, gs], ALU.mult)
            sc.tt(dst, t1[64:128, :], t2[64:128, :], ALU.add)

        def mixer_b(l):
            wt = WP.next()
            pairs = []
            wcq = load_w(wt, w_in[l, :, B0:B0 + 192], 192, pairs=pairs)
            wckv = load_w(wt, w_in[l, :, B0 + 192:B0 + 320], 128, off=1536, pairs=pairs)
            wo = load_w(wt, w_out[l, 256:512, :], 1024, k=2, off=3584, pairs=pairs)
            wkv = wt[:, 6656:6656 + 512]
            pairs.append((wkv, w_ukv[l]))
            krs = load_w(wt, w_in[l, :, B0 + 320:B0 + 352], 32, off=7168, pairs=pairs)
            uqs = wt[:, 7424:7424 + 768].rearrange("p (k c) -> p k c", k=2)
            pairs.append((uqs[:, 0, :], w_uq[l, 0:128, :]))
            pairs.append((uqs[0:64, 1, :], w_uq[l, 128:192, :]))
            sc.dma("pool", wt.name, pairs)
            wkr = wt[:, 2560:2560 + 1024].rearrange("p (k c) -> p k c", k=8)
            sc.memset(wt[:, 2560:3584], 0.0)
            sc.cp(wkr[:, :, 64:96:2], krs[:, :, 0:16])
            sc.cp(wkr[:, :, 96:128:2], krs[:, :, 16:32])
            wq = wt[:, 5632:5632 + 1024].rearrange("p (k h c) -> p k h c", k=2, h=4)
            sc.memset(wt[:, 5632:6656], 0.0)
            uq4 = wt[:, 7424:7424 + 768].rearrange("p (k h c) -> p k h c", k=2, h=4)
            for kc, rows in ((0, 128), (1, 64)):
                sc.cp(wq[0:rows, kc, :, 0:64], uq4[0:rows, kc, :, 0:64])
                sc.cp(wq[0:rows, kc, :, 64:96:2], uq4[0:rows, kc, :, 64:80])
                sc.cp(wq[0:rows, kc, :, 96:128:2], uq4[0:rows, kc, :, 80:96])

            CQ0 = MID[0]; CQ1K = MID[1]; CKVN = MID[2]
            for g in range(NG):
                gs = slice(g * 512, (g + 1) * 512)
                pq0 = PS.next(); pq1 = PS.next(); pkv = PS.next(); pkr = PS.next()
                for k in range(8):
                    sc.mm(pq0[:, :], wcq[:, k, 0:128], XT[g][:, k, :], start=(k == 0), stop=(k == 7))
                for k in range(8):
                    sc.mm(pq1[0:64, :], wcq[:, k, 128:192], XT[g][:, k, :], start=(k == 0), stop=(k == 7))
                for k in range(8):
                    sc.mm(pkv[:, :], wckv[:, k, :], XT[g][:, k, :], start=(k == 0), stop=(k == 7))
                for k in range(8):
                    sc.mm(pkr[:, :], wkr[:, k, :], XT[g][:, k, :], start=(k == 0), stop=(k == 7))
                s0 = SF.next(); s1 = SF.next(); s2 = SF.next()
                sc.act(s0[:, :], pq0[:, :], AF.Square)
                sc.act(s1[0:64, :], pq1[0:64, :], AF.Square)
                sc.act(s2[:, :], pkv[:, :], AF.Square)
                pss = PS.next()
                sc.mm(pss[:, :], ONEF, s0[:, :], start=True, stop=False)
                sc.mm(pss[:, :], CST[0:64, 384:512], s1[0:64, :], start=False, stop=True)
                rq = SF.next()
                sc.ts(rq[:, :], pss[:, :], 1.0 / 192, ALU.mult, 1e-6, ALU.add)
                sc.act(rq[:, :], rq[:, :], AF.Sqrt)
                sc.op("dve", lambda e, rq=rq: e.reciprocal(out=rq[:, :], in_=rq[:, :]), [rq[:]], [rq[:]])
                sc.stt(CQ0[:, gs], pq0[:, :], PP[:, 0:1], rq[:, :], ALU.mult, ALU.mult)
                sc.stt(CQ1K[0:64, gs], pq1[0:64, :], PP[0:64, 1:2], rq[0:64, :], ALU.mult, ALU.mult)
                pss2 = PS.next()
                sc.mm(pss2[:, :], ONEF, s2[:, :])
                rk = SF.next()
                sc.ts(rk[:, :], pss2[:, :], 1.0 / 128, ALU.mult, 1e-6, ALU.add)
                sc.act(rk[:, :], rk[:, :], AF.Sqrt)
                sc.op("dve", lambda e, rk=rk: e.reciprocal(out=rk[:, :], in_=rk[:, :]), [rk[:]], [rk[:]])
                sc.stt(CKVN[:, gs], pkv[:, :], PP[:, 2:3], rk[:, :], ALU.mult, ALU.mult)
                rope64(CQ1K[64:128, gs], pkr, gs)

            scale = 96.0 ** -0.5
            for h in range(4):
                QT = MID[3]; KT = MID[4]
                va = BIG[h % 2][:, :].bitcast(BF16)[:, 0:2048].rearrange("p (t c) -> p t c", t=T)
                for g in range(NG):
                    gs = slice(g * 512, (g + 1) * 512)
                    pq = PS.next()
                    sc.mm(pq[:, :], wq[:, 0, h, :], CQ0[:, gs], start=True, stop=False)
                    sc.mm(pq[:, :], wq[0:64, 1, h, :], CQ1K[0:64, gs], start=False, stop=True)
                    sc.cp(QT[0:64, gs], pq[0:64, :], eng="act")
                    rope64(QT[64:128, gs], pq, gs)
                    pk = PS.next()
                    sc.mm(pk[0:64, :], wkv[:, h * 128:h * 128 + 64], CKVN[:, gs])
                    sc.cp(KT[0:64, gs], pk[0:64, :], eng="act")
                    sc.cp(KT[64:128, gs], CQ1K[64:128, gs], eng="pool")
                    pv = PS.next()
                    for j in range(4):
                        t = g * 4 + j
                        sc.mm(pv[:, j * 64:(j + 1) * 64], CKVN[:, t * 128:(t + 1) * 128], wkv[:, h * 128 + 64:h * 128 + 128])
                    sc.cp(va[:, g * 4:(g + 1) * 4, 0:64], pv[:, 0:256].rearrange("p (j c) -> p j c", j=4), eng="act")
                    sc.memset(va[:, g * 4:(g + 1) * 4, 64:128], 1.0, eng="pool")
                for Qb in range(4):
                    po = PSL.next()
                    nkb = 4 * Qb + 4
                    for kb in range(nkb):
                        qs = max(Qb * 512, kb * 128)
                        qe = (Qb + 1) * 512
                        n = qe - qs
                        ps = PS.next()
                        sc.mm(ps[:, 0:n], KT[:, kb * 128:(kb + 1) * 128], QT[:, qs:qe])
                        pt = SB.next()
                        sc.act(pt[:, 0:n], ps[:, 0:n], AF.Exp, scale=scale)
                        if kb * 128 >= Qb * 512:
                            sc.tt(pt[:, 0:128], pt[:, 0:128], TRIB[:], ALU.mult, eng="pool")
                        sc.mm(po[:, qs - Qb * 512:512], va[:, kb, :], pt[:, 0:n], start=(kb == 0), stop=(kb == nkb - 1))
                    rd = SF.next()
                    sc.cp(rd[0:64, :], po[64:128, :], eng="act")
                    sc.op("dve", lambda e, rd=rd: e.reciprocal(out=rd[0:64, :], in_=rd[0:64, :]), [rd[:]], [rd[:]])
                    hp = (h % 2) * 64
                    sc.tt(YB[Qb][hp:hp + 64, h // 2, :], po[0:64, :], rd[0:64, :], ALU.mult)
            dump(1)
            outproj_partial(l, 1, wo)

        def mixer_c(l):
            wt = WP.next()
            pairs = []
            wc = load_w(wt, w_in[l, :, C0:C0 + 768], 768, pairs=pairs)
            wo = load_w(wt, w_out[l, 512:768, :], 1024, k=2, off=6144, pairs=pairs)
            sc.dma("pool", wt.name, pairs)
            QC = [MID[0], MID[1]]; KC = [MID[2], MID[3]]; VCt = MID[4]
            for g in range(NG):
                gs = slice(g * 512, (g + 1) * 512)
                for ci in range(4):
                    ps = PS.next()
                    for k in range(8):
                        sc.mm(ps[:, :], wc[:, k, ci * 128:(ci + 1) * 128], XT[g][:, k, :], start=(k == 0), stop=(k == 7))
                    dst = (QC if ci < 2 else KC)[ci % 2]
                    t1 = SF.next(); t2 = SF.next()
                    sc.tt(t1[:, :], ps[:, :], COSC[:, gs], ALU.mult)
                    for q in range(4):
                        src = (q ^ 1) * 32
                        sc.tt(t2[q * 32:(q + 1) * 32, :], ps[src:src + 32, :], SINC[src:src + 32, gs], ALU.mult)
                    sc.tt(dst[:, gs], t1[:, :], t2[:, :], ALU.add)
            vcnt = 0
            for h in range(4):
                c, hp = h // 2, (h % 2) * 64
                if h % 2 == 0:
                    for g in range(NG):
                        ps = PS.next()
                        for k in range(8):
                            sc.mm(ps[:, :], wc[:, k, (4 + c) * 128:(5 + c) * 128], XT[g][:, k, :], start=(k == 0), stop=(k == 7))
                        sc.cp(VCt[:, g * 512:(g + 1) * 512], ps[:, :], eng="act")
                ACC = BIG[1]
                first = True
                for d in (1, 4, 16):
                    nb = 16 // d
                    va = BIG[0][:, :].bitcast(BF16)[:, (vcnt % 2) * 2048:(vcnt % 2 + 1) * 2048].rearrange("p (b c) -> p b c", b=16)
                    vcnt += 1

                    def sel(r, n, d=d):
                        st = n * 128 * d + r
                        return slice(st, st + 127 * d + 1, d)
                    blocks = [(r, n) for r in range(d) for n in range(nb)]
                    for bi in range(0, 16, 4):
                        pt = PS.next()
                        ptb = pt[:, 0:128].bitcast(BF16)
                        for q in range(4):
                            r, n = blocks[bi + q]
                            sc.tr(ptb[:, q * 64:(q + 1) * 64], VCt[hp:hp + 64, sel(r, n)], IDB[hp:hp + 64, hp:hp + 64])
                        sc.cp(va[:, bi:bi + 4, 0:64], ptb[:, :].rearrange("p (q c) -> p q c", q=4), eng="act")
                    sc.memset(va[:, :, 64:128], 1.0, eng="pool")
                    for bi, (r, n) in enumerate(blocks):
                        ps = PS.next()
                        two = n > 0
                        if two:
                            sc.mm(ps[:, 0:128], KC[c][hp:hp + 64, sel(r, n - 1)], QC[c][hp:hp + 64, sel(r, n)])
                        sc.mm(ps[:, 128:256], KC[c][hp:hp + 64, sel(r, n)], QC[c][hp:hp + 64, sel(r, n)])
                        pt = SB.next()
                        lo = 0 if two else 128
                        sc.act(pt[:, lo:256], ps[:, lo:256], AF.Exp, scale=0.125)
                        sc.tt(pt[:, lo:256], pt[:, lo:256], MSK2[:, :, :].rearrange("p a b -> p (a b)")[:, lo:256], ALU.mult, eng=("pool" if bi % 2 == 0 else "dve"))
                        po = PS.next()
                        if two:
                            sc.mm(po[:, 0:128], va[:, bi - 1, :], pt[:, 0:128], start=True, stop=False)
                        sc.mm(po[:, 0:128], va[:, bi, :], pt[:, 128:256], start=(not two), stop=True)
                        if first:
                            sc.cp(ACC[:, sel(r, n)], po[:, 0:128], eng="act")
                        else:
                            sc.tt(ACC[:, sel(r, n)], ACC[:, sel(r, n)], po[:, 0:128], ALU.add)
                    first = False
                for g in range(NG):
                    gs = slice(g * 512, (g + 1) * 512)
                    rd = SF.next()
                    sc.cp(rd[0:64, :], ACC[64:128, gs], eng="act")
                    sc.op("dve", lambda e, rd=rd: e.reciprocal(out=rd[0:64, :], in_=rd[0:64, :]), [rd[:]], [rd[:]])
                    sc.tt(YB[g][hp:hp + 64, c, :], ACC[0:64, gs], rd[0:64, :], ALU.mult)
            dump(2)
            outproj_partial(l, 2, wo)

        def mixer_d(l):
            wt = WP.next()
            pairs = []
            wqk = load_w(wt, w_in[l, :, D0:D0 + 512], 512, pairs=pairs)
            wvo = load_w(wt, w_in[l, :, D0 + 512:D0 + 1024], 512, off=4096, pairs=pairs)
            sc.dma("pool", wt.name, pairs)
            wg8 = load_w(WG8, w_in[l, :, D0 + 1024:D0 + 1032], 8)
            wo = load_w(WOD, w_out[l, 768:1024, :], 1024, k=2)
            pg = PSL.next()
            for t in range(T):
                g, j = divmod(t, 4)
                for k in range(8):
                    sc.mm(pg[:, t * 8:(t + 1) * 8], XT[g][:, k, j * 128:(j + 1) * 128], wg8[:, k, :], start=(k == 0), stop=(k == 7))

            def tb(i):
                return GB[:, i * 64:(i + 1) * 64]

            def tb3(i):
                return GB[:, i * 64:(i + 1) * 64].rearrange("p (c h) -> p c h", h=4)
            pg3 = pg[:, 0:128].rearrange("p (c e) -> p c e", e=8)
            GI, GF, AB, EX, LN_, FC, BI_, BB, AA, WK, THR, DEC, TOT = range(13)
            sc.tt(tb3(GI), pg3[:, :, 0:4], PR[:, 512:516].unsqueeze(1).broadcast_to([128, 16, 4]), ALU.add)
            sc.tt(tb3(GF), pg3[:, :, 4:8], PR[:, 516:520].unsqueeze(1).broadcast_to([128, 16, 4]), ALU.add)
            sc.act(tb(AB), tb(GF), AF.Abs)
            sc.act(tb(EX), tb(AB), AF.Exp, scale=-1.0)
            sc.act(tb(LN_), tb(EX), AF.Ln, bias=1.0)
            sc.stt(tb(FC), tb(GF), 0.0, tb(LN_), ALU.min, ALU.subtract)
            if DSTOP == 1:
                return
            pc = PS.next()
            sc.mm(pc[:, 0:64], TRIF, tb(FC))
            sc.mm(pc[:, 64:128], ONEF, tb(FC))
            sc.cp(tb(TOT), pc[:, 64:128])
            for h in range(4):
                v_in = GB[:, TOT * 64 + h:TOT * 64 + 64:4]
                v_out = GB[:, BI_ * 64 + h:BI_ * 64 + 64:4]
                sc.op("dve", lambda e, v_in=v_in, v_out=v_out: e.tensor_tensor_scan(
                    out=v_out, data0=CST[:, 384:400], data1=v_in, initial=0.0, op0=ALU.mult, op1=ALU.add), [GB[:], CST[:]], [GB[:]])
            sc.tt(tb(BI_), tb(BI_), tb(TOT), ALU.subtract)
            sc.tt(tb(BB), pc[:, 0:64], tb(BI_), ALU.add)
            sc.tt(tb(AA), tb(GI), tb(BB), ALU.subtract)
            if DSTOP == 2:
                return
            pa = PS.next()
            sc.tr(pa[0:64, 0:128], tb(AA), IDF)
            cm = SM.next()
            sc.op("dve", lambda e: e.tensor_reduce(out=cm[0:64, 0:1], in_=pa[0:64, 0:128], axis=mybir.AxisListType.X, op=ALU.max),
                  [pa[:]], [cm[:]])
            pb = PS.next()
            sc.tr(pb[0:1, 0:64], cm[0:64, 0:1], CST[0:64, 0:64])
            R = SF.next()
            sc.cp(R[0:1, 128:192], pb[0:1, 0:64])
            for h in range(4):
                sc.op("dve", lambda e, h=h: e.tensor_tensor_scan(
                    out=R[0:1, h:64:4], data0=R[0:1, 128 + h:192:4], data1=CST[0:1, 260:276], initial=0.0,
                    op0=ALU.max, op1=ALU.max), [R[:], CST[:]], [R[:]])
            sc.memset(R[0:1, 64:68], 0.0)
            sc.cp(R[0:1, 68:128], R[0:1, 0:60])
            pm = PS.next()
            sc.mm(pm[:, 0:128], CST[0:1, 384:512], R[0:1, 0:128])
            MR = SF.next()
            sc.cp(MR[:, 0:128], pm[:, 0:128])
            sc.tt(tb(WK), tb(AA), MR[:, 0:64], ALU.subtract)
            sc.act(tb(WK), tb(WK), AF.Exp)
            sc.ts(tb(WK), tb(WK), 0.125, ALU.mult)
            sc.tt(tb(THR), tb(BB), MR[:, 0:64], ALU.add)
            sc.act(tb(THR), tb(THR), AF.Exp, scale=-1.0)
            sc.tt(tb(DEC), MR[:, 64:128], MR[:, 0:64], ALU.subtract)
            sc.act(tb(DEC), tb(DEC), AF.Exp)
            if DSTOP == 3:
                return
            QK = [MID[0], MID[1], MID[2], MID[3]]
            for ci in range(4):
                Z = BIG[0]
                for g in range(NG):
                    ps = PS.next()
                    for k in range(8):
                        sc.mm(ps[:, :], wqk[:, k, ci * 128:(ci + 1) * 128], XT[g][:, k, :], start=(k == 0), stop=(k == 7))
                    sc.cp(Z[:, g * 512:(g + 1) * 512], ps[:, :], eng="act")
                A = BIG[1]
                sc.ts(A[:, :], Z[:, :], PP[:, 3 + ci * 4 + 3:4 + ci * 4 + 3], ALU.mult, PP[:, 19 + ci:20 + ci], ALU.add)
                for j in range(3):
                    sh = 3 - j
                    sc.stt(A[:, sh:S], Z[:, 0:S - sh], PP[:, 3 + ci * 4 + j:4 + ci * 4 + j], A[:, sh:S], ALU.mult, ALU.add)
                sc.act(QK[ci][:, :], A[:, :], AF.Silu)
            if DSTOP == 4:
                return
            CS = SF.next()
            cs3 = CS[:, 0:256].rearrange("p (a c) -> p a c", a=2)
            sc.memset(CS[:, 0:256], 0.0)
            SFd = [BIG[2][:, i * 512:(i + 1) * 512] for i in range(4)]
            for c in range(T):
                g, j = divmod(c, 4)
                cs = slice(c * 128, (c + 1) * 128)
                pvo = PS.next()
                for k in range(8):
                    sc.mm(pvo[:, :], XT[g][:, k, j * 128:(j + 1) * 128], wvo[:, k, :], start=(k == 0), stop=(k == 7))
                vaug = SB.next()
                va3 = vaug[:, 0:512].rearrange("p (h c) -> p h c", h=4)
                sc.cp(va3[:, :, 0:64], pvo[:, 0:256].rearrange("p (h c) -> p h c", h=4), eng="act")
                sc.memset(va3[:, :, 64:128], 1.0, eng="pool")
                so = MID[4][:, (c % 2) * 256:(c % 2) * 256 + 256]
                sc.act(so, pvo[:, 256:512], AF.Sigmoid)
                pk = PS.next()
                pkb = pk[:, 0:128].bitcast(BF16)
                for i in range(2):
                    sc.tr(pkb[:, i * 128:(i + 1) * 128], QK[2 + i][:, cs], IDB[:])
                kw = SB.next()
                sc.tt(kw[:, 0:256].rearrange("p (h c) -> p h c", h=4), pkb[:, :].rearrange("p (h c) -> p h c", h=4),
                      GB[:, WK * 64 + c * 4:WK * 64 + c * 4 + 4].unsqueeze(2).broadcast_to([128, 4, 64]), ALU.mult)
                pS2 = [PS.next(), PS.next()]
                for h in range(4):
                    hp = (h % 2) * 64
                    sc.mm(pS2[h % 2][:, (h // 2) * 128:(h // 2 + 1) * 128], QK[2 + h // 2][hp:hp + 64, cs], QK[h // 2][hp:hp + 64, cs])
                sq = SB.next()
                for h in range(4):
                    sc.stt(sq[:, h * 128:(h + 1) * 128], pS2[h % 2][:, (h // 2) * 128:(h // 2 + 1) * 128],
                           GB[:, WK * 64 + c * 4 + h:WK * 64 + c * 4 + h + 1], TRIB[:], ALU.mult, ALU.mult)
                for hf in range(2):
                    rows = slice(hf * 64, hf * 64 + 64)
                    dec = GB[rows, DEC * 64 + c * 4 + hf:DEC * 64 + c * 4 + 4:2]
                    sc.tt(cs3[rows, :, :], cs3[rows, :, :], dec.unsqueeze(2).broadcast_to([64, 2, 128]), ALU.mult)
                cb = SB.next()
                cb3 = cb[:, 0:256].rearrange("p (a c) -> p a c", a=2)
                sc.cp(cb[:, 0:256], CS[:, 0:256], eng="act")
                ph2 = [PS.next(), PS.next()]
                for h in range(4):
                    hp = (h % 2) * 64
                    po_ = ph2[h % 2][:, (h // 2) * 128:(h // 2 + 1) * 128]
                    sc.mm(po_, sq[:, h * 128:(h + 1) * 128], va3[:, h, :], start=True, stop=False)
                    sc.mm(po_, QK[h // 2][hp:hp + 64, cs], cb3[hp:hp + 64, h // 2, :], start=False, stop=True)
                pu = PS.next()
                for a in range(2):
                    sc.mm(pu[:, a * 256:(a + 1) * 256], kw[:, a * 128:(a + 1) * 128], vaug[:, a * 256:(a + 1) * 256])
                pu3 = pu[:, 0:512].rearrange("p (a c) -> p a c", a=2)
                sc.tt(cs3[0:64, :, :], cs3[0:64, :, :], pu3[0:64, :, 0:128], ALU.add)
                sc.tt(cs3[64:128, :, :], cs3[64:128, :, :], pu3[64:128, :, 128:256], ALU.add)
                dn = SM.next()
                for par in range(2):
                    p3 = ph2[par][:, 0:256].rearrange("p (a c) -> p a c", a=2)
                    sc.act(dn[:, par:4:2], p3[:, :, 64], AF.Abs)
                sc.tt(dn[:, 0:4], dn[:, 0:4], GB[:, THR * 64 + c * 4:THR * 64 + c * 4 + 4], ALU.max)
                sc.op("dve", lambda e, dn=dn: e.reciprocal(out=dn[:, 4:8], in_=dn[:, 0:4]), [dn[:]], [dn[:]])
                hy = SFd[c % 4]
                hy3 = hy[:, 0:256].rearrange("p (h c) -> p h c", h=4)
                for par in range(2):
                    p3 = ph2[par][:, 0:256].rearrange("p (a c) -> p a c", a=2)
                    sc.tt(hy3[:, par:4:2, :], p3[:, :, 0:64],
                          dn[:, 4 + par:8:2].unsqueeze(2).broadcast_to([128, 2, 64]), ALU.mult)
                yd = SB.next()
                sc.tt(yd[:, 0:256], hy[:, 0:256], so, ALU.mult, eng="pool")
                pt = PS.next()
                ptb = pt[:, 0:128].bitcast(BF16)
                for k in range(2):
                    sc.tr(ptb[:, k * 128:(k + 1) * 128], yd[:, k * 128:(k + 1) * 128], IDB[:])
                sc.cp(YB[g][:, :, j * 128:(j + 1) * 128], ptb[:, :].rearrange("p (k c) -> p k c", k=2), eng="act")
            dump(3)
            outproj_partial(l, 3, wo)

        def ffn(l):
            chunks = [(i * 512, 512) for i in range(5)] + [(2560, 256)]
            for ci, (c0, cw) in enumerate(chunks):
                nhb = cw // 128
                wt = WP.next()
                pairs = []
                wg = load_w(wt, w_gate[l, :, c0:c0 + cw], cw, pairs=pairs)
                wu = load_w(wt, w_up[l, :, c0:c0 + cw], cw, off=4096, pairs=pairs)
                sc.dma("pool", wt.name, pairs)
                wt2 = BIG[1 + ci % 2][:, :].bitcast(BF16)
                vd = wt2[:, 0:nhb * 1024].rearrange("p (k c) -> p k c", k=nhb)
                sc.dma("pool", f"BIG{1 + ci % 2}", [(vd, w_down[l, c0:c0 + cw, :].rearrange("(k p) c -> p k c", p=128))])
                wd = vd
                for g in range(NG):
                    hT = MID[(ci * NG + g) % 2]
                    for hb in range(nhb):
                        pg_ = PS.next(); pu_ = PS.next()
                        for k in range(8):
                            sc.mm(pg_[:, :], wg[:, k, hb * 128:(hb + 1) * 128], XT[g][:, k, :], start=(k == 0), stop=(k == 7))
                        for k in range(8):
                            sc.mm(pu_[:, :], wu[:, k, hb * 128:(hb + 1) * 128], XT[g][:, k, :], start=(k == 0), stop=(k == 7))
                        sl = SF.next()
                        sc.act(sl[:, :], pg_[:, :], AF.Silu)
                        sc.tt(hT[:, hb * 512:(hb + 1) * 512], sl[:, :], pu_[:, :], ALU.mult)
                    for j in range(4):
                        t = g * 4 + j
                        for cb in range(2):
                            ps = PS.next()
                            for hb in range(nhb):
                                sc.mm(ps[:, :], hT[:, hb * 512 + j * 128:hb * 512 + (j + 1) * 128], wd[:, hb, cb * 512:(cb + 1) * 512],
                                      start=(hb == 0), stop=(hb == nhb - 1))
                            xs = X[t][:, cb * 512:(cb + 1) * 512]
                            if ci == 0:
                                sc.stt(xs, xs, ALPHA, ps[:, :], ALU.mult, ALU.add)
                            else:
                                sc.tt(xs, xs, ps[:, :], ALU.add)

        def rope_tables(s):
            PI = XTt[0][:, :].bitcast(F32)
            PIi = XTt[0][:, :].bitcast(I32)
            PF = XTt[1][:, :].bitcast(F32)
            RR = XTt[2][:, :].bitcast(F32)
            KK = XTt[3][:, :].bitcast(F32)
            KKi = XTt[3][:, :].bitcast(I32)
            sc.dma("sp", "XT0", [(PIi, pos_d[s:s + 1, :].broadcast_to([128, S]))])
            sc.cp(PF, PIi)
            two_pi = 2.0 * math.pi
            c1 = 6.28125
            c2 = float(np.float32(two_pi - c1))
            c3 = float(two_pi - c1 - c2)
            sc.ts(RR, PF, CST[:, 513:514], ALU.mult)
            for shift, dst, sgn in ((0.0, SINC, True), (math.pi / 2, COSC, False)):
                sc.ts(KKi, RR, shift, ALU.add, 1.0 / two_pi, ALU.mult)
                sc.cp(PI, KKi)
                sc.stt(KK, PI, -c1, RR, ALU.mult, ALU.add)
                sc.stt(KK, PI, -c2, KK, ALU.mult, ALU.add)
                sc.stt(KK, PI, -c3, KK, ALU.mult, ALU.add)
                sc.ts(KK, KK, shift, ALU.add)
                sc.ts(KK, KK, math.pi, ALU.min, -math.pi, ALU.max)
                if sgn:
                    sc.act(KK, KK, AF.Sin)
                    sc.ts(dst[:, :], KK, CST[:, 514:515], ALU.mult)
                else:
                    sc.act(dst[:, :], KK, AF.Sin)

        for s in range(NS):
            rope_tables(s)
            for q in range(4):
                sc.dma("sp", f"Xld{q}", [(X[q * 4 + i][:, :], x_d[s, (q * 4 + i) * 128:(q * 4 + i + 1) * 128, :]) for i in range(4)])
            for t in range(T):
                make_xt(t)
            for l in range(NL):
                sc.dma("sp", "PP", [(PP[:, :], pp_d[l])])
                sc.dma("sp", "PR", [(PR[:, :], pr_d[l:l + 1, :].broadcast_to([128, NPR]))])
                acc_m = [0]
                for nm, fn in (("a", mixer_a), ("b", mixer_b), ("c", mixer_c), ("d", mixer_d)):
                    if dbg is None or nm in dbg:
                        fn(l)
                load_lngb(l, 0)
                for t in range(T):
                    layer_norm(t, 0)
                    make_xt(t)
                load_lngb(l, 1)
                if dbg is None or "f" in dbg:
                    ffn(l)
                for t in range(T):
                    layer_norm(t, 1)
                    if l < NL - 1:
                        make_xt(t)
            for q in range(4):
                sc.dma("sp", f"Xld{q}", [(out_d[s, (q * 4 + i) * 128:(q * 4 + i + 1) * 128, :], X[q * 4 + i][:, :]) for i in range(4)])
        sc.op("sp", lambda e: e.nop(), [X[t][:] for t in range(T)], [X[t][:] for t in range(T)])
        nops = sc.emit()
    return nc, nops


def make_consts():
    c = np.zeros((128, NCONST), np.float32)
    p = np.arange(128)
    c[:, 0:128] = np.eye(128, dtype=np.float32)
    c[:, 128:256] = (p[:, None] <= p[None, :]).astype(np.float32)
    c[:, 256:384] = (p[:, None] >= p[None, :]).astype(np.float32)
    c[:, 384:512] = 1.0
    th = np.float32(10000.0)
    c[:, 512] = np.power(th, -(np.arange(16, dtype=np.float32) / np.float32(16)))[p % 16]
    c[:, 513] = np.power(th, -(np.arange(32, dtype=np.float32) / np.float32(32)))[p % 32]
    c[:, 514] = np.where((p % 64) < 32, 1.0, -1.0)
    return c


def prep_inputs(inp, NL=4):
    f = lambda a: np.ascontiguousarray(np.asarray(a, dtype=np.float32))
    lnp = np.stack([f(inp["ln1_g"]), f(inp["ln1_b"]), f(inp["ln2_g"]), f(inp["ln2_b"])], axis=1)[:NL]
    pp = np.zeros((NL, 128, NPP), np.float32)
    pr = np.zeros((NL, NPR), np.float32)
    for l in range(NL):
        qn = f(inp["b_q_norm"])[l]
        pp[l, :, 0] = qn[0:128]
        pp[l, 0:64, 1] = qn[128:192]
        pp[l, :, 2] = f(inp["b_kv_norm"])[l]
        cw = f(inp["d_conv_w"])[l]
        for ci in range(4):
            for j in range(4):
                pp[l, :, 3 + ci * 4 + j] = cw[j, ci * 128:(ci + 1) * 128]
            pp[l, :, 19 + ci] = f(inp["d_conv_b"])[l, ci * 128:(ci + 1) * 128]
        pp[l, :, 24:28] = f(inp["a_bs"])[l].T
        pr[l, 0:256] = f(inp["a_ln_g"])[l]
        pr[l, 256:512] = f(inp["a_ln_b"])[l]
        pr[l, 512:516] = f(inp["d_igate_b"])[l]
        pr[l, 516:520] = f(inp["d_fgate_b"])[l]
    shared = dict(w_in=f(inp["w_in"])[:NL], a_ws=f(inp["a_ws"])[:NL], b_w_uq=f(inp["b_w_uq"])[:NL],
                  b_w_ukv=f(inp["b_w_ukv"])[:NL], w_out=f(inp["w_out"])[:NL], w_gate=f(inp["w_gate"])[:NL],
                  w_up=f(inp["w_up"])[:NL], w_down=f(inp["w_down"])[:NL], lnp=np.ascontiguousarray(lnp),
                  pp=pp, pr=pr, cst=make_consts())
    return shared


_CACHE = {}


def kernel(**inputs):
    n = 8
    x = np.asarray(inputs["x"], dtype=np.float32)
    pos = np.asarray(inputs["positions"], dtype=np.int32)
    shared = prep_inputs(inputs)
    if "nc" not in _CACHE:
        _CACHE["nc"] = build(4, 4)[0]
    nc = _CACHE["nc"]
    in_maps = []
    for c in range(n):
        m = dict(shared)
        m["x"] = np.ascontiguousarray(x[c * 4:(c + 1) * 4])
        m["pos"] = np.ascontiguousarray(pos[c * 4:(c + 1) * 4])
        in_maps.append(m)
    res = run_bass_kernel_spmd(nc, in_maps, core_ids=list(range(n)))
    return np.concatenate([r["out"] for r in res.results], axis=0)
```

```python
import math
import os
import numpy as np
from contextlib import ExitStack
import concourse.bass as bass
import concourse.mybir as mybir
from concourse.bass_utils import run_bass_kernel_spmd

F32 = mybir.dt.float32
BF16 = mybir.dt.bfloat16
I32 = mybir.dt.int32
ALU = mybir.AluOpType
AF = mybir.ActivationFunctionType

D = 1024
S = 2048
T = 16
NG = 4
DFF = 2816
ALPHA = 8.0 ** 0.25
A0, B0, C0, D0 = 0, 512, 864, 1632
NCONST = 520
NPP = 28
NPR = 520
DSTOP = int(os.environ.get('DSTOP', '0'))
REORDER = int(os.environ.get('KREORDER', '600'))
SAME_GAP = int(os.environ.get('KSAMEGAP', '1000000000'))


class Sched:
    def __init__(self, nc, es):
        self.nc = nc
        self.es = es
        self.E = {"pe": nc.tensor, "act": nc.scalar, "dve": nc.vector, "pool": nc.gpsimd, "sp": nc.sync}
        self.ops = []
        self.tiles = {}
        self.chan_ops = {}

    def tile(self, name, shape, dt, psum=False):
        ctx = (self.nc.psum_tensor if psum else self.nc.sbuf_tensor)(name, list(shape), dt)
        t = self.es.enter_context(ctx)
        self.tiles[name] = [None, {}]
        return t

    def _names(self, aps):
        r = []
        for a in aps:
            if a is None or isinstance(a, (int, float)):
                continue
            n = a.tensor.name
            if n in self.tiles and n not in r:
                r.append(n)
        return r

    def op(self, eng, fn, reads, writes, dma=None, ndma=1, cost=200.0, tail=0.0):
        rn = self._names(reads)
        wn = self._names(writes)
        deps = set()
        for n in rn:
            st = self.tiles[n]
            if st[0] is not None:
                deps.add(st[0])
        for n in wn:
            st = self.tiles[n]
            if st[0] is not None:
                deps.add(st[0])
            deps.update(st[1].values())
        oid = len(self.ops)
        chan = ("dma:" + dma) if dma else eng
        self.ops.append(dict(eng=eng, fn=fn, deps=deps, chan=chan, ndma=ndma, isdma=dma is not None, cost=cost, tail=tail))
        self.chan_ops.setdefault(chan, []).append(oid)
        for n in rn:
            if n not in wn:
                self.tiles[n][1][chan] = oid
        for n in wn:
            self.tiles[n][0] = oid
            self.tiles[n][1] = {}
        return oid

    def reorder(self, window=600):
        import heapq
        ops = self.ops
        n = len(ops)
        succ = [[] for _ in range(n)]
        indeg = [0] * n
        for i, o in enumerate(ops):
            for d in o["deps"]:
                succ[d].append(i)
            indeg[i] = len(o["deps"])
        blevel = [0.0] * n
        for i in range(n - 1, -1, -1):
            o = ops[i]
            b = 0.0
            for j in succ[i]:
                v = blevel[j] + (60.0 if ops[j]["eng"] == o["eng"] else 350.0)
                if v > b:
                    b = v
            blevel[i] = b + o["cost"] + o["tail"]
        fin = [0.0] * n
        ready_t = [0.0] * n
        engs = list(self.E)
        free = {e: 0.0 for e in engs}
        fut = {e: [] for e in engs}
        av = {e: [] for e in engs}
        deferred = []
        done = [False] * n
        base = 0
        order = []

        def admit(i):
            if i < base + window:
                heapq.heappush(fut[ops[i]["eng"]], (ready_t[i], i))
            else:
                heapq.heappush(deferred, i)
        for i in range(n):
            if indeg[i] == 0:
                admit(i)
        nsched = 0
        while nsched < n:
            best = None
            for e in engs:
                f, a = fut[e], av[e]
                while f and f[0][0] <= free[e]:
                    rt, i = heapq.heappop(f)
                    heapq.heappush(a, i)
                if a:
                    cand = (free[e], a[0], e, True)
                elif f:
                    cand = (f[0][0], f[0][1], e, False)
                else:
                    continue
                if best is None or cand[:2] < best[:2]:
                    best = cand
            if best is None:
                i = heapq.heappop(deferred)
                heapq.heappush(fut[ops[i]["eng"]], (ready_t[i], i))
                continue
            st, i, e, from_av = best
            if from_av:
                heapq.heappop(av[e])
            else:
                heapq.heappop(fut[e])
            o = ops[i]
            free[e] = st + o["cost"]
            fin[i] = st + o["cost"] + o["tail"]
            done[i] = True
            order.append(i)
            nsched += 1
            while base < n and done[base]:
                base += 1
            while deferred and deferred[0] < base + window:
                j = heapq.heappop(deferred)
                heapq.heappush(fut[ops[j]["eng"]], (ready_t[j], j))
            for j in succ[i]:
                lat = 60.0 if ops[j]["eng"] == e else 350.0
                if fin[i] + lat > ready_t[j]:
                    ready_t[j] = fin[i] + lat
                indeg[j] -= 1
                if indeg[j] == 0:
                    admit(j)
        remap = {old: new for new, old in enumerate(order)}
        new_ops = []
        for old in order:
            o = ops[old]
            o["deps"] = {remap[d] for d in o["deps"]}
            new_ops.append(o)
        self.ops = new_ops
        self.chan_ops = {}
        for i, o in enumerate(new_ops):
            self.chan_ops.setdefault(o["chan"], []).append(i)
        self.sim_ns = max(fin) if fin else 0.0

    def emit(self):
        if REORDER:
            self.reorder(REORDER)
        ops = self.ops
        pos = {}
        for c, l in self.chan_ops.items():
            for i, o in enumerate(l):
                pos[o] = (c, i)
        seen = {e: {} for e in self.E}
        sig = set()
        for oid, o in enumerate(ops):
            need = {}
            e = o["eng"]
            for d in o["deps"]:
                c, i = pos[d]
                if c == "pe" and e == "pe":
                    continue
                if c == e and e in ("dve", "act") and pos[oid][1] - i - 1 >= SAME_GAP:
                    continue
                if i > seen[e].get(c, -1) and i > need.get(c, -1):
                    need[c] = i
            o["waits"] = need
            for c, i in need.items():
                seen[e][c] = i
                sig.add(self.chan_ops[c][i])
        sems = {}
        for c in self.chan_ops:
            sems[c] = self.es.enter_context(self.nc.semaphore("s_" + c.replace(":", "_")))
        cnt = {c: 0 for c in self.chan_ops}
        sigval = {}
        for oid, o in enumerate(ops):
            c = o["chan"]
            if o["isdma"]:
                cnt[c] += 16 * o["ndma"]
                sigval[oid] = cnt[c]
            elif oid in sig:
                cnt[c] += 1
                sigval[oid] = cnt[c]
        for oid, o in enumerate(ops):
            e = self.E[o["eng"]]
            for c, i in o["waits"].items():
                e.wait_ge(sems[c], sigval[self.chan_ops[c][i]])
            ins = o["fn"](e)
            if o["isdma"]:
                if not isinstance(ins, (list, tuple)):
                    ins = [ins]
                assert len(ins) == o["ndma"]
                for x in ins:
                    x.then_inc(sems[o["chan"]], 16)
            elif oid in sig:
                ins.then_inc(sems[o["chan"]], 1)
        return len(ops)

    @staticmethod
    def _fs(ap):
        n = 1
        for d in list(ap.shape)[1:]:
            n *= int(d)
        return n

    def mm(self, out, lhsT, rhs, start=True, stop=True):
        c = max(64, self._fs(rhs)) / 2.2 * (4.0 if rhs.dtype == F32 else 1.0) + 8.0
        self.op("pe", lambda e: e.matmul(out, lhsT, rhs, start=start, stop=stop), [lhsT, rhs], [out], cost=c, tail=150.0)

    def tr(self, out, in_, ident):
        c = max(64, self._fs(ident)) / 2.2 + 8.0
        self.op("pe", lambda e: e.transpose(out, in_, ident), [in_, ident], [out], cost=c, tail=150.0)

    def act(self, out, in_, func, bias=None, scale=None, eng="act"):
        kw = {}
        if bias is not None:
            kw["bias"] = bias
        if scale is not None:
            kw["scale"] = scale
        self.op(eng, lambda e: e.activation(out, in_, func, **kw), [in_, bias, scale], [out], cost=200.0 + self._fs(out) / 1.2)

    def tt(self, out, in0, in1, op, eng="dve"):
        self.op(eng, lambda e: e.tensor_tensor(out=out, in0=in0, in1=in1, op=op), [in0, in1], [out],
                cost=(70.0 + self._fs(out) / 0.96) if eng == "dve" else (200.0 + self._fs(out) / 0.5))

    def ts(self, out, in0, s1, op0, s2=None, op1=None, eng="dve"):
        if op1 is None:
            self.op(eng, lambda e: e.tensor_scalar(out=out, in0=in0, scalar1=s1, scalar2=None, op0=op0),
                    [in0, s1], [out], cost=70.0 + self._fs(out) / 0.96)
        else:
            self.op(eng, lambda e: e.tensor_scalar(out=out, in0=in0, scalar1=s1, scalar2=s2, op0=op0, op1=op1),
                    [in0, s1, s2], [out], cost=70.0 + self._fs(out) / 0.96)

    def stt(self, out, in0, scalar, in1, op0, op1):
        self.op("dve", lambda e: e.scalar_tensor_tensor(out=out, in0=in0, scalar=scalar, in1=in1, op0=op0, op1=op1),
                [in0, scalar, in1], [out], cost=70.0 + self._fs(out) / 0.96)

    def cp(self, out, in_, eng="dve"):
        if eng == "act":
            self.op("act", lambda e: e.copy(out, in_), [in_], [out], cost=200.0 + self._fs(out) / 1.2)
        else:
            self.op(eng, lambda e: e.tensor_copy(out=out, in_=in_), [in_], [out],
                    cost=(70.0 + self._fs(out) / 0.96) if eng == "dve" else (200.0 + self._fs(out) / 0.6))

    def memset(self, ap, v, eng="dve"):
        self.op(eng, lambda e: e.memset(ap, v), [], [ap], cost=(70.0 + self._fs(ap) / 0.96) if eng == "dve" else (200.0 + self._fs(ap) / 0.6))

    def dma(self, eng, chan, pairs, reads=(), writes=()):
        pairs = list(pairs)
        self.op(eng, lambda e: [e.dma_start(out=o, in_=i) for (o, i) in pairs],
                list(reads) + [i for (_, i) in pairs], list(writes) + [o for (o, _) in pairs],
                dma=chan, ndma=len(pairs), cost=(1200.0 if eng == "pool" else 150.0) * len(pairs),
                tail=2500.0 + sum(self._fs(o) * 128 * 4 for (o, _) in pairs) / 150.0)


class Pool:
    def __init__(self, sch, name, shape, dt, n, psum=False):
        self.t = [sch.tile(f"{name}{i}", shape, dt, psum) for i in range(n)]
        self.i = 0

    def next(self):
        t = self.t[self.i % len(self.t)]
        self.i += 1
        return t


def build(NL=4, NS=4, dbg=None):
    nc = bass.Bass("TRN2", target_bir_lowering=False, dynamic_dma_scratch_size=4096)

    def din(name, shape, dt=F32):
        return nc.dram_tensor(name, list(shape), dt, kind="ExternalInput").ap()

    x_d = din("x", [NS, S, D])
    pos_d = din("pos", [NS, S], I32)
    w_in = din("w_in", [NL, D, 2664])
    a_ws = din("a_ws", [NL, 4, 128, 128])
    w_uq = din("b_w_uq", [NL, 192, 384])
    w_ukv = din("b_w_ukv", [NL, 128, 512])
    w_out = din("w_out", [NL, D, D])
    w_gate = din("w_gate", [NL, D, DFF])
    w_up = din("w_up", [NL, D, DFF])
    w_down = din("w_down", [NL, DFF, D])
    lnp = din("lnp", [NL, 4, D])
    pp_d = din("pp", [NL, 128, NPP])
    pr_d = din("pr", [NL, NPR])
    cst_d = din("cst", [128, NCONST])
    out_d = nc.dram_tensor("out", [NS, S, D], F32, kind="ExternalOutput").ap()
    dbg_d = None
    if dbg:
        dbg_d = nc.dram_tensor("dbg", [128, 8, S], F32, kind="ExternalOutput").ap()

    with ExitStack() as es:
        sc = Sched(nc, es)
        X = [sc.tile(f"X{t}", [128, D], F32) for t in range(T)]
        XTt = [sc.tile(f"XT{g}", [128, 4096], BF16) for g in range(NG)]
        XT = [t[:, :].rearrange("p (k c) -> p k c", k=8) for t in XTt]
        YB = [sc.tile(f"YB{g}", [128, 2, 512], BF16) for g in range(NG)]
        CST = sc.tile("CST", [128, NCONST], F32)
        IDB = sc.tile("IDB", [128, 128], BF16)
        TRIB = sc.tile("TRIB", [128, 128], BF16)
        LOWB = sc.tile("LOWB", [128, 128], BF16)
        MSK2 = sc.tile("MSK2", [128, 2, 128], BF16)
        ONEB = sc.tile("ONEB", [128, 128], BF16)
        MSK4 = sc.tile("MSK4", [128, 512], BF16)
        MSKT4 = sc.tile("MSKT4", [128, 512], BF16)
        IDF = CST[:, 0:128]
        TRIF = CST[:, 128:256]
        ONEF = CST[:, 384:512]
        COSC = sc.tile("COSC", [128, S], BF16)
        SINC = sc.tile("SINC", [128, S], BF16)
        PP = sc.tile("PP", [128, NPP], F32)
        PR = sc.tile("PR", [128, NPR], F32)
        WP = Pool(sc, "WP", [128, 8192], BF16, 2)
        PS = Pool(sc, "PS", [128, 512], F32, 6, psum=True)
        PSL = Pool(sc, "PSL", [128, 512], F32, 2, psum=True)
        SF = Pool(sc, "SF", [128, 512], F32, 4)
        SB = Pool(sc, "SB", [128, 512], BF16, 6)
        SM = Pool(sc, "SM", [128, 64], F32, 8)
        BIG = [sc.tile(f"BIG{i}", [128, 2048], F32) for i in range(3)]
        GB = sc.tile("GB", [128, 1024], F32)
        MID = [sc.tile(f"MID{i}", [128, 2048], BF16) for i in range(5)]
        LNGB = BIG[0][:, :].rearrange("p (a d) -> p a d", a=2)
        WOD = sc.tile("WOD", [128, 2048], BF16)
        WG8 = sc.tile("WG8", [128, 64], BF16)

        sc.dma("sp", "CST", [(CST[:], cst_d[:, :])])
        sc.cp(IDB[:], CST[:, 0:128])
        sc.cp(TRIB[:], CST[:, 128:256])
        sc.cp(LOWB[:], CST[:, 256:384])
        sc.cp(ONEB[:], CST[:, 384:512])
        sc.cp(MSK2[:, 0, :], CST[:, 256:384])
        sc.cp(MSK2[:, 1, :], CST[:, 128:256])
        for q in range(4):
            sc.cp(MSK4[:, q * 128:(q + 1) * 128], CST[:, 256:384] if q % 2 == 0 else CST[:, 128:256])
            sc.cp(MSKT4[:, q * 128:(q + 1) * 128], CST[:, 128:256])

        def wview(wt, n, k=8):
            return wt[:, 0:k * n].rearrange("p (k c) -> p k c", k=k)

        def load_w(wt, src2d, n, k=8, off=0, pairs=None):
            v = wt[:, off:off + k * n].rearrange("p (k c) -> p k c", k=k)
            pr_ = (v, src2d.rearrange("(k p) c -> p k c", p=128))
            if pairs is None:
                sc.dma("pool", wt.name, [pr_])
            else:
                pairs.append(pr_)
            return v

        def make_xt(t):
            g, j = divmod(t, 4)
            for half in range(2):
                ps = PS.next()
                for kk in range(4):
                    k = half * 4 + kk
                    sc.tr(ps[:, kk * 128:(kk + 1) * 128], X[t][:, k * 128:(k + 1) * 128], IDF)
                dst = XT[g][:, half * 4:(half + 1) * 4, j * 128:(j + 1) * 128]
                src = ps[:, :].rearrange("p (k c) -> p k c", k=4)
                sc.cp(dst, src, eng="act")

        def layer_norm(t, gi):
            st = SM.next()
            for h in range(2):
                sc.op("dve", lambda e, h=h, st=st: e.bn_stats(out=st[:, h * 6:(h + 1) * 6], in_=X[t][:, h * 512:(h + 1) * 512]),
                      [X[t][:]], [st[:]])
            sc.op("dve", lambda e, st=st: e.bn_aggr(out=st[:, 16:18], in_=st[:, 0:12]), [st[:]], [st[:]])
            sc.ts(st[:, 18:19], st[:, 17:18], 1e-5, ALU.add)
            sc.act(st[:, 19:20], st[:, 18:19], AF.Sqrt)
            sc.op("dve", lambda e, st=st: e.reciprocal(out=st[:, 20:21], in_=st[:, 19:20]), [st[:]], [st[:]])
            sc.stt(st[:, 21:22], st[:, 16:17], -1.0, st[:, 20:21], ALU.mult, ALU.mult)
            sc.act(X[t][:], X[t][:], AF.Identity, bias=st[:, 21:22], scale=st[:, 20:21])
            sc.tt(X[t][:], X[t][:], LNGB[:, 0, :], ALU.mult)
            sc.tt(X[t][:], X[t][:], LNGB[:, 1, :], ALU.add)

        def load_lngb(l, which):
            sc.dma("sp", "LNGB", [(LNGB[:, 0, :], lnp[l, 2 * which:2 * which + 1, :].broadcast_to([128, D])),
                                  (LNGB[:, 1, :], lnp[l, 2 * which + 1:2 * which + 2, :].broadcast_to([128, D]))])

        first_acc = [True]

        def outproj_partial(l, m, wo):
            for t in range(T):
                g, j = divmod(t, 4)
                for cb in range(2):
                    ps = PS.next()
                    for k in range(2):
                        sc.mm(ps[:, :], YB[g][:, k, j * 128:(j + 1) * 128], wo[:, k, cb * 512:(cb + 1) * 512],
                              start=(k == 0), stop=(k == 1))
                    xs = X[t][:, cb * 512:(cb + 1) * 512]
                    if m == 0:
                        sc.stt(xs, xs, ALPHA, ps[:, :], ALU.mult, ALU.add)
                    else:
                        sc.tt(xs, xs, ps[:, :], ALU.add)

        def dump(m):
            if dbg:
                for g in range(NG):
                    tmp = SF.next()
                    for k in range(2):
                        sc.cp(tmp[:, :], YB[g][:, k, :])
                        sc.dma("sp", tmp.name, [(dbg_d[:, 2 * m + k, g * 512:(g + 1) * 512], tmp[:, :])])

        def mixer_a(l):
            wt = WP.next()
            pairs = []
            wa = load_w(wt, w_in[l, :, A0:A0 + 512], 512, pairs=pairs)
            wo = load_w(wt, w_out[l, 0:256, :], 1024, k=2, off=4096, pairs=pairs)
            sc.dma("pool", wt.name, pairs)
            wsT = MID[0]
            wsr = BIG[1]
            sc.dma("sp", wsr.name, [(wsr[:, 0:512].rearrange("p (h s) -> p h s", h=4),
                                     a_ws[l].rearrange("h t s -> t h s"))])
            ps = PS.next()
            for h in range(4):
                sc.tr(ps[:, h * 128:(h + 1) * 128], wsr[:, h * 128:(h + 1) * 128], IDF)
            sc.tt(wsT[:, 0:512].rearrange("p (h t) -> p h t", h=4), ps[:, :].rearrange("p (h t) -> p h t", h=4),
                  CST[:, 128:256].unsqueeze(1).broadcast_to([128, 4, 128]), ALU.mult)
            for t in range(T):
                g, j = divmod(t, 4)
                ps = PS.next()
                for k in range(8):
                    sc.mm(ps[:, :], XT[g][:, k, j * 128:(j + 1) * 128], wa[:, k, :], start=(k == 0), stop=(k == 7))
                gl = SF.next()
                sc.act(gl[:, :], ps[:, :], AF.Gelu)
                st = SM.next()
                sc.op("dve", lambda e, st=st, gl=gl: e.bn_stats(out=st[:, 0:6], in_=gl[:, 256:512]), [gl[:]], [st[:]])
                sc.op("dve", lambda e, st=st: e.bn_aggr(out=st[:, 16:18], in_=st[:, 0:6]), [st[:]], [st[:]])
                sc.ts(st[:, 18:19], st[:, 17:18], 1e-5, ALU.add)
                sc.act(st[:, 19:20], st[:, 18:19], AF.Sqrt)
                sc.op("dve", lambda e, st=st: e.reciprocal(out=st[:, 20:21], in_=st[:, 19:20]), [st[:]], [st[:]])
                sc.ts(gl[:, 256:512], gl[:, 256:512], st[:, 16:17], ALU.subtract, st[:, 20:21], ALU.mult)
                sc.tt(gl[:, 256:512], gl[:, 256:512], PR[:, 0:256], ALU.mult)
                vn = SB.next()
                sc.tt(vn[:, 0:256], gl[:, 256:512], PR[:, 256:512], ALU.add)
                pm = PS.next()
                for h in range(4):
                    sc.mm(pm[:, h * 64:(h + 1) * 64], wsT[:, h * 128:(h + 1) * 128], vn[:, h * 64:(h + 1) * 64])
                ya = SB.next()
                for h in range(4):
                    sc.stt(ya[:, h * 64:(h + 1) * 64], pm[:, h * 64:(h + 1) * 64], PP[:, 24 + h:25 + h],
                           gl[:, h * 64:(h + 1) * 64], ALU.add, ALU.mult)
                pt = PS.next()
                ptb = pt[:, 0:128].bitcast(BF16)
                for k in range(2):
                    sc.tr(ptb[:, k * 128:(k + 1) * 128], ya[:, k * 128:(k + 1) * 128], IDB[:])
                sc.cp(YB[g][:, :, j * 128:(j + 1) * 128], ptb[:, :].rearrange("p (k c) -> p k c", k=2), eng="act")
            dump(0)
            outproj_partial(l, 0, wo)

        def rope64(dst, src, gs):
            t1 = SF.next(); t2 = SF.next()
            sc.tt(t1[64:128, :], src[64:128, :], COSC[64:128, gs], ALU.mult)
            sc.tt(t2[64:96, :], src[96:128, :], SINC[96:128, gs], ALU.mult)
            sc.tt(t2[96:128, :], src[64:96, :], SINC[64:96, gs], ALU.mult)
            sc.tt(dst, t1[64:128, :], t2[64:128, :], ALU.add)

        def mixer_b(l):
            wt = WP.next()
            pairs = []
            wcq = load_w(wt, w_in[l, :, B0:B0 + 192], 192, pairs=pairs)
            wckv = load_w(wt, w_in[l, :, B0 + 192:B0 + 320], 128, off=1536, pairs=pairs)
            wo = load_w(wt, w_out[l, 256:512, :], 1024, k=2, off=3584, pairs=pairs)
            wkv = wt[:, 6656:6656 + 512]
            pairs.append((wkv, w_ukv[l]))
            krs = load_w(wt, w_in[l, :, B0 + 320:B0 + 352], 32, off=7168, pairs=pairs)
            uqs = wt[:, 7424:7424 + 768].rearrange("p (k c) -> p k c", k=2)
            pairs.append((uqs[:, 0, :], w_uq[l, 0:128, :]))
            pairs.append((uqs[0:64, 1, :], w_uq[l, 128:192, :]))
            sc.dma("pool", wt.name, pairs)
            wkr = wt[:, 2560:2560 + 1024].rearrange("p (k c) -> p k c", k=8)
            sc.memset(wt[:, 2560:3584], 0.0)
            sc.cp(wkr[:, :, 64:96:2], krs[:, :, 0:16])
            sc.cp(wkr[:, :, 96:128:2], krs[:, :, 16:32])
            wq = wt[:, 5632:5632 + 1024].rearrange("p (k h c) -> p k h c", k=2, h=4)
            sc.memset(wt[:, 5632:6656], 0.0)
            uq4 = wt[:, 7424:7424 + 768].rearrange("p (k h c) -> p k h c", k=2, h=4)
            for kc, rows in ((0, 128), (1, 64)):
                sc.cp(wq[0:rows, kc, :, 0:64], uq4[0:rows, kc, :, 0:64])
                sc.cp(wq[0:rows, kc, :, 64:96:2], uq4[0:rows, kc, :, 64:80])
                sc.cp(wq[0:rows, kc, :, 96:128:2], uq4[0:rows, kc, :, 80:96])

            CQ0 = MID[0]; CQ1K = MID[1]; CKVN = MID[2]
            for g in range(NG):
                gs = slice(g * 512, (g + 1) * 512)
                pq0 = PS.next(); pq1 = PS.next(); pkv = PS.next(); pkr = PS.next()
                for k in range(8):
                    sc.mm(pq0[:, :], wcq[:, k, 0:128], XT[g][:, k, :], start=(k == 0), stop=(k == 7))
                for k in range(8):
                    sc.mm(pq1[0:64, :], wcq[:, k, 128:192], XT[g][:, k, :], start=(k == 0), stop=(k == 7))
                for k in range(8):
                    sc.mm(pkv[:, :], wckv[:, k, :], XT[g][:, k, :], start=(k == 0), stop=(k == 7))
                for k in range(8):
                    sc.mm(pkr[:, :], wkr[:, k, :], XT[g][:, k, :], start=(k == 0), stop=(k == 7))
                s0 = SF.next(); s1 = SF.next(); s2 = SF.next()
                sc.act(s0[:, :], pq0[:, :], AF.Square)
                sc.act(s1[0:64, :], pq1[0:64, :], AF.Square)
                sc.act(s2[:, :], pkv[:, :], AF.Square)
                pss = PS.next()
                sc.mm(pss[:, :], ONEF, s0[:, :], start=True, stop=False)
                sc.mm(pss[:, :], CST[0:64, 384:512], s1[0:64, :], start=False, stop=True)
                rq = SF.next()
                sc.ts(rq[:, :], pss[:, :], 1.0 / 192, ALU.mult, 1e-6, ALU.add)
                sc.act(rq[:, :], rq[:, :], AF.Sqrt)
                sc.op("dve", lambda e, rq=rq: e.reciprocal(out=rq[:, :], in_=rq[:, :]), [rq[:]], [rq[:]])
                sc.stt(CQ0[:, gs], pq0[:, :], PP[:, 0:1], rq[:, :], ALU.mult, ALU.mult)
                sc.stt(CQ1K[0:64, gs], pq1[0:64, :], PP[0:64, 1:2], rq[0:64, :], ALU.mult, ALU.mult)
                pss2 = PS.next()
                sc.mm(pss2[:, :], ONEF, s2[:, :])
                rk = SF.next()
                sc.ts(rk[:, :], pss2[:, :], 1.0 / 128, ALU.mult, 1e-6, ALU.add)
                sc.act(rk[:, :], rk[:, :], AF.Sqrt)
                sc.op("dve", lambda e, rk=rk: e.reciprocal(out=rk[:, :], in_=rk[:, :]), [rk[:]], [rk[:]])
                sc.stt(CKVN[:, gs], pkv[:, :], PP[:, 2:3], rk[:, :], ALU.mult, ALU.mult)
                rope64(CQ1K[64:128, gs], pkr, gs)

            scale = 96.0 ** -0.5
            for h in range(4):
                QT = MID[3]; KT = MID[4]
                va = BIG[h % 2][:, :].bitcast(BF16)[:, 0:2048].rearrange("p (t c) -> p t c", t=T)
                for g in range(NG):
                    gs = slice(g * 512, (g + 1) * 512)
                    pq = PS.next()
                    sc.mm(pq[:, :], wq[:, 0, h, :], CQ0[:, gs], start=True, stop=False)
                    sc.mm(pq[:, :], wq[0:64, 1, h, :], CQ1K[0:64, gs], start=False, stop=True)
                    sc.cp(QT[0:64, gs], pq[0:64, :], eng="act")
                    rope64(QT[64:128, gs], pq, gs)
                    pk = PS.next()
                    sc.mm(pk[0:64, :], wkv[:, h * 128:h * 128 + 64], CKVN[:, gs])
                    sc.cp(KT[0:64, gs], pk[0:64, :], eng="act")
                    sc.cp(KT[64:128, gs], CQ1K[64:128, gs], eng="dve")
                    pv = PS.next()
                    for j in range(4):
                        t = g * 4 + j
                        sc.mm(pv[:, j * 64:(j + 1) * 64], CKVN[:, t * 128:(t + 1) * 128], wkv[:, h * 128 + 64:h * 128 + 128])
                    sc.cp(va[:, g * 4:(g + 1) * 4, 0:64], pv[:, 0:256].rearrange("p (j c) -> p j c", j=4), eng="act")
                    if h < 2:
                        sc.memset(va[:, g * 4:(g + 1) * 4, 64:128], 1.0, eng="pool")
                for Qb in range(4):
                    po = PSL.next()
                    nkb = 4 * Qb + 4
                    for kb in range(nkb):
                        qs = max(Qb * 512, kb * 128)
                        qe = (Qb + 1) * 512
                        n = qe - qs
                        ps = PS.next()
                        sc.mm(ps[:, 0:n], KT[:, kb * 128:(kb + 1) * 128], QT[:, qs:qe])
                        pt = SB.next()
                        sc.act(pt[:, 0:n], ps[:, 0:n], AF.Exp, scale=scale)
                        if kb * 128 >= Qb * 512:
                            sc.tt(pt[:, 0:128], pt[:, 0:128], TRIB[:], ALU.mult, eng="pool")
                        sc.mm(po[:, qs - Qb * 512:512], va[:, kb, :], pt[:, 0:n], start=(kb == 0), stop=(kb == nkb - 1))
                    rd = SF.next()
                    sc.cp(rd[0:64, :], po[64:128, :], eng="act")
                    sc.op("dve", lambda e, rd=rd: e.reciprocal(out=rd[0:64, :], in_=rd[0:64, :]), [rd[:]], [rd[:]])
                    hp = (h % 2) * 64
                    sc.tt(YB[Qb][hp:hp + 64, h // 2, :], po[0:64, :], rd[0:64, :], ALU.mult)
            dump(1)
            outproj_partial(l, 1, wo)

        def mixer_c(l):
            wt = WP.next()
            pairs = []
            wc = load_w(wt, w_in[l, :, C0:C0 + 768], 768, pairs=pairs)
            wo = load_w(wt, w_out[l, 512:768, :], 1024, k=2, off=6144, pairs=pairs)
            sc.dma("pool", wt.name, pairs)
            QC = [MID[0], MID[1]]; KC = [MID[2], MID[3]]; VCt = MID[4]
            for g in range(NG):
                gs = slice(g * 512, (g + 1) * 512)
                for ci in range(4):
                    ps = PS.next()
                    for k in range(8):
                        sc.mm(ps[:, :], wc[:, k, ci * 128:(ci + 1) * 128], XT[g][:, k, :], start=(k == 0), stop=(k == 7))
                    dst = (QC if ci < 2 else KC)[ci % 2]
                    t1 = SF.next(); t2 = SF.next()
                    sc.tt(t1[:, :], ps[:, :], COSC[:, gs], ALU.mult)
                    for q in range(4):
                        src = (q ^ 1) * 32
                        sc.tt(t2[q * 32:(q + 1) * 32, :], ps[src:src + 32, :], SINC[src:src + 32, gs], ALU.mult)
                    sc.tt(dst[:, gs], t1[:, :], t2[:, :], ALU.add)
            vcnt = 0
            for h in range(4):
                c, hp = h // 2, (h % 2) * 64
                if h % 2 == 0:
                    for g in range(NG):
                        ps = PS.next()
                        for k in range(8):
                            sc.mm(ps[:, :], wc[:, k, (4 + c) * 128:(5 + c) * 128], XT[g][:, k, :], start=(k == 0), stop=(k == 7))
                        sc.cp(VCt[:, g * 512:(g + 1) * 512], ps[:, :], eng="act")
                ACC = BIG[1]
                first = True
                for d in (1, 4, 16):
                    nb = 16 // d
                    va = BIG[0][:, :].bitcast(BF16)[:, (vcnt % 2) * 2048:(vcnt % 2 + 1) * 2048].rearrange("p (b c) -> p b c", b=16)
                    vcnt += 1

                    def sel(r, n, d=d):
                        st = n * 128 * d + r
                        return slice(st, st + 127 * d + 1, d)
                    blocks = [(r, n) for r in range(d) for n in range(nb)]
                    for bi in range(0, 16, 4):
                        pt = PS.next()
                        ptb = pt[:, 0:128].bitcast(BF16)
                        for q in range(4):
                            r, n = blocks[bi + q]
                            sc.tr(ptb[:, q * 64:(q + 1) * 64], VCt[hp:hp + 64, sel(r, n)], IDB[hp:hp + 64, hp:hp + 64])
                        sc.cp(va[:, bi:bi + 4, 0:64], ptb[:, :].rearrange("p (q c) -> p q c", q=4), eng="act")
                    if vcnt <= 2:
                        sc.memset(va[:, :, 64:128], 1.0, eng="pool")
                    it = 0
                    if d < 16:
                        for r in range(d):
                            for n0 in range(0, nb, 2):
                                bi0 = r * nb + n0
                                ps = PS.next()
                                for q in range(2):
                                    n = n0 + q
                                    if n > 0:
                                        sc.mm(ps[:, q * 256:q * 256 + 128], KC[c][hp:hp + 64, sel(r, n - 1)], QC[c][hp:hp + 64, sel(r, n)])
                                    sc.mm(ps[:, q * 256 + 128:q * 256 + 256], KC[c][hp:hp + 64, sel(r, n)], QC[c][hp:hp + 64, sel(r, n)])
                                lo = 128 if n0 == 0 else 0
                                pt = SB.next()
                                sc.act(pt[:, lo:512], ps[:, lo:512], AF.Exp, scale=0.125)
                                sc.tt(pt[:, lo:512], pt[:, lo:512], MSK4[:, lo:512], ALU.mult, eng=("pool" if it % 2 == 0 else "dve"))
                                it += 1
                                po = PS.next()
                                for q in range(2):
                                    n = n0 + q
                                    bi = bi0 + q
                                    if n > 0:
                                        sc.mm(po[:, q * 128:(q + 1) * 128], va[:, bi - 1, :], pt[:, q * 256:q * 256 + 128], start=True, stop=False)
                                    sc.mm(po[:, q * 128:(q + 1) * 128], va[:, bi, :], pt[:, q * 256 + 128:q * 256 + 256], start=(n == 0), stop=True)
                                st0 = n0 * 128 * d + r
                                dst = ACC[:, st0:st0 + 255 * d + 1:d]
                                if first:
                                    sc.cp(dst, po[:, 0:256], eng="act")
                                else:
                                    sc.tt(dst, dst, po[:, 0:256], ALU.add)
                    else:
                        for r0 in range(0, 16, 4):
                            ps = PS.next()
                            for q in range(4):
                                sc.mm(ps[:, q * 128:(q + 1) * 128], KC[c][hp:hp + 64, sel(r0 + q, 0)], QC[c][hp:hp + 64, sel(r0 + q, 0)])
                            pt = SB.next()
                            sc.act(pt[:, 0:512], ps[:, 0:512], AF.Exp, scale=0.125)
                            sc.tt(pt[:, 0:512], pt[:, 0:512], MSKT4[:, 0:512], ALU.mult, eng=("pool" if it % 2 == 0 else "dve"))
                            it += 1
                            po = PS.next()
                            for q in range(4):
                                sc.mm(po[:, q * 128:(q + 1) * 128], va[:, r0 + q, :], pt[:, q * 128:(q + 1) * 128])
                            dst = ACC[:, :].rearrange("p (i r) -> p r i", r=16)[:, r0:r0 + 4, :]
                            sc.tt(dst, dst, po[:, 0:512].rearrange("p (q i) -> p q i", q=4), ALU.add)
                    first = False
                for g in range(NG):
                    gs = slice(g * 512, (g + 1) * 512)
                    rd = SF.next()
                    sc.cp(rd[0:64, :], ACC[64:128, gs], eng="act")
                    sc.op("dve", lambda e, rd=rd: e.reciprocal(out=rd[0:64, :], in_=rd[0:64, :]), [rd[:]], [rd[:]])
                    sc.tt(YB[g][hp:hp + 64, c, :], ACC[0:64, gs], rd[0:64, :], ALU.mult)
            dump(2)
            outproj_partial(l, 2, wo)

        def mixer_d(l):
            wt = WP.next()
            pairs = []
            wqk = load_w(wt, w_in[l, :, D0:D0 + 512], 512, pairs=pairs)
            wvo = load_w(wt, w_in[l, :, D0 + 512:D0 + 1024], 512, off=4096, pairs=pairs)
            sc.dma("pool", wt.name, pairs)
            wg8 = load_w(WG8, w_in[l, :, D0 + 1024:D0 + 1032], 8)
            wo = load_w(WOD, w_out[l, 768:1024, :], 1024, k=2)
            pg = PSL.next()
            for t in range(T):
                g, j = divmod(t, 4)
                for k in range(8):
                    sc.mm(pg[:, t * 8:(t + 1) * 8], XT[g][:, k, j * 128:(j + 1) * 128], wg8[:, k, :], start=(k == 0), stop=(k == 7))

            def tb(i):
                return GB[:, i * 64:(i + 1) * 64]

            def tb3(i):
                return GB[:, i * 64:(i + 1) * 64].rearrange("p (c h) -> p c h", h=4)
            pg3 = pg[:, 0:128].rearrange("p (c e) -> p c e", e=8)
            GI, GF, AB, EX, LN_, FC, BI_, BB, AA, WK, THR, DEC, TOT = range(13)
            sc.tt(tb3(GI), pg3[:, :, 0:4], PR[:, 512:516].unsqueeze(1).broadcast_to([128, 16, 4]), ALU.add)
            sc.tt(tb3(GF), pg3[:, :, 4:8], PR[:, 516:520].unsqueeze(1).broadcast_to([128, 16, 4]), ALU.add)
            sc.act(tb(AB), tb(GF), AF.Abs)
            sc.act(tb(EX), tb(AB), AF.Exp, scale=-1.0)
            sc.act(tb(LN_), tb(EX), AF.Ln, bias=1.0)
            sc.stt(tb(FC), tb(GF), 0.0, tb(LN_), ALU.min, ALU.subtract)
            if DSTOP == 1:
                return
            pc = PS.next()
            sc.mm(pc[:, 0:64], TRIF, tb(FC))
            sc.mm(pc[:, 64:128], ONEF, tb(FC))
            sc.cp(tb(TOT), pc[:, 64:128])
            for h in range(4):
                v_in = GB[:, TOT * 64 + h:TOT * 64 + 64:4]
                v_out = GB[:, BI_ * 64 + h:BI_ * 64 + 64:4]
                sc.op("dve", lambda e, v_in=v_in, v_out=v_out: e.tensor_tensor_scan(
                    out=v_out, data0=CST[:, 384:400], data1=v_in, initial=0.0, op0=ALU.mult, op1=ALU.add), [GB[:], CST[:]], [GB[:]])
            sc.tt(tb(BI_), tb(BI_), tb(TOT), ALU.subtract)
            sc.tt(tb(BB), pc[:, 0:64], tb(BI_), ALU.add)
            sc.tt(tb(AA), tb(GI), tb(BB), ALU.subtract)
            if DSTOP == 2:
                return
            pa = PS.next()
            sc.tr(pa[0:64, 0:128], tb(AA), IDF)
            cm = SM.next()
            sc.op("dve", lambda e: e.tensor_reduce(out=cm[0:64, 0:1], in_=pa[0:64, 0:128], axis=mybir.AxisListType.X, op=ALU.max),
                  [pa[:]], [cm[:]])
            pb = PS.next()
            sc.tr(pb[0:1, 0:64], cm[0:64, 0:1], CST[0:64, 0:64])
            R = SF.next()
            sc.cp(R[0:1, 128:192], pb[0:1, 0:64])
            for h in range(4):
                sc.op("dve", lambda e, h=h: e.tensor_tensor_scan(
                    out=R[0:1, h:64:4], data0=R[0:1, 128 + h:192:4], data1=CST[0:1, 260:276], initial=0.0,
                    op0=ALU.max, op1=ALU.max), [R[:], CST[:]], [R[:]])
            sc.memset(R[0:1, 64:68], 0.0)
            sc.cp(R[0:1, 68:128], R[0:1, 0:60])
            pm = PS.next()
            sc.mm(pm[:, 0:128], CST[0:1, 384:512], R[0:1, 0:128])
            MR = SF.next()
            sc.cp(MR[:, 0:128], pm[:, 0:128])
            sc.tt(tb(WK), tb(AA), MR[:, 0:64], ALU.subtract)
            sc.act(tb(WK), tb(WK), AF.Exp)
            sc.ts(tb(WK), tb(WK), 0.125, ALU.mult)
            sc.tt(tb(THR), tb(BB), MR[:, 0:64], ALU.add)
            sc.act(tb(THR), tb(THR), AF.Exp, scale=-1.0)
            sc.tt(tb(DEC), MR[:, 64:128], MR[:, 0:64], ALU.subtract)
            sc.act(tb(DEC), tb(DEC), AF.Exp)
            if DSTOP == 3:
                return
            QK = [MID[0], MID[1], MID[2], MID[3]]
            for ci in range(4):
                Z = BIG[0]
                for g in range(NG):
                    ps = PS.next()
                    for k in range(8):
                        sc.mm(ps[:, :], wqk[:, k, ci * 128:(ci + 1) * 128], XT[g][:, k, :], start=(k == 0), stop=(k == 7))
                    sc.cp(Z[:, g * 512:(g + 1) * 512], ps[:, :], eng="act")
                A = BIG[1]
                sc.ts(A[:, :], Z[:, :], PP[:, 3 + ci * 4 + 3:4 + ci * 4 + 3], ALU.mult, PP[:, 19 + ci:20 + ci], ALU.add)
                for j in range(3):
                    sh = 3 - j
                    sc.stt(A[:, sh:S], Z[:, 0:S - sh], PP[:, 3 + ci * 4 + j:4 + ci * 4 + j], A[:, sh:S], ALU.mult, ALU.add)
                sc.act(QK[ci][:, :], A[:, :], AF.Silu)
            if DSTOP == 4:
                return
            CS = SF.next()
            cs3 = CS[:, 0:256].rearrange("p (a c) -> p a c", a=2)
            sc.memset(CS[:, 0:256], 0.0)
            SFd = [BIG[2][:, i * 512:(i + 1) * 512] for i in range(4)]
            for c in range(T):
                g, j = divmod(c, 4)
                cs = slice(c * 128, (c + 1) * 128)
                pvo = PS.next()
                for k in range(8):
                    sc.mm(pvo[:, :], XT[g][:, k, j * 128:(j + 1) * 128], wvo[:, k, :], start=(k == 0), stop=(k == 7))
                vaug = SB.next()
                va3 = vaug[:, 0:512].rearrange("p (h c) -> p h c", h=4)
                sc.cp(va3[:, :, 0:64], pvo[:, 0:256].rearrange("p (h c) -> p h c", h=4), eng="act")
                sc.memset(va3[:, :, 64:128], 1.0, eng="pool")
                so = MID[4][:, (c % 2) * 256:(c % 2) * 256 + 256]
                sc.act(so, pvo[:, 256:512], AF.Sigmoid)
                pk = PS.next()
                pkb = pk[:, 0:128].bitcast(BF16)
                for i in range(2):
                    sc.tr(pkb[:, i * 128:(i + 1) * 128], QK[2 + i][:, cs], IDB[:])
                kw = SB.next()
                sc.tt(kw[:, 0:256].rearrange("p (h c) -> p h c", h=4), pkb[:, :].rearrange("p (h c) -> p h c", h=4),
                      GB[:, WK * 64 + c * 4:WK * 64 + c * 4 + 4].unsqueeze(2).broadcast_to([128, 4, 64]), ALU.mult)
                pS2 = [PS.next(), PS.next()]
                for h in range(4):
                    hp = (h % 2) * 64
                    sc.mm(pS2[h % 2][:, (h // 2) * 128:(h // 2 + 1) * 128], QK[2 + h // 2][hp:hp + 64, cs], QK[h // 2][hp:hp + 64, cs])
                sq = SB.next()
                for h in range(4):
                    sc.stt(sq[:, h * 128:(h + 1) * 128], pS2[h % 2][:, (h // 2) * 128:(h // 2 + 1) * 128],
                           GB[:, WK * 64 + c * 4 + h:WK * 64 + c * 4 + h + 1], TRIB[:], ALU.mult, ALU.mult)
                for hf in range(2):
                    rows = slice(hf * 64, hf * 64 + 64)
                    dec = GB[rows, DEC * 64 + c * 4 + hf:DEC * 64 + c * 4 + 4:2]
                    sc.tt(cs3[rows, :, :], cs3[rows, :, :], dec.unsqueeze(2).broadcast_to([64, 2, 128]), ALU.mult)
                cb = SB.next()
                cb3 = cb[:, 0:256].rearrange("p (a c) -> p a c", a=2)
                sc.cp(cb[:, 0:256], CS[:, 0:256], eng="act")
                ph2 = [PS.next(), PS.next()]
                for h in range(4):
                    hp = (h % 2) * 64
                    po_ = ph2[h % 2][:, (h // 2) * 128:(h // 2 + 1) * 128]
                    sc.mm(po_, sq[:, h * 128:(h + 1) * 128], va3[:, h, :], start=True, stop=False)
                    sc.mm(po_, QK[h // 2][hp:hp + 64, cs], cb3[hp:hp + 64, h // 2, :], start=False, stop=True)
                pu = PS.next()
                for a in range(2):
                    sc.mm(pu[:, a * 256:(a + 1) * 256], kw[:, a * 128:(a + 1) * 128], vaug[:, a * 256:(a + 1) * 256])
                pu3 = pu[:, 0:512].rearrange("p (a c) -> p a c", a=2)
                sc.tt(cs3[0:64, :, :], cs3[0:64, :, :], pu3[0:64, :, 0:128], ALU.add)
                sc.tt(cs3[64:128, :, :], cs3[64:128, :, :], pu3[64:128, :, 128:256], ALU.add)
                dn = SM.next()
                for par in range(2):
                    p3 = ph2[par][:, 0:256].rearrange("p (a c) -> p a c", a=2)
                    sc.act(dn[:, par:4:2], p3[:, :, 64], AF.Abs)
                sc.tt(dn[:, 0:4], dn[:, 0:4], GB[:, THR * 64 + c * 4:THR * 64 + c * 4 + 4], ALU.max)
                sc.op("dve", lambda e, dn=dn: e.reciprocal(out=dn[:, 4:8], in_=dn[:, 0:4]), [dn[:]], [dn[:]])
                hy = SFd[c % 4]
                hy3 = hy[:, 0:256].rearrange("p (h c) -> p h c", h=4)
                for par in range(2):
                    p3 = ph2[par][:, 0:256].rearrange("p (a c) -> p a c", a=2)
                    sc.tt(hy3[:, par:4:2, :], p3[:, :, 0:64],
                          dn[:, 4 + par:8:2].unsqueeze(2).broadcast_to([128, 2, 64]), ALU.mult)
                yd = SB.next()
                sc.tt(yd[:, 0:256], hy[:, 0:256], so, ALU.mult)
                pt = PS.next()
                ptb = pt[:, 0:128].bitcast(BF16)
                for k in range(2):
                    sc.tr(ptb[:, k * 128:(k + 1) * 128], yd[:, k * 128:(k + 1) * 128], IDB[:])
                sc.cp(YB[g][:, :, j * 128:(j + 1) * 128], ptb[:, :].rearrange("p (k c) -> p k c", k=2), eng="act")
            dump(3)
            outproj_partial(l, 3, wo)

        def ffn(l):
            chunks = [(i * 512, 512) for i in range(5)] + [(2560, 256)]
            for ci, (c0, cw) in enumerate(chunks):
                nhb = cw // 128
                wt = WP.next()
                pairs = []
                wg = load_w(wt, w_gate[l, :, c0:c0 + cw], cw, pairs=pairs)
                wu = load_w(wt, w_up[l, :, c0:c0 + cw], cw, off=4096, pairs=pairs)
                sc.dma("pool", wt.name, pairs)
                wt2 = BIG[1 + ci % 2][:, :].bitcast(BF16)
                vd = wt2[:, 0:nhb * 1024].rearrange("p (k c) -> p k c", k=nhb)
                sc.dma("pool", f"BIG{1 + ci % 2}", [(vd, w_down[l, c0:c0 + cw, :].rearrange("(k p) c -> p k c", p=128))])
                wd = vd
                for g in range(NG):
                    hT = MID[(ci * NG + g) % 2]
                    for hb in range(nhb):
                        pg_ = PS.next(); pu_ = PS.next()
                        for k in range(8):
                            sc.mm(pg_[:, :], wg[:, k, hb * 128:(hb + 1) * 128], XT[g][:, k, :], start=(k == 0), stop=(k == 7))
                        for k in range(8):
                            sc.mm(pu_[:, :], wu[:, k, hb * 128:(hb + 1) * 128], XT[g][:, k, :], start=(k == 0), stop=(k == 7))
                        sl = SF.next()
                        sc.act(sl[:, :], pg_[:, :], AF.Silu)
                        sc.tt(hT[:, hb * 512:(hb + 1) * 512], sl[:, :], pu_[:, :], ALU.mult)
                    for j in range(4):
                        t = g * 4 + j
                        for cb in range(2):
                            ps = PS.next()
                            for hb in range(nhb):
                                sc.mm(ps[:, :], hT[:, hb * 512 + j * 128:hb * 512 + (j + 1) * 128], wd[:, hb, cb * 512:(cb + 1) * 512],
                                      start=(hb == 0), stop=(hb == nhb - 1))
                            xs = X[t][:, cb * 512:(cb + 1) * 512]
                            if ci == 0:
                                sc.stt(xs, xs, ALPHA, ps[:, :], ALU.mult, ALU.add)
                            else:
                                sc.tt(xs, xs, ps[:, :], ALU.add)

        def rope_tables(s):
            PI = XTt[0][:, :].bitcast(F32)
            PIi = XTt[0][:, :].bitcast(I32)
            PF = XTt[1][:, :].bitcast(F32)
            RR = XTt[2][:, :].bitcast(F32)
            KK = XTt[3][:, :].bitcast(F32)
            KKi = XTt[3][:, :].bitcast(I32)
            sc.dma("sp", "XT0", [(PIi, pos_d[s:s + 1, :].broadcast_to([128, S]))])
            sc.cp(PF, PIi)
            two_pi = 2.0 * math.pi
            c1 = 6.28125
            c2 = float(np.float32(two_pi - c1))
            c3 = float(two_pi - c1 - c2)
            sc.ts(RR, PF, CST[:, 513:514], ALU.mult)
            for shift, dst, sgn in ((0.0, SINC, True), (math.pi / 2, COSC, False)):
                sc.ts(KKi, RR, shift, ALU.add, 1.0 / two_pi, ALU.mult)
                sc.cp(PI, KKi)
                sc.stt(KK, PI, -c1, RR, ALU.mult, ALU.add)
                sc.stt(KK, PI, -c2, KK, ALU.mult, ALU.add)
                sc.stt(KK, PI, -c3, KK, ALU.mult, ALU.add)
                sc.ts(KK, KK, shift, ALU.add)
                sc.ts(KK, KK, math.pi, ALU.min, -math.pi, ALU.max)
                if sgn:
                    sc.act(KK, KK, AF.Sin)
                    sc.ts(dst[:, :], KK, CST[:, 514:515], ALU.mult)
                else:
                    sc.act(dst[:, :], KK, AF.Sin)

        for s in range(NS):
            rope_tables(s)
            for q in range(4):
                sc.dma("sp", f"Xld{q}", [(X[q * 4 + i][:, :], x_d[s, (q * 4 + i) * 128:(q * 4 + i + 1) * 128, :]) for i in range(4)])
            for t in range(T):
                make_xt(t)
            for l in range(NL):
                sc.dma("sp", "PP", [(PP[:, :], pp_d[l])])
                sc.dma("sp", "PR", [(PR[:, :], pr_d[l:l + 1, :].broadcast_to([128, NPR]))])
                acc_m = [0]
                for nm, fn in (("a", mixer_a), ("b", mixer_b), ("c", mixer_c), ("d", mixer_d)):
                    if dbg is None or nm in dbg:
                        fn(l)
                load_lngb(l, 0)
                for t in range(T):
                    layer_norm(t, 0)
                    make_xt(t)
                load_lngb(l, 1)
                if dbg is None or "f" in dbg:
                    ffn(l)
                for t in range(T):
                    layer_norm(t, 1)
                    if l < NL - 1:
                        make_xt(t)
            for q in range(4):
                sc.dma("sp", f"Xld{q}", [(out_d[s, (q * 4 + i) * 128:(q * 4 + i + 1) * 128, :], X[q * 4 + i][:, :]) for i in range(4)])
        sc.op("sp", lambda e: e.nop(), [X[t][:] for t in range(T)], [X[t][:] for t in range(T)])
        nops = sc.emit()
    return nc, nops


def make_consts():
    c = np.zeros((128, NCONST), np.float32)
    p = np.arange(128)
    c[:, 0:128] = np.eye(128, dtype=np.float32)
    c[:, 128:256] = (p[:, None] <= p[None, :]).astype(np.float32)
    c[:, 256:384] = (p[:, None] >= p[None, :]).astype(np.float32)
    c[:, 384:512] = 1.0
    th = np.float32(10000.0)
    c[:, 512] = np.power(th, -(np.arange(16, dtype=np.float32) / np.float32(16)))[p % 16]
    c[:, 513] = np.power(th, -(np.arange(32, dtype=np.float32) / np.float32(32)))[p % 32]
    c[:, 514] = np.where((p % 64) < 32, 1.0, -1.0)
    return c


def prep_inputs(inp, NL=4):
    f = lambda a: np.ascontiguousarray(np.asarray(a, dtype=np.float32))
    lnp = np.stack([f(inp["ln1_g"]), f(inp["ln1_b"]), f(inp["ln2_g"]), f(inp["ln2_b"])], axis=1)[:NL]
    pp = np.zeros((NL, 128, NPP), np.float32)
    pr = np.zeros((NL, NPR), np.float32)
    for l in range(NL):
        qn = f(inp["b_q_norm"])[l]
        pp[l, :, 0] = qn[0:128]
        pp[l, 0:64, 1] = qn[128:192]
        pp[l, :, 2] = f(inp["b_kv_norm"])[l]
        cw = f(inp["d_conv_w"])[l]
        for ci in range(4):
            for j in range(4):
                pp[l, :, 3 + ci * 4 + j] = cw[j, ci * 128:(ci + 1) * 128]
            pp[l, :, 19 + ci] = f(inp["d_conv_b"])[l, ci * 128:(ci + 1) * 128]
        pp[l, :, 24:28] = f(inp["a_bs"])[l].T
        pr[l, 0:256] = f(inp["a_ln_g"])[l]
        pr[l, 256:512] = f(inp["a_ln_b"])[l]
        pr[l, 512:516] = f(inp["d_igate_b"])[l]
        pr[l, 516:520] = f(inp["d_fgate_b"])[l]
    shared = dict(w_in=f(inp["w_in"])[:NL], a_ws=f(inp["a_ws"])[:NL], b_w_uq=f(inp["b_w_uq"])[:NL],
                  b_w_ukv=f(inp["b_w_ukv"])[:NL], w_out=f(inp["w_out"])[:NL], w_gate=f(inp["w_gate"])[:NL],
                  w_up=f(inp["w_up"])[:NL], w_down=f(inp["w_down"])[:NL], lnp=np.ascontiguousarray(lnp),
                  pp=pp, pr=pr, cst=make_consts())
    return shared


_CACHE = {}


def kernel(**inputs):
    n = 8
    x = np.asarray(inputs["x"], dtype=np.float32)
    pos = np.asarray(inputs["positions"], dtype=np.int32)
    shared = prep_inputs(inputs)
    if "nc" not in _CACHE:
        _CACHE["nc"] = build(4, 4)[0]
    nc = _CACHE["nc"]
    in_maps = []
    for c in range(n):
        m = dict(shared)
        m["x"] = np.ascontiguousarray(x[c * 4:(c + 1) * 4])
        m["pos"] = np.ascontiguousarray(pos[c * 4:(c + 1) * 4])
        in_maps.append(m)
    res = run_bass_kernel_spmd(nc, in_maps, core_ids=list(range(n)))
    return np.concatenate([r["out"] for r in res.results], axis=0)
```

```python
import math
import os
import numpy as np
from contextlib import ExitStack
import concourse.bass as bass
import concourse.mybir as mybir
from concourse.bass_utils import run_bass_kernel_spmd

F32 = mybir.dt.float32
BF16 = mybir.dt.bfloat16
I32 = mybir.dt.int32
ALU = mybir.AluOpType
AF = mybir.ActivationFunctionType

D = 1024
S = 2048
T = 16
NG = 4
DFF = 2816
ALPHA = 8.0 ** 0.25
A0, B0, C0, D0 = 0, 512, 864, 1632
NCONST = 520
NPP = 28
NPR = 520
DSTOP = int(os.environ.get('DSTOP', '0'))
REORDER = int(os.environ.get('KREORDER', '600'))
SAME_GAP = int(os.environ.get('KSAMEGAP', '1000000000'))


class Sched:
    def __init__(self, nc, es):
        self.nc = nc
        self.es = es
        self.E = {"pe": nc.tensor, "act": nc.scalar, "dve": nc.vector, "pool": nc.gpsimd, "sp": nc.sync}
        self.ops = []
        self.tiles = {}
        self.chan_ops = {}

    def tile(self, name, shape, dt, psum=False):
        ctx = (self.nc.psum_tensor if psum else self.nc.sbuf_tensor)(name, list(shape), dt)
        t = self.es.enter_context(ctx)
        self.tiles[name] = [None, {}]
        return t

    def _names(self, aps):
        r = []
        for a in aps:
            if a is None or isinstance(a, (int, float)):
                continue
            n = a.tensor.name
            if n in self.tiles and n not in r:
                r.append(n)
        return r

    def op(self, eng, fn, reads, writes, dma=None, ndma=1, cost=200.0, tail=0.0):
        rn = self._names(reads)
        wn = self._names(writes)
        deps = set()
        for n in rn:
            st = self.tiles[n]
            if st[0] is not None:
                deps.add(st[0])
        for n in wn:
            st = self.tiles[n]
            if st[0] is not None:
                deps.add(st[0])
            deps.update(st[1].values())
        oid = len(self.ops)
        chan = ("dma:" + dma) if dma else eng
        self.ops.append(dict(eng=eng, fn=fn, deps=deps, chan=chan, ndma=ndma, isdma=dma is not None, cost=cost, tail=tail))
        self.chan_ops.setdefault(chan, []).append(oid)
        for n in rn:
            if n not in wn:
                self.tiles[n][1][chan] = oid
        for n in wn:
            self.tiles[n][0] = oid
            self.tiles[n][1] = {}
        return oid

    def reorder(self, window=600):
        import heapq
        ops = self.ops
        n = len(ops)
        succ = [[] for _ in range(n)]
        indeg = [0] * n
        for i, o in enumerate(ops):
            for d in o["deps"]:
                succ[d].append(i)
            indeg[i] = len(o["deps"])
        blevel = [0.0] * n
        for i in range(n - 1, -1, -1):
            o = ops[i]
            b = 0.0
            for j in succ[i]:
                v = blevel[j] + (60.0 if ops[j]["eng"] == o["eng"] else 350.0)
                if v > b:
                    b = v
            blevel[i] = b + o["cost"] + o["tail"]
        fin = [0.0] * n
        ready_t = [0.0] * n
        engs = list(self.E)
        free = {e: 0.0 for e in engs}
        fut = {e: [] for e in engs}
        av = {e: [] for e in engs}
        deferred = []
        done = [False] * n
        base = 0
        order = []

        def admit(i):
            if i < base + window:
                heapq.heappush(fut[ops[i]["eng"]], (ready_t[i], i))
            else:
                heapq.heappush(deferred, i)
        for i in range(n):
            if indeg[i] == 0:
                admit(i)
        nsched = 0
        while nsched < n:
            best = None
            for e in engs:
                f, a = fut[e], av[e]
                while f and f[0][0] <= free[e]:
                    rt, i = heapq.heappop(f)
                    heapq.heappush(a, i)
                if a:
                    cand = (free[e], a[0], e, True)
                elif f:
                    cand = (f[0][0], f[0][1], e, False)
                else:
                    continue
                if best is None or cand[:2] < best[:2]:
                    best = cand
            if best is None:
                i = heapq.heappop(deferred)
                heapq.heappush(fut[ops[i]["eng"]], (ready_t[i], i))
                continue
            st, i, e, from_av = best
            if from_av:
                heapq.heappop(av[e])
            else:
                heapq.heappop(fut[e])
            o = ops[i]
            free[e] = st + o["cost"]
            fin[i] = st + o["cost"] + o["tail"]
            done[i] = True
            order.append(i)
            nsched += 1
            while base < n and done[base]:
                base += 1
            while deferred and deferred[0] < base + window:
                j = heapq.heappop(deferred)
                heapq.heappush(fut[ops[j]["eng"]], (ready_t[j], j))
            for j in succ[i]:
                lat = 60.0 if ops[j]["eng"] == e else 350.0
                if fin[i] + lat > ready_t[j]:
                    ready_t[j] = fin[i] + lat
                indeg[j] -= 1
                if indeg[j] == 0:
                    admit(j)
        remap = {old: new for new, old in enumerate(order)}
        new_ops = []
        for old in order:
            o = ops[old]
            o["deps"] = {remap[d] for d in o["deps"]}
            new_ops.append(o)
        self.ops = new_ops
        self.chan_ops = {}
        for i, o in enumerate(new_ops):
            self.chan_ops.setdefault(o["chan"], []).append(i)
        self.sim_ns = max(fin) if fin else 0.0

    def emit(self):
        if REORDER:
            self.reorder(REORDER)
        ops = self.ops
        pos = {}
        for c, l in self.chan_ops.items():
            for i, o in enumerate(l):
                pos[o] = (c, i)
        seen = {e: {} for e in self.E}
        sig = set()
        for oid, o in enumerate(ops):
            need = {}
            e = o["eng"]
            for d in o["deps"]:
                c, i = pos[d]
                if c == "pe" and e == "pe":
                    continue
                if c == e and e in ("dve", "act") and pos[oid][1] - i - 1 >= SAME_GAP:
                    continue
                if i > seen[e].get(c, -1) and i > need.get(c, -1):
                    need[c] = i
            o["waits"] = need
            for c, i in need.items():
                seen[e][c] = i
                sig.add(self.chan_ops[c][i])
        sems = {}
        for c in self.chan_ops:
            sems[c] = self.es.enter_context(self.nc.semaphore("s_" + c.replace(":", "_")))
        cnt = {c: 0 for c in self.chan_ops}
        sigval = {}
        for oid, o in enumerate(ops):
            c = o["chan"]
            if o["isdma"]:
                cnt[c] += 16 * o["ndma"]
                sigval[oid] = cnt[c]
            elif oid in sig:
                cnt[c] += 1
                sigval[oid] = cnt[c]
        for oid, o in enumerate(ops):
            e = self.E[o["eng"]]
            for c, i in o["waits"].items():
                e.wait_ge(sems[c], sigval[self.chan_ops[c][i]])
            ins = o["fn"](e)
            if o["isdma"]:
                if not isinstance(ins, (list, tuple)):
                    ins = [ins]
                assert len(ins) == o["ndma"]
                for x in ins:
                    x.then_inc(sems[o["chan"]], 16)
            elif oid in sig:
                ins.then_inc(sems[o["chan"]], 1)
        return len(ops)

    @staticmethod
    def _fs(ap):
        n = 1
        for d in list(ap.shape)[1:]:
            n *= int(d)
        return n

    def mm(self, out, lhsT, rhs, start=True, stop=True):
        c = max(64, self._fs(rhs)) / 2.2 * (4.0 if rhs.dtype == F32 else 1.0) + 8.0
        self.op("pe", lambda e: e.matmul(out, lhsT, rhs, start=start, stop=stop), [lhsT, rhs], [out], cost=c, tail=150.0)

    def tr(self, out, in_, ident):
        c = max(64, self._fs(ident)) / 2.2 + 8.0
        self.op("pe", lambda e: e.transpose(out, in_, ident), [in_, ident], [out], cost=c, tail=150.0)

    def act(self, out, in_, func, bias=None, scale=None, eng="act"):
        kw = {}
        if bias is not None:
            kw["bias"] = bias
        if scale is not None:
            kw["scale"] = scale
        self.op(eng, lambda e: e.activation(out, in_, func, **kw), [in_, bias, scale], [out], cost=200.0 + self._fs(out) / 1.2)

    def tt(self, out, in0, in1, op, eng="dve"):
        self.op(eng, lambda e: e.tensor_tensor(out=out, in0=in0, in1=in1, op=op), [in0, in1], [out],
                cost=(70.0 + self._fs(out) / 0.96) if eng == "dve" else (200.0 + self._fs(out) / 0.5))

    def ts(self, out, in0, s1, op0, s2=None, op1=None, eng="dve"):
        if op1 is None:
            self.op(eng, lambda e: e.tensor_scalar(out=out, in0=in0, scalar1=s1, scalar2=None, op0=op0),
                    [in0, s1], [out], cost=70.0 + self._fs(out) / 0.96)
        else:
            self.op(eng, lambda e: e.tensor_scalar(out=out, in0=in0, scalar1=s1, scalar2=s2, op0=op0, op1=op1),
                    [in0, s1, s2], [out], cost=70.0 + self._fs(out) / 0.96)

    def stt(self, out, in0, scalar, in1, op0, op1):
        self.op("dve", lambda e: e.scalar_tensor_tensor(out=out, in0=in0, scalar=scalar, in1=in1, op0=op0, op1=op1),
                [in0, scalar, in1], [out], cost=70.0 + self._fs(out) / 0.96)

    def cp(self, out, in_, eng="dve"):
        if eng == "act":
            self.op("act", lambda e: e.copy(out, in_), [in_], [out], cost=200.0 + self._fs(out) / 1.2)
        else:
            self.op(eng, lambda e: e.tensor_copy(out=out, in_=in_), [in_], [out],
                    cost=(70.0 + self._fs(out) / 0.96) if eng == "dve" else (200.0 + self._fs(out) / 0.6))

    def memset(self, ap, v, eng="dve"):
        self.op(eng, lambda e: e.memset(ap, v), [], [ap], cost=(70.0 + self._fs(ap) / 0.96) if eng == "dve" else (200.0 + self._fs(ap) / 0.6))

    def dma(self, eng, chan, pairs, reads=(), writes=()):
        pairs = list(pairs)
        self.op(eng, lambda e: [e.dma_start(out=o, in_=i) for (o, i) in pairs],
                list(reads) + [i for (_, i) in pairs], list(writes) + [o for (o, _) in pairs],
                dma=chan, ndma=len(pairs), cost=(1200.0 if eng == "pool" else 150.0) * len(pairs),
                tail=2500.0 + sum(self._fs(o) * 128 * 4 for (o, _) in pairs) / 150.0)


class Pool:
    def __init__(self, sch, name, shape, dt, n, psum=False):
        self.t = [sch.tile(f"{name}{i}", shape, dt, psum) for i in range(n)]
        self.i = 0

    def next(self):
        t = self.t[self.i % len(self.t)]
        self.i += 1
        return t


def build(NL=4, NS=4, dbg=None):
    nc = bass.Bass("TRN2", target_bir_lowering=False, dynamic_dma_scratch_size=4096)

    def din(name, shape, dt=F32):
        return nc.dram_tensor(name, list(shape), dt, kind="ExternalInput").ap()

    x_d = din("x", [NS, S, D])
    pos_d = din("pos", [NS, S], I32)
    w_in = din("w_in", [NL, D, 2664])
    a_ws = din("a_ws", [NL, 4, 128, 128])
    w_uq = din("b_w_uq", [NL, 192, 384])
    w_ukv = din("b_w_ukv", [NL, 128, 512])
    w_out = din("w_out", [NL, D, D])
    w_gate = din("w_gate", [NL, D, DFF])
    w_up = din("w_up", [NL, D, DFF])
    w_down = din("w_down", [NL, DFF, D])
    lnp = din("lnp", [NL, 4, D])
    pp_d = din("pp", [NL, 128, NPP])
    pr_d = din("pr", [NL, NPR])
    cst_d = din("cst", [128, NCONST])
    out_d = nc.dram_tensor("out", [NS, S, D], F32, kind="ExternalOutput").ap()
    dbg_d = None
    if dbg:
        dbg_d = nc.dram_tensor("dbg", [128, 8, S], F32, kind="ExternalOutput").ap()

    with ExitStack() as es:
        sc = Sched(nc, es)
        X = [sc.tile(f"X{t}", [128, D], F32) for t in range(T)]
        XTt = [sc.tile(f"XT{g}", [128, 4096], BF16) for g in range(NG)]
        XT = [t[:, :].rearrange("p (k c) -> p k c", k=8) for t in XTt]
        YB = [sc.tile(f"YB{g}", [128, 2, 512], BF16) for g in range(NG)]
        CST = sc.tile("CST", [128, NCONST], F32)
        IDB = sc.tile("IDB", [128, 128], BF16)
        TRIB = sc.tile("TRIB", [128, 128], BF16)
        LOWB = sc.tile("LOWB", [128, 128], BF16)
        MSK2 = sc.tile("MSK2", [128, 2, 128], BF16)
        ONEB = sc.tile("ONEB", [128, 128], BF16)
        MSK4 = sc.tile("MSK4", [128, 512], BF16)
        MSKT4 = sc.tile("MSKT4", [128, 512], BF16)
        IDF = CST[:, 0:128]
        TRIF = CST[:, 128:256]
        ONEF = CST[:, 384:512]
        COSC = sc.tile("COSC", [128, S], BF16)
        SINC = sc.tile("SINC", [128, S], BF16)
        PP = sc.tile("PP", [128, NPP], F32)
        PR = sc.tile("PR", [128, NPR], F32)
        WP = Pool(sc, "WP", [128, 8192], BF16, 2)
        PS = Pool(sc, "PS", [128, 512], F32, 6, psum=True)
        PSL = Pool(sc, "PSL", [128, 512], F32, 2, psum=True)
        SF = Pool(sc, "SF", [128, 512], F32, 4)
        SB = Pool(sc, "SB", [128, 512], BF16, 6)
        SM = Pool(sc, "SM", [128, 64], F32, 8)
        BIG = [sc.tile(f"BIG{i}", [128, 2048], F32) for i in range(3)]
        GB = sc.tile("GB", [128, 1024], F32)
        MID = [sc.tile(f"MID{i}", [128, 2048], BF16) for i in range(5)]
        LNGB = BIG[0][:, :].rearrange("p (a d) -> p a d", a=2)
        WOD = sc.tile("WOD", [128, 2048], BF16)
        WG8 = sc.tile("WG8", [128, 64], BF16)

        sc.dma("sp", "CST", [(CST[:], cst_d[:, :])])
        sc.cp(IDB[:], CST[:, 0:128])
        sc.cp(TRIB[:], CST[:, 128:256])
        sc.cp(LOWB[:], CST[:, 256:384])
        sc.cp(ONEB[:], CST[:, 384:512])
        sc.cp(MSK2[:, 0, :], CST[:, 256:384])
        sc.cp(MSK2[:, 1, :], CST[:, 128:256])
        for q in range(4):
            sc.cp(MSK4[:, q * 128:(q + 1) * 128], CST[:, 256:384] if q % 2 == 0 else CST[:, 128:256])
            sc.cp(MSKT4[:, q * 128:(q + 1) * 128], CST[:, 128:256])

        def wview(wt, n, k=8):
            return wt[:, 0:k * n].rearrange("p (k c) -> p k c", k=k)

        def load_w(wt, src2d, n, k=8, off=0, pairs=None):
            v = wt[:, off:off + k * n].rearrange("p (k c) -> p k c", k=k)
            pr_ = (v, src2d.rearrange("(k p) c -> p k c", p=128))
            if pairs is None:
                sc.dma("pool", wt.name, [pr_])
            else:
                pairs.append(pr_)
            return v

        def make_xt(t):
            g, j = divmod(t, 4)
            for half in range(2):
                ps = PS.next()
                for kk in range(4):
                    k = half * 4 + kk
                    sc.tr(ps[:, kk * 128:(kk + 1) * 128], X[t][:, k * 128:(k + 1) * 128], IDF)
                dst = XT[g][:, half * 4:(half + 1) * 4, j * 128:(j + 1) * 128]
                src = ps[:, :].rearrange("p (k c) -> p k c", k=4)
                sc.cp(dst, src, eng="act")

        def layer_norm(t, gi):
            st = SM.next()
            for h in range(2):
                sc.op("dve", lambda e, h=h, st=st: e.bn_stats(out=st[:, h * 6:(h + 1) * 6], in_=X[t][:, h * 512:(h + 1) * 512]),
                      [X[t][:]], [st[:]])
            sc.op("dve", lambda e, st=st: e.bn_aggr(out=st[:, 16:18], in_=st[:, 0:12]), [st[:]], [st[:]])
            sc.ts(st[:, 18:19], st[:, 17:18], 1e-5, ALU.add)
            sc.act(st[:, 19:20], st[:, 18:19], AF.Sqrt)
            sc.op("dve", lambda e, st=st: e.reciprocal(out=st[:, 20:21], in_=st[:, 19:20]), [st[:]], [st[:]])
            sc.stt(st[:, 21:22], st[:, 16:17], -1.0, st[:, 20:21], ALU.mult, ALU.mult)
            sc.act(X[t][:], X[t][:], AF.Identity, bias=st[:, 21:22], scale=st[:, 20:21])
            sc.tt(X[t][:], X[t][:], LNGB[:, 0, :], ALU.mult)
            sc.tt(X[t][:], X[t][:], LNGB[:, 1, :], ALU.add)

        def load_lngb(l, which):
            sc.dma("sp", "LNGB", [(LNGB[:, 0, :], lnp[l, 2 * which:2 * which + 1, :].broadcast_to([128, D])),
                                  (LNGB[:, 1, :], lnp[l, 2 * which + 1:2 * which + 2, :].broadcast_to([128, D]))])

        first_acc = [True]

        def outproj_partial(l, m, wo):
            for t in range(T):
                g, j = divmod(t, 4)
                for cb in range(2):
                    ps = PS.next()
                    for k in range(2):
                        sc.mm(ps[:, :], YB[g][:, k, j * 128:(j + 1) * 128], wo[:, k, cb * 512:(cb + 1) * 512],
                              start=(k == 0), stop=(k == 1))
                    xs = X[t][:, cb * 512:(cb + 1) * 512]
                    if m == 0:
                        sc.stt(xs, xs, ALPHA, ps[:, :], ALU.mult, ALU.add)
                    else:
                        sc.tt(xs, xs, ps[:, :], ALU.add)

        def dump(m):
            if dbg:
                for g in range(NG):
                    tmp = SF.next()
                    for k in range(2):
                        sc.cp(tmp[:, :], YB[g][:, k, :])
                        sc.dma("sp", tmp.name, [(dbg_d[:, 2 * m + k, g * 512:(g + 1) * 512], tmp[:, :])])

        def mixer_a(l):
            wt = WP.next()
            pairs = []
            wa = load_w(wt, w_in[l, :, A0:A0 + 512], 512, pairs=pairs)
            wo = load_w(wt, w_out[l, 0:256, :], 1024, k=2, off=4096, pairs=pairs)
            sc.dma("pool", wt.name, pairs)
            wsT = MID[0]
            wsr = BIG[1]
            sc.dma("sp", wsr.name + "h", [(wsr[:, 0:512].rearrange("p (h s) -> p h s", h=4),
                                     a_ws[l].rearrange("h t s -> t h s"))])
            ps = PS.next()
            for h in range(4):
                sc.tr(ps[:, h * 128:(h + 1) * 128], wsr[:, h * 128:(h + 1) * 128], IDF)
            sc.tt(wsT[:, 0:512].rearrange("p (h t) -> p h t", h=4), ps[:, :].rearrange("p (h t) -> p h t", h=4),
                  CST[:, 128:256].unsqueeze(1).broadcast_to([128, 4, 128]), ALU.mult)
            for t in range(T):
                g, j = divmod(t, 4)
                ps = PS.next()
                for k in range(8):
                    sc.mm(ps[:, :], XT[g][:, k, j * 128:(j + 1) * 128], wa[:, k, :], start=(k == 0), stop=(k == 7))
                gl = SF.next()
                sc.act(gl[:, :], ps[:, :], AF.Gelu)
                st = SM.next()
                sc.op("dve", lambda e, st=st, gl=gl: e.bn_stats(out=st[:, 0:6], in_=gl[:, 256:512]), [gl[:]], [st[:]])
                sc.op("dve", lambda e, st=st: e.bn_aggr(out=st[:, 16:18], in_=st[:, 0:6]), [st[:]], [st[:]])
                sc.ts(st[:, 18:19], st[:, 17:18], 1e-5, ALU.add)
                sc.act(st[:, 19:20], st[:, 18:19], AF.Sqrt)
                sc.op("dve", lambda e, st=st: e.reciprocal(out=st[:, 20:21], in_=st[:, 19:20]), [st[:]], [st[:]])
                sc.ts(gl[:, 256:512], gl[:, 256:512], st[:, 16:17], ALU.subtract, st[:, 20:21], ALU.mult)
                sc.tt(gl[:, 256:512], gl[:, 256:512], PR[:, 0:256], ALU.mult)
                vn = SB.next()
                sc.tt(vn[:, 0:256], gl[:, 256:512], PR[:, 256:512], ALU.add)
                pm = PS.next()
                for h in range(4):
                    sc.mm(pm[:, h * 64:(h + 1) * 64], wsT[:, h * 128:(h + 1) * 128], vn[:, h * 64:(h + 1) * 64])
                ya = SB.next()
                for h in range(4):
                    sc.stt(ya[:, h * 64:(h + 1) * 64], pm[:, h * 64:(h + 1) * 64], PP[:, 24 + h:25 + h],
                           gl[:, h * 64:(h + 1) * 64], ALU.add, ALU.mult)
                pt = PS.next()
                ptb = pt[:, 0:128].bitcast(BF16)
                for k in range(2):
                    sc.tr(ptb[:, k * 128:(k + 1) * 128], ya[:, k * 128:(k + 1) * 128], IDB[:])
                sc.cp(YB[g][:, :, j * 128:(j + 1) * 128], ptb[:, :].rearrange("p (k c) -> p k c", k=2), eng="act")
            dump(0)
            outproj_partial(l, 0, wo)

        def rope64(dst, src, gs):
            t1 = SF.next(); t2 = SF.next()
            sc.tt(t1[64:128, :], src[64:128, :], COSC[64:128, gs], ALU.mult)
            sc.tt(t2[64:96, :], src[96:128, :], SINC[96:128, gs], ALU.mult)
            sc.tt(t2[96:128, :], src[64:96, :], SINC[64:96, gs], ALU.mult)
            sc.tt(dst, t1[64:128, :], t2[64:128, :], ALU.add)

        def mixer_b(l):
            wt = WP.next()
            pairs = []
            wcq = load_w(wt, w_in[l, :, B0:B0 + 192], 192, pairs=pairs)
            wckv = load_w(wt, w_in[l, :, B0 + 192:B0 + 320], 128, off=1536, pairs=pairs)
            wo = load_w(wt, w_out[l, 256:512, :], 1024, k=2, off=3584, pairs=pairs)
            wkv = wt[:, 6656:6656 + 512]
            pairs.append((wkv, w_ukv[l]))
            krs = load_w(wt, w_in[l, :, B0 + 320:B0 + 352], 32, off=7168, pairs=pairs)
            uqs = wt[:, 7424:7424 + 768].rearrange("p (k c) -> p k c", k=2)
            pairs.append((uqs[:, 0, :], w_uq[l, 0:128, :]))
            pairs.append((uqs[0:64, 1, :], w_uq[l, 128:192, :]))
            sc.dma("pool", wt.name, pairs)
            wkr = wt[:, 2560:2560 + 1024].rearrange("p (k c) -> p k c", k=8)
            sc.memset(wt[:, 2560:3584], 0.0)
            sc.cp(wkr[:, :, 64:96:2], krs[:, :, 0:16])
            sc.cp(wkr[:, :, 96:128:2], krs[:, :, 16:32])
            wq = wt[:, 5632:5632 + 1024].rearrange("p (k h c) -> p k h c", k=2, h=4)
            sc.memset(wt[:, 5632:6656], 0.0)
            uq4 = wt[:, 7424:7424 + 768].rearrange("p (k h c) -> p k h c", k=2, h=4)
            for kc, rows in ((0, 128), (1, 64)):
                sc.cp(wq[0:rows, kc, :, 0:64], uq4[0:rows, kc, :, 0:64])
                sc.cp(wq[0:rows, kc, :, 64:96:2], uq4[0:rows, kc, :, 64:80])
                sc.cp(wq[0:rows, kc, :, 96:128:2], uq4[0:rows, kc, :, 80:96])

            CQ0 = MID[0]; CQ1K = MID[1]; CKVN = MID[2]
            for g in range(NG):
                gs = slice(g * 512, (g + 1) * 512)
                pq0 = PS.next(); pq1 = PS.next(); pkv = PS.next(); pkr = PS.next()
                for k in range(8):
                    sc.mm(pq0[:, :], wcq[:, k, 0:128], XT[g][:, k, :], start=(k == 0), stop=(k == 7))
                for k in range(8):
                    sc.mm(pq1[0:64, :], wcq[:, k, 128:192], XT[g][:, k, :], start=(k == 0), stop=(k == 7))
                for k in range(8):
                    sc.mm(pkv[:, :], wckv[:, k, :], XT[g][:, k, :], start=(k == 0), stop=(k == 7))
                for k in range(8):
                    sc.mm(pkr[:, :], wkr[:, k, :], XT[g][:, k, :], start=(k == 0), stop=(k == 7))
                s0 = SF.next(); s1 = SF.next(); s2 = SF.next()
                sc.act(s0[:, :], pq0[:, :], AF.Square)
                sc.act(s1[0:64, :], pq1[0:64, :], AF.Square)
                sc.act(s2[:, :], pkv[:, :], AF.Square)
                pss = PS.next()
                sc.mm(pss[:, :], ONEF, s0[:, :], start=True, stop=False)
                sc.mm(pss[:, :], CST[0:64, 384:512], s1[0:64, :], start=False, stop=True)
                rq = SF.next()
                sc.ts(rq[:, :], pss[:, :], 1.0 / 192, ALU.mult, 1e-6, ALU.add)
                sc.act(rq[:, :], rq[:, :], AF.Sqrt)
                sc.op("dve", lambda e, rq=rq: e.reciprocal(out=rq[:, :], in_=rq[:, :]), [rq[:]], [rq[:]])
                sc.stt(CQ0[:, gs], pq0[:, :], PP[:, 0:1], rq[:, :], ALU.mult, ALU.mult)
                sc.stt(CQ1K[0:64, gs], pq1[0:64, :], PP[0:64, 1:2], rq[0:64, :], ALU.mult, ALU.mult)
                pss2 = PS.next()
                sc.mm(pss2[:, :], ONEF, s2[:, :])
                rk = SF.next()
                sc.ts(rk[:, :], pss2[:, :], 1.0 / 128, ALU.mult, 1e-6, ALU.add)
                sc.act(rk[:, :], rk[:, :], AF.Sqrt)
                sc.op("dve", lambda e, rk=rk: e.reciprocal(out=rk[:, :], in_=rk[:, :]), [rk[:]], [rk[:]])
                sc.stt(CKVN[:, gs], pkv[:, :], PP[:, 2:3], rk[:, :], ALU.mult, ALU.mult)
                rope64(CQ1K[64:128, gs], pkr, gs)

            scale = 96.0 ** -0.5
            for h in range(4):
                QT = MID[3]; KT = MID[4]
                va = BIG[h % 2][:, :].bitcast(BF16)[:, 0:2048].rearrange("p (t c) -> p t c", t=T)
                for g in range(NG):
                    gs = slice(g * 512, (g + 1) * 512)
                    pq = PS.next()
                    sc.mm(pq[:, :], wq[:, 0, h, :], CQ0[:, gs], start=True, stop=False)
                    sc.mm(pq[:, :], wq[0:64, 1, h, :], CQ1K[0:64, gs], start=False, stop=True)
                    sc.cp(QT[0:64, gs], pq[0:64, :], eng="act")
                    rope64(QT[64:128, gs], pq, gs)
                    pk = PS.next()
                    sc.mm(pk[0:64, :], wkv[:, h * 128:h * 128 + 64], CKVN[:, gs])
                    sc.cp(KT[0:64, gs], pk[0:64, :], eng="act")
                    sc.cp(KT[64:128, gs], CQ1K[64:128, gs], eng="dve")
                    pv = PS.next()
                    for j in range(4):
                        t = g * 4 + j
                        sc.mm(pv[:, j * 64:(j + 1) * 64], CKVN[:, t * 128:(t + 1) * 128], wkv[:, h * 128 + 64:h * 128 + 128])
                    sc.cp(va[:, g * 4:(g + 1) * 4, 0:64], pv[:, 0:256].rearrange("p (j c) -> p j c", j=4), eng="act")
                    if h < 2:
                        sc.memset(va[:, g * 4:(g + 1) * 4, 64:128], 1.0, eng="pool")
                for Qb in range(4):
                    po = PSL.next()
                    nkb = 4 * Qb + 4
                    for kb in range(nkb):
                        qs = max(Qb * 512, kb * 128)
                        qe = (Qb + 1) * 512
                        n = qe - qs
                        ps = PS.next()
                        sc.mm(ps[:, 0:n], KT[:, kb * 128:(kb + 1) * 128], QT[:, qs:qe])
                        pt = SB.next()
                        sc.act(pt[:, 0:n], ps[:, 0:n], AF.Exp, scale=scale)
                        if kb * 128 >= Qb * 512:
                            sc.tt(pt[:, 0:128], pt[:, 0:128], TRIB[:], ALU.mult, eng="pool")
                        sc.mm(po[:, qs - Qb * 512:512], va[:, kb, :], pt[:, 0:n], start=(kb == 0), stop=(kb == nkb - 1))
                    rd = SF.next()
                    sc.cp(rd[0:64, :], po[64:128, :], eng="act")
                    sc.op("dve", lambda e, rd=rd: e.reciprocal(out=rd[0:64, :], in_=rd[0:64, :]), [rd[:]], [rd[:]])
                    hp = (h % 2) * 64
                    sc.tt(YB[Qb][hp:hp + 64, h // 2, :], po[0:64, :], rd[0:64, :], ALU.mult)
            dump(1)
            outproj_partial(l, 1, wo)

        def mixer_c(l):
            wt = WP.next()
            pairs = []
            wc = load_w(wt, w_in[l, :, C0:C0 + 768], 768, pairs=pairs)
            wo = load_w(wt, w_out[l, 512:768, :], 1024, k=2, off=6144, pairs=pairs)
            sc.dma("pool", wt.name, pairs)
            QC = [MID[0], MID[1]]; KC = [MID[2], MID[3]]; VCt = MID[4]
            for g in range(NG):
                gs = slice(g * 512, (g + 1) * 512)
                for ci in range(4):
                    ps = PS.next()
                    for k in range(8):
                        sc.mm(ps[:, :], wc[:, k, ci * 128:(ci + 1) * 128], XT[g][:, k, :], start=(k == 0), stop=(k == 7))
                    dst = (QC if ci < 2 else KC)[ci % 2]
                    t1 = SF.next(); t2 = SF.next()
                    sc.tt(t1[:, :], ps[:, :], COSC[:, gs], ALU.mult)
                    for q in range(4):
                        src = (q ^ 1) * 32
                        sc.tt(t2[q * 32:(q + 1) * 32, :], ps[src:src + 32, :], SINC[src:src + 32, gs], ALU.mult)
                    sc.tt(dst[:, gs], t1[:, :], t2[:, :], ALU.add)
            vcnt = 0
            for h in range(4):
                c, hp = h // 2, (h % 2) * 64
                if h % 2 == 0:
                    for g in range(NG):
                        ps = PS.next()
                        for k in range(8):
                            sc.mm(ps[:, :], wc[:, k, (4 + c) * 128:(5 + c) * 128], XT[g][:, k, :], start=(k == 0), stop=(k == 7))
                        sc.cp(VCt[:, g * 512:(g + 1) * 512], ps[:, :], eng="act")
                ACC = BIG[1]
                first = True
                for d in (1, 4, 16):
                    nb = 16 // d
                    va = BIG[0][:, :].bitcast(BF16)[:, (vcnt % 2) * 2048:(vcnt % 2 + 1) * 2048].rearrange("p (b c) -> p b c", b=16)
                    vcnt += 1

                    def sel(r, n, d=d):
                        st = n * 128 * d + r
                        return slice(st, st + 127 * d + 1, d)
                    blocks = [(r, n) for r in range(d) for n in range(nb)]
                    for bi in range(0, 16, 4):
                        pt = PS.next()
                        ptb = pt[:, 0:128].bitcast(BF16)
                        for q in range(4):
                            r, n = blocks[bi + q]
                            sc.tr(ptb[:, q * 64:(q + 1) * 64], VCt[hp:hp + 64, sel(r, n)], IDB[hp:hp + 64, hp:hp + 64])
                        sc.cp(va[:, bi:bi + 4, 0:64], ptb[:, :].rearrange("p (q c) -> p q c", q=4), eng="act")
                    if vcnt <= 2:
                        sc.memset(va[:, :, 64:128], 1.0, eng="pool")
                    it = 0
                    if d < 16:
                        for r in range(d):
                            for n0 in range(0, nb, 2):
                                bi0 = r * nb + n0
                                ps = PS.next()
                                for q in range(2):
                                    n = n0 + q
                                    if n > 0:
                                        sc.mm(ps[:, q * 256:q * 256 + 128], KC[c][hp:hp + 64, sel(r, n - 1)], QC[c][hp:hp + 64, sel(r, n)])
                                    sc.mm(ps[:, q * 256 + 128:q * 256 + 256], KC[c][hp:hp + 64, sel(r, n)], QC[c][hp:hp + 64, sel(r, n)])
                                lo = 128 if n0 == 0 else 0
                                pt = SB.next()
                                sc.act(pt[:, lo:512], ps[:, lo:512], AF.Exp, scale=0.125)
                                sc.tt(pt[:, lo:512], pt[:, lo:512], MSK4[:, lo:512], ALU.mult, eng=("pool" if it % 2 == 0 else "dve"))
                                it += 1
                                po = PS.next()
                                for q in range(2):
                                    n = n0 + q
                                    bi = bi0 + q
                                    if n > 0:
                                        sc.mm(po[:, q * 128:(q + 1) * 128], va[:, bi - 1, :], pt[:, q * 256:q * 256 + 128], start=True, stop=False)
                                    sc.mm(po[:, q * 128:(q + 1) * 128], va[:, bi, :], pt[:, q * 256 + 128:q * 256 + 256], start=(n == 0), stop=True)
                                st0 = n0 * 128 * d + r
                                dst = ACC[:, st0:st0 + 255 * d + 1:d]
                                if first:
                                    sc.cp(dst, po[:, 0:256], eng="act")
                                else:
                                    sc.tt(dst, dst, po[:, 0:256], ALU.add)
                    else:
                        for r0 in range(0, 16, 4):
                            ps = PS.next()
                            for q in range(4):
                                sc.mm(ps[:, q * 128:(q + 1) * 128], KC[c][hp:hp + 64, sel(r0 + q, 0)], QC[c][hp:hp + 64, sel(r0 + q, 0)])
                            pt = SB.next()
                            sc.act(pt[:, 0:512], ps[:, 0:512], AF.Exp, scale=0.125)
                            sc.tt(pt[:, 0:512], pt[:, 0:512], MSKT4[:, 0:512], ALU.mult, eng=("pool" if it % 2 == 0 else "dve"))
                            it += 1
                            po = PS.next()
                            for q in range(4):
                                sc.mm(po[:, q * 128:(q + 1) * 128], va[:, r0 + q, :], pt[:, q * 128:(q + 1) * 128])
                            dst = ACC[:, :].rearrange("p (i r) -> p r i", r=16)[:, r0:r0 + 4, :]
                            sc.tt(dst, dst, po[:, 0:512].rearrange("p (q i) -> p q i", q=4), ALU.add)
                    first = False
                for g in range(NG):
                    gs = slice(g * 512, (g + 1) * 512)
                    rd = SF.next()
                    sc.cp(rd[0:64, :], ACC[64:128, gs], eng="act")
                    sc.op("dve", lambda e, rd=rd: e.reciprocal(out=rd[0:64, :], in_=rd[0:64, :]), [rd[:]], [rd[:]])
                    sc.tt(YB[g][hp:hp + 64, c, :], ACC[0:64, gs], rd[0:64, :], ALU.mult)
            dump(2)
            outproj_partial(l, 2, wo)

        def mixer_d(l):
            wt = WP.next()
            pairs = []
            wqk = load_w(wt, w_in[l, :, D0:D0 + 512], 512, pairs=pairs)
            wvo = load_w(wt, w_in[l, :, D0 + 512:D0 + 1024], 512, off=4096, pairs=pairs)
            sc.dma("pool", wt.name, pairs)
            wg8 = load_w(WG8, w_in[l, :, D0 + 1024:D0 + 1032], 8)
            wo = load_w(WOD, w_out[l, 768:1024, :], 1024, k=2)
            pg = PSL.next()
            for t in range(T):
                g, j = divmod(t, 4)
                for k in range(8):
                    sc.mm(pg[:, t * 8:(t + 1) * 8], XT[g][:, k, j * 128:(j + 1) * 128], wg8[:, k, :], start=(k == 0), stop=(k == 7))

            def tb(i):
                return GB[:, i * 64:(i + 1) * 64]

            def tb3(i):
                return GB[:, i * 64:(i + 1) * 64].rearrange("p (c h) -> p c h", h=4)
            pg3 = pg[:, 0:128].rearrange("p (c e) -> p c e", e=8)
            GI, GF, AB, EX, LN_, FC, BI_, BB, AA, WK, THR, DEC, TOT = range(13)
            sc.tt(tb3(GI), pg3[:, :, 0:4], PR[:, 512:516].unsqueeze(1).broadcast_to([128, 16, 4]), ALU.add)
            sc.tt(tb3(GF), pg3[:, :, 4:8], PR[:, 516:520].unsqueeze(1).broadcast_to([128, 16, 4]), ALU.add)
            sc.act(tb(AB), tb(GF), AF.Abs)
            sc.act(tb(EX), tb(AB), AF.Exp, scale=-1.0)
            sc.act(tb(LN_), tb(EX), AF.Ln, bias=1.0)
            sc.stt(tb(FC), tb(GF), 0.0, tb(LN_), ALU.min, ALU.subtract)
            if DSTOP == 1:
                return
            pc = PS.next()
            sc.mm(pc[:, 0:64], TRIF, tb(FC))
            sc.mm(pc[:, 64:128], ONEF, tb(FC))
            sc.cp(tb(TOT), pc[:, 64:128])
            for h in range(4):
                v_in = GB[:, TOT * 64 + h:TOT * 64 + 64:4]
                v_out = GB[:, BI_ * 64 + h:BI_ * 64 + 64:4]
                sc.op("dve", lambda e, v_in=v_in, v_out=v_out: e.tensor_tensor_scan(
                    out=v_out, data0=CST[:, 384:400], data1=v_in, initial=0.0, op0=ALU.mult, op1=ALU.add), [GB[:], CST[:]], [GB[:]])
            sc.tt(tb(BI_), tb(BI_), tb(TOT), ALU.subtract)
            sc.tt(tb(BB), pc[:, 0:64], tb(BI_), ALU.add)
            sc.tt(tb(AA), tb(GI), tb(BB), ALU.subtract)
            if DSTOP == 2:
                return
            pa = PS.next()
            sc.tr(pa[0:64, 0:128], tb(AA), IDF)
            cm = SM.next()
            sc.op("dve", lambda e: e.tensor_reduce(out=cm[0:64, 0:1], in_=pa[0:64, 0:128], axis=mybir.AxisListType.X, op=ALU.max),
                  [pa[:]], [cm[:]])
            pb = PS.next()
            sc.tr(pb[0:1, 0:64], cm[0:64, 0:1], CST[0:64, 0:64])
            R = SF.next()
            sc.cp(R[0:1, 128:192], pb[0:1, 0:64])
            for h in range(4):
                sc.op("dve", lambda e, h=h: e.tensor_tensor_scan(
                    out=R[0:1, h:64:4], data0=R[0:1, 128 + h:192:4], data1=CST[0:1, 260:276], initial=0.0,
                    op0=ALU.max, op1=ALU.max), [R[:], CST[:]], [R[:]])
            sc.memset(R[0:1, 64:68], 0.0)
            sc.cp(R[0:1, 68:128], R[0:1, 0:60])
            pm = PS.next()
            sc.mm(pm[:, 0:128], CST[0:1, 384:512], R[0:1, 0:128])
            MR = SF.next()
            sc.cp(MR[:, 0:128], pm[:, 0:128])
            sc.tt(tb(WK), tb(AA), MR[:, 0:64], ALU.subtract)
            sc.act(tb(WK), tb(WK), AF.Exp)
            sc.ts(tb(WK), tb(WK), 0.125, ALU.mult)
            sc.tt(tb(THR), tb(BB), MR[:, 0:64], ALU.add)
            sc.act(tb(THR), tb(THR), AF.Exp, scale=-1.0)
            sc.tt(tb(DEC), MR[:, 64:128], MR[:, 0:64], ALU.subtract)
            sc.act(tb(DEC), tb(DEC), AF.Exp)
            if DSTOP == 3:
                return
            QK = [MID[0], MID[1], MID[2], MID[3]]
            for ci in range(4):
                Z = BIG[0]
                for g in range(NG):
                    ps = PS.next()
                    for k in range(8):
                        sc.mm(ps[:, :], wqk[:, k, ci * 128:(ci + 1) * 128], XT[g][:, k, :], start=(k == 0), stop=(k == 7))
                    sc.cp(Z[:, g * 512:(g + 1) * 512], ps[:, :], eng="act")
                A = BIG[1]
                sc.ts(A[:, :], Z[:, :], PP[:, 3 + ci * 4 + 3:4 + ci * 4 + 3], ALU.mult, PP[:, 19 + ci:20 + ci], ALU.add)
                for j in range(3):
                    sh = 3 - j
                    sc.stt(A[:, sh:S], Z[:, 0:S - sh], PP[:, 3 + ci * 4 + j:4 + ci * 4 + j], A[:, sh:S], ALU.mult, ALU.add)
                sc.act(QK[ci][:, :], A[:, :], AF.Silu)
            if DSTOP == 4:
                return
            CS = SF.next()
            cs3 = CS[:, 0:256].rearrange("p (a c) -> p a c", a=2)
            sc.memset(CS[:, 0:256], 0.0)
            SFd = [BIG[2][:, i * 512:(i + 1) * 512] for i in range(4)]
            for c in range(T):
                g, j = divmod(c, 4)
                cs = slice(c * 128, (c + 1) * 128)
                pvo = PS.next()
                for k in range(8):
                    sc.mm(pvo[:, :], XT[g][:, k, j * 128:(j + 1) * 128], wvo[:, k, :], start=(k == 0), stop=(k == 7))
                vaug = SB.next()
                va3 = vaug[:, 0:512].rearrange("p (h c) -> p h c", h=4)
                sc.cp(va3[:, :, 0:64], pvo[:, 0:256].rearrange("p (h c) -> p h c", h=4), eng="act")
                sc.memset(va3[:, :, 64:128], 1.0, eng="pool")
                so = MID[4][:, (c % 2) * 256:(c % 2) * 256 + 256]
                sc.act(so, pvo[:, 256:512], AF.Sigmoid)
                pk = PS.next()
                pkb = pk[:, 0:128].bitcast(BF16)
                for i in range(2):
                    sc.tr(pkb[:, i * 128:(i + 1) * 128], QK[2 + i][:, cs], IDB[:])
                kw = SB.next()
                sc.tt(kw[:, 0:256].rearrange("p (h c) -> p h c", h=4), pkb[:, :].rearrange("p (h c) -> p h c", h=4),
                      GB[:, WK * 64 + c * 4:WK * 64 + c * 4 + 4].unsqueeze(2).broadcast_to([128, 4, 64]), ALU.mult)
                pS2 = [PS.next(), PS.next()]
                for h in range(4):
                    hp = (h % 2) * 64
                    sc.mm(pS2[h % 2][:, (h // 2) * 128:(h // 2 + 1) * 128], QK[2 + h // 2][hp:hp + 64, cs], QK[h // 2][hp:hp + 64, cs])
                sq = SB.next()
                for h in range(4):
                    sc.stt(sq[:, h * 128:(h + 1) * 128], pS2[h % 2][:, (h // 2) * 128:(h // 2 + 1) * 128],
                           GB[:, WK * 64 + c * 4 + h:WK * 64 + c * 4 + h + 1], TRIB[:], ALU.mult, ALU.mult)
                for hf in range(2):
                    rows = slice(hf * 64, hf * 64 + 64)
                    dec = GB[rows, DEC * 64 + c * 4 + hf:DEC * 64 + c * 4 + 4:2]
                    sc.tt(cs3[rows, :, :], cs3[rows, :, :], dec.unsqueeze(2).broadcast_to([64, 2, 128]), ALU.mult)
                cb = SB.next()
                cb3 = cb[:, 0:256].rearrange("p (a c) -> p a c", a=2)
                sc.cp(cb[:, 0:256], CS[:, 0:256], eng="act")
                ph2 = [PS.next(), PS.next()]
                for h in range(4):
                    hp = (h % 2) * 64
                    po_ = ph2[h % 2][:, (h // 2) * 128:(h // 2 + 1) * 128]
                    sc.mm(po_, sq[:, h * 128:(h + 1) * 128], va3[:, h, :], start=True, stop=False)
                    sc.mm(po_, QK[h // 2][hp:hp + 64, cs], cb3[hp:hp + 64, h // 2, :], start=False, stop=True)
                pu = PS.next()
                for a in range(2):
                    sc.mm(pu[:, a * 256:(a + 1) * 256], kw[:, a * 128:(a + 1) * 128], vaug[:, a * 256:(a + 1) * 256])
                pu3 = pu[:, 0:512].rearrange("p (a c) -> p a c", a=2)
                sc.tt(cs3[0:64, :, :], cs3[0:64, :, :], pu3[0:64, :, 0:128], ALU.add)
                sc.tt(cs3[64:128, :, :], cs3[64:128, :, :], pu3[64:128, :, 128:256], ALU.add)
                dn = SM.next()
                for par in range(2):
                    p3 = ph2[par][:, 0:256].rearrange("p (a c) -> p a c", a=2)
                    sc.act(dn[:, par:4:2], p3[:, :, 64], AF.Abs)
                sc.tt(dn[:, 0:4], dn[:, 0:4], GB[:, THR * 64 + c * 4:THR * 64 + c * 4 + 4], ALU.max)
                sc.op("dve", lambda e, dn=dn: e.reciprocal(out=dn[:, 4:8], in_=dn[:, 0:4]), [dn[:]], [dn[:]])
                hy = SFd[c % 4]
                hy3 = hy[:, 0:256].rearrange("p (h c) -> p h c", h=4)
                for par in range(2):
                    p3 = ph2[par][:, 0:256].rearrange("p (a c) -> p a c", a=2)
                    sc.tt(hy3[:, par:4:2, :], p3[:, :, 0:64],
                          dn[:, 4 + par:8:2].unsqueeze(2).broadcast_to([128, 2, 64]), ALU.mult)
                yd = SB.next()
                sc.tt(yd[:, 0:256], hy[:, 0:256], so, ALU.mult)
                pt = PS.next()
                ptb = pt[:, 0:128].bitcast(BF16)
                for k in range(2):
                    sc.tr(ptb[:, k * 128:(k + 1) * 128], yd[:, k * 128:(k + 1) * 128], IDB[:])
                sc.cp(YB[g][:, :, j * 128:(j + 1) * 128], ptb[:, :].rearrange("p (k c) -> p k c", k=2), eng="act")
            dump(3)
            outproj_partial(l, 3, wo)

        def ffn(l):
            chunks = [(i * 512, 512) for i in range(5)] + [(2560, 256)]
            for ci, (c0, cw) in enumerate(chunks):
                nhb = cw // 128
                wt = WP.next()
                pairs = []
                wg = load_w(wt, w_gate[l, :, c0:c0 + cw], cw, pairs=pairs)
                wu = load_w(wt, w_up[l, :, c0:c0 + cw], cw, off=4096, pairs=pairs)
                sc.dma("pool", wt.name, pairs)
                wt2 = BIG[1 + ci % 2][:, :].bitcast(BF16)
                vd = wt2[:, 0:nhb * 1024].rearrange("p (k c) -> p k c", k=nhb)
                sc.dma("pool", f"BIG{1 + ci % 2}", [(vd, w_down[l, c0:c0 + cw, :].rearrange("(k p) c -> p k c", p=128))])
                wd = vd
                for g in range(NG):
                    hT = MID[(ci * NG + g) % 2]
                    for hb in range(nhb):
                        pg_ = PS.next(); pu_ = PS.next()
                        for k in range(8):
                            sc.mm(pg_[:, :], wg[:, k, hb * 128:(hb + 1) * 128], XT[g][:, k, :], start=(k == 0), stop=(k == 7))
                        for k in range(8):
                            sc.mm(pu_[:, :], wu[:, k, hb * 128:(hb + 1) * 128], XT[g][:, k, :], start=(k == 0), stop=(k == 7))
                        sl = SF.next()
                        sc.act(sl[:, :], pg_[:, :], AF.Silu)
                        sc.tt(hT[:, hb * 512:(hb + 1) * 512], sl[:, :], pu_[:, :], ALU.mult)
                    for j in range(4):
                        t = g * 4 + j
                        for cb in range(2):
                            ps = PS.next()
                            for hb in range(nhb):
                                sc.mm(ps[:, :], hT[:, hb * 512 + j * 128:hb * 512 + (j + 1) * 128], wd[:, hb, cb * 512:(cb + 1) * 512],
                                      start=(hb == 0), stop=(hb == nhb - 1))
                            xs = X[t][:, cb * 512:(cb + 1) * 512]
                            if ci == 0:
                                sc.stt(xs, xs, ALPHA, ps[:, :], ALU.mult, ALU.add)
                            else:
                                sc.tt(xs, xs, ps[:, :], ALU.add)

        def rope_tables(s):
            PI = XTt[0][:, :].bitcast(F32)
            PIi = XTt[0][:, :].bitcast(I32)
            PF = XTt[1][:, :].bitcast(F32)
            RR = XTt[2][:, :].bitcast(F32)
            KK = XTt[3][:, :].bitcast(F32)
            KKi = XTt[3][:, :].bitcast(I32)
            sc.dma("sp", "XT0", [(PIi, pos_d[s:s + 1, :].broadcast_to([128, S]))])
            sc.cp(PF, PIi)
            two_pi = 2.0 * math.pi
            c1 = 6.28125
            c2 = float(np.float32(two_pi - c1))
            c3 = float(two_pi - c1 - c2)
            sc.ts(RR, PF, CST[:, 513:514], ALU.mult)
            for shift, dst, sgn in ((0.0, SINC, True), (math.pi / 2, COSC, False)):
                sc.ts(KKi, RR, shift, ALU.add, 1.0 / two_pi, ALU.mult)
                sc.cp(PI, KKi)
                sc.stt(KK, PI, -c1, RR, ALU.mult, ALU.add)
                sc.stt(KK, PI, -c2, KK, ALU.mult, ALU.add)
                sc.stt(KK, PI, -c3, KK, ALU.mult, ALU.add)
                sc.ts(KK, KK, shift, ALU.add)
                sc.ts(KK, KK, math.pi, ALU.min, -math.pi, ALU.max)
                if sgn:
                    sc.act(KK, KK, AF.Sin)
                    sc.ts(dst[:, :], KK, CST[:, 514:515], ALU.mult)
                else:
                    sc.act(dst[:, :], KK, AF.Sin)

        for s in range(NS):
            rope_tables(s)
            for q in range(4):
                sc.dma("sp", f"Xld{q}", [(X[q * 4 + i][:, :], x_d[s, (q * 4 + i) * 128:(q * 4 + i + 1) * 128, :]) for i in range(4)])
            for t in range(T):
                make_xt(t)
            for l in range(NL):
                sc.dma("sp", "PP", [(PP[:, :], pp_d[l])])
                sc.dma("sp", "PR", [(PR[:, :], pr_d[l:l + 1, :].broadcast_to([128, NPR]))])
                acc_m = [0]
                for nm, fn in (("a", mixer_a), ("b", mixer_b), ("c", mixer_c), ("d", mixer_d)):
                    if dbg is None or nm in dbg:
                        fn(l)
                load_lngb(l, 0)
                for t in range(T):
                    layer_norm(t, 0)
                    make_xt(t)
                load_lngb(l, 1)
                if dbg is None or "f" in dbg:
                    ffn(l)
                for t in range(T):
                    layer_norm(t, 1)
                    if l < NL - 1:
                        make_xt(t)
            for q in range(4):
                sc.dma("sp", f"Xld{q}", [(out_d[s, (q * 4 + i) * 128:(q * 4 + i + 1) * 128, :], X[q * 4 + i][:, :]) for i in range(4)])
        sc.op("sp", lambda e: e.nop(), [X[t][:] for t in range(T)], [X[t][:] for t in range(T)])
        nops = sc.emit()
    return nc, nops


def make_consts():
    c = np.zeros((128, NCONST), np.float32)
    p = np.arange(128)
    c[:, 0:128] = np.eye(128, dtype=np.float32)
    c[:, 128:256] = (p[:, None] <= p[None, :]).astype(np.float32)
    c[:, 256:384] = (p[:, None] >= p[None, :]).astype(np.float32)
    c[:, 384:512] = 1.0
    th = np.float32(10000.0)
    c[:, 512] = np.power(th, -(np.arange(16, dtype=np.float32) / np.float32(16)))[p % 16]
    c[:, 513] = np.power(th, -(np.arange(32, dtype=np.float32) / np.float32(32)))[p % 32]
    c[:, 514] = np.where((p % 64) < 32, 1.0, -1.0)
    return c


def prep_inputs(inp, NL=4):
    f = lambda a: np.ascontiguousarray(np.asarray(a, dtype=np.float32))
    lnp = np.stack([f(inp["ln1_g"]), f(inp["ln1_b"]), f(inp["ln2_g"]), f(inp["ln2_b"])], axis=1)[:NL]
    pp = np.zeros((NL, 128, NPP), np.float32)
    pr = np.zeros((NL, NPR), np.float32)
    for l in range(NL):
        qn = f(inp["b_q_norm"])[l]
        pp[l, :, 0] = qn[0:128]
        pp[l, 0:64, 1] = qn[128:192]
        pp[l, :, 2] = f(inp["b_kv_norm"])[l]
        cw = f(inp["d_conv_w"])[l]
        for ci in range(4):
            for j in range(4):
                pp[l, :, 3 + ci * 4 + j] = cw[j, ci * 128:(ci + 1) * 128]
            pp[l, :, 19 + ci] = f(inp["d_conv_b"])[l, ci * 128:(ci + 1) * 128]
        pp[l, :, 24:28] = f(inp["a_bs"])[l].T
        pr[l, 0:256] = f(inp["a_ln_g"])[l]
        pr[l, 256:512] = f(inp["a_ln_b"])[l]
        pr[l, 512:516] = f(inp["d_igate_b"])[l]
        pr[l, 516:520] = f(inp["d_fgate_b"])[l]
    shared = dict(w_in=f(inp["w_in"])[:NL], a_ws=f(inp["a_ws"])[:NL], b_w_uq=f(inp["b_w_uq"])[:NL],
                  b_w_ukv=f(inp["b_w_ukv"])[:NL], w_out=f(inp["w_out"])[:NL], w_gate=f(inp["w_gate"])[:NL],
                  w_up=f(inp["w_up"])[:NL], w_down=f(inp["w_down"])[:NL], lnp=np.ascontiguousarray(lnp),
                  pp=pp, pr=pr, cst=make_consts())
    return shared


_CACHE = {}


def kernel(**inputs):
    n = 8
    x = np.asarray(inputs["x"], dtype=np.float32)
    pos = np.asarray(inputs["positions"], dtype=np.int32)
    shared = prep_inputs(inputs)
    if "nc" not in _CACHE:
        _CACHE["nc"] = build(4, 4)[0]
    nc = _CACHE["nc"]
    in_maps = []
    for c in range(n):
        m = dict(shared)
        m["x"] = np.ascontiguousarray(x[c * 4:(c + 1) * 4])
        m["pos"] = np.ascontiguousarray(pos[c * 4:(c + 1) * 4])
        in_maps.append(m)
    res = run_bass_kernel_spmd(nc, in_maps, core_ids=list(range(n)))
    return np.concatenate([r["out"] for r in res.results], axis=0)
```
